# Optimizing a Trainium2 kernel written in Bass

```python
import jax, jax.numpy as jnp
from jax import lax
import numpy as np

D_MODEL = 2048
BATCH = 1
SEQ = 8192
DEPTH = 1

BLK = 128
EPS = 1e-6
NEG = -1e30
TINY = 1e-30
FORCE = 1e4

A_DILATIONS = (1, 4, 16)
A_WINDOWS = (128, 512, 2048)
A_GROUPS = 3
A_HEADS = 4
A_HD = 128
B_HEADS = 16
B_KV = 2
B_REP = B_HEADS // B_KV
B_HD = 64
CMP_LEN = 32
CMP_STRIDE = 16
CMP_HIDDEN = 128
SLC_LEN = 64
N_SEL = 16
WIN = 512
N_BRANCH = 2
X_HEADS = 4
X_HD = 128
N_MEM = 256
PEER_HEADS = 8
PEER_DK = 256
N_KEYS = 128
N_EXPERTS = N_KEYS * N_KEYS
PEER_TOPK = 16
PEER_V_SCALE = 0.25

A_QKV_COLS = 3 * A_GROUPS * A_HEADS * A_HD
B_Q_COLS = B_HEADS * B_HD
B_KV_COLS = 3 * 2 * B_KV * B_HD
B_GATE_COLS = B_HEADS * 3
MERGE_COLS = N_BRANCH * D_MODEL
IN_COLS = A_QKV_COLS + B_Q_COLS + B_KV_COLS + B_GATE_COLS + MERGE_COLS
SPLITS = [A_QKV_COLS, A_QKV_COLS + B_Q_COLS, A_QKV_COLS + B_Q_COLS + B_KV_COLS, A_QKV_COLS + B_Q_COLS + B_KV_COLS + B_GATE_COLS]

kernel_name = 'hybrid_dilated_nsa_peer_block'


def rmsnorm(x, g):
    xf = x.astype(jnp.float32)
    y = xf * lax.rsqrt(jnp.mean(xf * xf, axis=-1, keepdims=True) + EPS)
    return (y * g.astype(jnp.float32)).astype(x.dtype)


def alibi_slopes(n):
    return jnp.power(2.0, -8.0 * (jnp.arange(n, dtype=jnp.float32) + 1.0) / n)


def masked_probs(s, ok):
    s = jnp.where(ok, s, NEG)
    m = jnp.max(s, axis=-1, keepdims=True)
    p = jnp.where(ok, jnp.exp(s - m), 0.0)
    return p / jnp.maximum(jnp.sum(p, axis=-1, keepdims=True), TINY)


def dilated_window_attn(q, k, v, slopes, dil, steps):
    B, T, H, hd = q.shape
    L = T // dil
    Lp = -(-L // BLK) * BLK
    nb = Lp // BLK
    N = B * dil

    def strided(a):
        a = a.reshape(B, L, dil, H, hd).transpose(0, 2, 3, 1, 4).reshape(N, H, L, hd)
        return jnp.pad(a, ((0, 0), (0, 0), (0, Lp - L), (0, 0))).reshape(N, H, nb, BLK, hd)

    qb, kb, vb = strided(q), strided(k), strided(v)

    def with_prev(a):
        prev = jnp.concatenate([jnp.zeros_like(a[:, :, :1]), a[:, :, :-1]], axis=2)
        return jnp.concatenate([prev, a], axis=3)

    k2, v2 = with_prev(kb), with_prev(vb)
    s = jnp.einsum('nhbqd,nhbkd->nhbqk', qb, k2).astype(jnp.float32) * (hd ** -0.5)
    rel = jnp.arange(BLK)[:, None] + BLK - jnp.arange(2 * BLK)[None, :]
    ok = (rel >= 0) & (rel <= steps)
    ok = ok[None] & ((jnp.arange(nb)[:, None, None] > 0) | (jnp.arange(2 * BLK)[None, None, :] >= BLK))
    s = s - slopes.astype(jnp.float32)[:, None, None, None] * (rel * dil).astype(jnp.float32)
    s = jnp.where(ok, s, NEG)
    m = jnp.max(s, axis=-1, keepdims=True)
    p = jnp.exp(s - m)
    den = jnp.sum(p, axis=-1, keepdims=True)
    o = jnp.einsum('nhbqk,nhbkd->nhbqd', p, v2) / den
    lse = (m + jnp.log(den))[..., 0]
    o = o.reshape(N, H, Lp, hd)[:, :, :L].reshape(B, dil, H, L, hd).transpose(0, 3, 1, 2, 4).reshape(B, T, H, hd)
    lse = lse.reshape(N, H, Lp)[:, :, :L].reshape(B, dil, H, L).transpose(0, 3, 1, 2).reshape(B, T, H)
    return o, lse


def dilated_attention(qkv):
    B, T = qkv.shape[:2]
    slopes = alibi_slopes(A_GROUPS * A_HEADS).reshape(A_GROUPS, A_HEADS)
    outs, lses = [], []
    for g in range(A_GROUPS):
        o, l = dilated_window_attn(qkv[:, :, 0, g], qkv[:, :, 1, g], qkv[:, :, 2, g], slopes[g],
                                   A_DILATIONS[g], A_WINDOWS[g] // A_DILATIONS[g])
        outs.append(o)
        lses.append(l)
    w = jax.nn.softmax(jnp.stack(lses), axis=0)
    o = jnp.einsum('gbth,gbthd->bthd', w, jnp.stack(outs))
    return o.reshape(B, T, A_HEADS * A_HD)


def nsa_attention(q, kv, gates, w_ck1, w_ck2, pe_k, w_cv1, w_cv2, pe_v):
    B, T = q.shape[:2]
    scale = B_HD ** -0.5
    slopes = alibi_slopes(B_HEADS).reshape(B_KV, B_REP)[:, :, None, None]
    qg = q.reshape(B, T, B_KV, B_REP, B_HD).transpose(0, 2, 3, 1, 4)
    gg = gates.reshape(B, T, B_KV, B_REP, 3).transpose(0, 2, 3, 1, 4)
    kvg = kv.transpose(2, 3, 0, 4, 1, 5)
    k_c, v_c, k_s, v_s, k_w, v_w = kvg[0, 0], kvg[0, 1], kvg[1, 0], kvg[1, 1], kvg[2, 0], kvg[2, 1]

    n_cmp = (T - CMP_LEN) // CMP_STRIDE + 1
    c_idx = jnp.arange(n_cmp)[:, None] * CMP_STRIDE + jnp.arange(CMP_LEN)[None, :]

    def compress(a, pe, w1, w2):
        blocks = (a[:, :, c_idx] + pe).reshape(B, B_KV, n_cmp, CMP_LEN * B_HD)
        return jax.nn.gelu(blocks @ w1) @ w2

    kc = compress(k_c, pe_k, w_ck1, w_ck2)
    vc = compress(v_c, pe_v, w_cv1, w_cv2)
    c_end = c_idx[:, -1]
    n_slc = T // SLC_LEN
    c_start = jnp.arange(n_cmp)[:, None] * CMP_STRIDE
    s_start = jnp.arange(n_slc)[None, :] * SLC_LEN
    overlap = jnp.clip(jnp.minimum(c_start + CMP_LEN, s_start + SLC_LEN) - jnp.maximum(c_start, s_start),
                       0, None).astype(jnp.float32) / CMP_LEN
    ks_blk = k_s.reshape(B, B_KV, n_slc, SLC_LEN, B_HD)
    vs_blk = v_s.reshape(B, B_KV, n_slc, SLC_LEN, B_HD)
    nsel = min(N_SEL, n_slc)
    bi = jnp.arange(B)[:, None, None, None]
    gi = jnp.arange(B_KV)[None, :, None, None]
    kw_pad = jnp.pad(k_w, ((0, 0), (0, 0), (WIN, 0), (0, 0)))
    vw_pad = jnp.pad(v_w, ((0, 0), (0, 0), (WIN, 0), (0, 0)))
    j_slc = jnp.arange(n_slc)
    w_off = jnp.arange(BLK + WIN) - WIN

    def block(b):
        t0 = b * BLK
        t = t0 + jnp.arange(BLK)
        qb = lax.dynamic_slice_in_dim(qg, t0, BLK, axis=3)
        gb = lax.dynamic_slice_in_dim(gg, t0, BLK, axis=3)
        dist = t[:, None] - c_end[None, :]
        s = jnp.einsum('bgrqd,bgcd->bgrqc', qb, kc).astype(jnp.float32) * scale - slopes * dist.astype(jnp.float32)
        p_c = masked_probs(s, dist >= 0)
        o_c = jnp.einsum('bgrqc,bgcd->bgrqd', p_c, vc)
        imp = jnp.einsum('bgrqc,cj->bgqj', p_c, overlap)
        cur = (t // SLC_LEN)[:, None]
        forced = (j_slc == 0) | (j_slc == cur) | (j_slc == cur - 1)
        score = jnp.where(forced, FORCE, jnp.where(j_slc <= cur, imp, NEG))
        top_s, top_i = lax.top_k(score, nsel)
        ks = ks_blk[bi, gi, top_i].reshape(B, B_KV, BLK, nsel * SLC_LEN, B_HD)
        vs = vs_blk[bi, gi, top_i].reshape(B, B_KV, BLK, nsel * SLC_LEN, B_HD)
        pos = (top_i[..., None] * SLC_LEN + jnp.arange(SLC_LEN)).reshape(B, B_KV, BLK, nsel * SLC_LEN)
        d_s = t[:, None] - pos
        ok_s = (d_s >= 0) & jnp.repeat(top_s > NEG / 2, SLC_LEN, axis=-1)
        s = jnp.einsum('bgrqd,bgqkd->bgrqk', qb, ks).astype(jnp.float32) * scale - slopes * d_s[:, :, None].astype(jnp.float32)
        p_s = masked_probs(s, ok_s[:, :, None])
        o_s = jnp.einsum('bgrqk,bgqkd->bgrqd', p_s, vs)
        kw = lax.dynamic_slice_in_dim(kw_pad, t0, BLK + WIN, axis=2)
        vw = lax.dynamic_slice_in_dim(vw_pad, t0, BLK + WIN, axis=2)
        pos_w = t0 + w_off
        d_w = t[:, None] - pos_w[None, :]
        ok_w = (d_w >= 0) & (d_w < WIN) & (pos_w >= 0)[None, :]
        s = jnp.einsum('bgrqd,bgkd->bgrqk', qb, kw).astype(jnp.float32) * scale - slopes * d_w.astype(jnp.float32)
        p_w = masked_probs(s, ok_w)
        o_w = jnp.einsum('bgrqk,bgkd->bgrqd', p_w, vw)
        return gb[..., 0:1] * o_c + gb[..., 1:2] * o_s + gb[..., 2:3] * o_w

    out = lax.map(block, jnp.arange(T // BLK))
    return out.transpose(1, 0, 4, 2, 3, 5).reshape(B, T, B_HEADS * B_HD)


def memory_cross_attention(hn, mn, w_q, w_kv, w_o):
    B, T, _ = hn.shape
    q = (hn @ w_q).reshape(B, T, X_HEADS, X_HD)
    kv = (mn @ w_kv).reshape(B, mn.shape[1], 2, X_HEADS, X_HD)
    s = jnp.einsum('bthd,bmhd->bhtm', q, kv[:, :, 0]).astype(jnp.float32) * (X_HD ** -0.5)
    p = jax.nn.softmax(s, axis=-1)
    o = jnp.einsum('bhtm,bmhd->bthd', p, kv[:, :, 1]).reshape(B, T, X_HEADS * X_HD)
    return o @ w_o


def peer(xn, w_pq, sub_k1, sub_k2, u_tab, v_tab):
    B, T, D = xn.shape
    half = PEER_DK // 2
    q = (xn @ w_pq).reshape(B, T, PEER_HEADS, PEER_DK)
    s1 = jnp.einsum('bthd,kd->bthk', q[..., :half], sub_k1).astype(jnp.float32)
    s2 = jnp.einsum('bthd,kd->bthk', q[..., half:], sub_k2).astype(jnp.float32)
    v1, i1 = lax.top_k(s1, PEER_TOPK)
    v2, i2 = lax.top_k(s2, PEER_TOPK)
    cand = (v1[..., :, None] + v2[..., None, :]).reshape(B, T, PEER_HEADS, PEER_TOPK * PEER_TOPK)
    vs, ci = lax.top_k(cand, PEER_TOPK)
    e1 = jnp.take_along_axis(i1, ci // PEER_TOPK, axis=-1)
    e2 = jnp.take_along_axis(i2, ci % PEER_TOPK, axis=-1)
    eid = e1 * N_KEYS + e2
    g = jax.nn.softmax(vs, axis=-1)
    n_chunks = (B * T) // BLK
    xf = xn.reshape(n_chunks, BLK, D)
    ef = eid.reshape(n_chunks, BLK, PEER_HEADS * PEER_TOPK)
    gf = g.reshape(n_chunks, BLK, PEER_HEADS * PEER_TOPK)

    def chunk(args):
        xb, eb, gb = args
        a = jax.nn.gelu(jnp.einsum('nd,ned->ne', xb, u_tab[eb]))
        return jnp.einsum('ne,ned->nd', a * gb, v_tab[eb])

    return lax.map(chunk, (xf, ef, gf)).reshape(B, T, D)


def setup_inputs(seed: int = 0) -> dict:
    key = jax.random.key(seed)
    ks = jax.random.split(key, 25)
    L, D = DEPTH, D_MODEL

    def nrm(k, shape, scale):
        return jax.random.normal(k, shape, jnp.float32) * scale

    def gain(k, shape):
        return 1.0 + 0.01 * jax.random.normal(k, shape, jnp.float32)

    return {
        'x': nrm(ks[0], (BATCH, SEQ, D), 1.0),
        'mem': nrm(ks[1], (BATCH, N_MEM, D), 1.0),
        'norm_mix': gain(ks[2], (L, D)),
        'w_in': nrm(ks[3], (L, D, IN_COLS), D ** -0.5),
        'w_cmp_k1': nrm(ks[4], (L, CMP_LEN * B_HD, CMP_HIDDEN), (CMP_LEN * B_HD) ** -0.5),
        'w_cmp_k2': nrm(ks[5], (L, CMP_HIDDEN, B_HD), CMP_HIDDEN ** -0.5),
        'pe_cmp_k': nrm(ks[6], (L, CMP_LEN, B_HD), 0.1),
        'w_cmp_v1': nrm(ks[7], (L, CMP_LEN * B_HD, CMP_HIDDEN), (CMP_LEN * B_HD) ** -0.5),
        'w_cmp_v2': nrm(ks[8], (L, CMP_HIDDEN, B_HD), CMP_HIDDEN ** -0.5),
        'pe_cmp_v': nrm(ks[9], (L, CMP_LEN, B_HD), 0.1),
        'w_up_a': nrm(ks[10], (L, A_HEADS * A_HD, D), (A_HEADS * A_HD) ** -0.5),
        'w_up_b': nrm(ks[11], (L, B_HEADS * B_HD, D), (B_HEADS * B_HD) ** -0.5),
        'w_out': nrm(ks[12], (L, D, D), D ** -0.5),
        'norm_x': gain(ks[13], (L, D)),
        'norm_mem': gain(ks[14], (L, D)),
        'w_xq': nrm(ks[15], (L, D, X_HEADS * X_HD), D ** -0.5),
        'w_xkv': nrm(ks[16], (L, D, 2 * X_HEADS * X_HD), D ** -0.5),
        'w_xo': nrm(ks[17], (L, X_HEADS * X_HD, D), (X_HEADS * X_HD) ** -0.5),
        'norm_ffn': gain(ks[18], (L, D)),
        'w_pq': nrm(ks[19], (L, D, PEER_HEADS * PEER_DK), D ** -0.5),
        'sub_keys1': nrm(ks[20], (L, N_KEYS, PEER_DK // 2), (PEER_DK // 2) ** -0.5),
        'sub_keys2': nrm(ks[21], (L, N_KEYS, PEER_DK // 2), (PEER_DK // 2) ** -0.5),
        'expert_u': nrm(ks[22], (L, N_EXPERTS, D), D ** -0.5),
        'expert_v': nrm(ks[23], (L, N_EXPERTS, D), PEER_V_SCALE),
        'norm_final': gain(ks[24], (D,)),
    }


def reference(x, mem, norm_mix, w_in, w_cmp_k1, w_cmp_k2, pe_cmp_k, w_cmp_v1, w_cmp_v2, pe_cmp_v,
              w_up_a, w_up_b, w_out, norm_x, norm_mem, w_xq, w_xkv, w_xo, norm_ffn, w_pq,
              sub_keys1, sub_keys2, expert_u, expert_v, norm_final):
    B, T, D = x.shape
    h = x
    for l in range(DEPTH):
        n = rmsnorm(h, norm_mix[l])
        proj = n @ w_in[l]
        a_qkv, b_q, b_kv, b_gate, m_gate = jnp.split(proj, SPLITS, axis=-1)
        y_a = dilated_attention(a_qkv.reshape(B, T, 3, A_GROUPS, A_HEADS, A_HD))
        y_b = nsa_attention(b_q.reshape(B, T, B_HEADS, B_HD),
                            b_kv.reshape(B, T, 3, 2, B_KV, B_HD),
                            jax.nn.sigmoid(b_gate.reshape(B, T, B_HEADS, 3)),
                            w_cmp_k1[l], w_cmp_k2[l], pe_cmp_k[l], w_cmp_v1[l], w_cmp_v2[l], pe_cmp_v[l])
        gates = jax.nn.sigmoid(m_gate.reshape(B, T, N_BRANCH, D))
        mix = gates[:, :, 0] * (y_a @ w_up_a[l]) + gates[:, :, 1] * (y_b @ w_up_b[l])
        h = h + mix @ w_out[l]
        h = h + memory_cross_attention(rmsnorm(h, norm_x[l]), rmsnorm(mem, norm_mem[l]), w_xq[l], w_xkv[l], w_xo[l])
        h = h + peer(rmsnorm(h, norm_ffn[l]), w_pq[l], sub_keys1[l], sub_keys2[l], expert_u[l], expert_v[l])
    return rmsnorm(h, norm_final).astype(x.dtype)
```

```python
import contextlib
from concourse.bass_utils import run_bass_kernel_spmd
import numpy as np
import concourse.bass as bass
import concourse.mybir as mybir

F32 = mybir.dt.float32
BF16 = mybir.dt.bfloat16
I32 = mybir.dt.int32
U32 = mybir.dt.uint32
ALU = mybir.AluOpType
AF = mybir.ActivationFunctionType
AX = mybir.AxisListType


class T:
    def __init__(self, name, ap):
        self.name = name
        self.ap = ap
        self.w = None
        self.r = []

    def __getitem__(self, k):
        return V(self, self.ap[k])


class V:
    def __init__(self, t, ap):
        self.t = t
        self.ap = ap

    def __getitem__(self, k):
        return V(self.t, self.ap[k])


class Ctx:
    ENG = ("pe", "act", "dve", "pool", "sp")

    def __init__(self, nc, stack):
        self.nc = nc
        self.stack = stack
        self.eng = {"pe": nc.tensor, "act": nc.scalar, "dve": nc.vector,
                    "pool": nc.gpsimd, "sp": nc.sync}
        self.CE = ("pe", "act", "dve", "pool")
        self.sem = {e: stack.enter_context(nc.semaphore("s_" + e)) for e in self.CE}
        self.cnt = {e: 0 for e in self.CE}
        self.NS = 16
        self.dsem = {q: [stack.enter_context(nc.semaphore(f"d_{q}{i}")) for i in range(self.NS)]
                     for q in ("sp", "pool")}
        self.dcnt = {q: 0 for q in ("sp", "pool")}
        self.waited = {e: {} for e in self.ENG}
        self.n_inst = 0
        self.uid = 0

    def sb(self, shape, dt, name=None):
        self.uid += 1
        name = name or f"sb{self.uid}"
        h = self.stack.enter_context(self.nc.sbuf_tensor(name, list(shape), dt))
        return T(name, h)

    def ps(self, shape, dt=F32, name=None):
        self.uid += 1
        name = name or f"ps{self.uid}"
        h = self.stack.enter_context(self.nc.psum_tensor(name, list(shape), dt))
        return T(name, h)

    def dram(self, name, shape, dt, kind="Internal"):
        h = self.nc.dram_tensor(name, list(shape), dt, kind=kind)
        return T(name, h.ap())

    def _semof(self, key):
        if isinstance(key, tuple):
            return self.dsem[key[0]][key[1]]
        return self.sem[key]

    def _need(self, e, dep):
        if dep is None:
            return
        key, val = dep
        if self.waited[e].get(key, 0) >= val:
            return
        self.eng[e].wait_ge(self._semof(key), val)
        self.waited[e][key] = val

    def op(self, e, fn, reads=(), writes=(), pe_chain=False, dma=False):
        rt = [v.t if isinstance(v, V) else v for v in reads]
        wt = [v.t if isinstance(v, V) else v for v in writes]
        for t in rt:
            self._need(e, t.w)
        for t in wt:
            if not (pe_chain and t.w is not None and t.w[0] == e):
                self._need(e, t.w)
            for d in t.r:
                self._need(e, d)
        if dma:
            n = self.dcnt[e]
            slot = n % self.NS
            val = 16 * (n // self.NS + 1)
            key = (e, slot)
            if n >= self.NS:
                self._need(e, (key, val - 16))
            ins = fn()
            ins.then_inc(self.dsem[e][slot], 16)
            self.dcnt[e] += 1
            tok = (key, val)
        else:
            ins = fn()
            self.cnt[e] += 1
            ins.then_inc(self.sem[e], 1)
            tok = (e, self.cnt[e])
        for t in rt:
            t.r.append(tok)
            if len(t.r) > 48:
                last = {}
                for (x, s_) in t.r:
                    last[x] = max(last.get(x, 0), s_)
                t.r = list(last.items())
        for t in wt:
            t.w = tok
            t.r = []
        self.n_inst += 1
        return ins

    def barrier(self):
        for e in self.ENG:
            for x in self.CE:
                if x != e and self.cnt[x] > 0:
                    self._need(e, (x, self.cnt[x]))
            for q in ("sp", "pool"):
                n = self.dcnt[q]
                for slot in range(min(n, self.NS)):
                    k = (n - 1 - slot) // self.NS + 1 if n - 1 >= slot else 0
                    cnt_slot = (n - slot + self.NS - 1) // self.NS
                    if cnt_slot > 0:
                        self._need(e, ((q, slot), 16 * cnt_slot))

    def dma(self, out, in_, eng="sp", **kw):
        return self.op(eng, lambda: self.eng[eng].dma_start(out=out.ap, in_=in_.ap, **kw),
                       reads=[in_], writes=[out], dma=True)

    def mm(self, out, lhsT, rhs, start=True, stop=True):
        return self.op("pe", lambda: self.nc.tensor.matmul(out.ap, lhsT.ap, rhs.ap, start=start, stop=stop),
                       reads=[lhsT, rhs], writes=[out], pe_chain=True)

    def transpose(self, out, in_, ident):
        return self.op("pe", lambda: self.nc.tensor.transpose(out.ap, in_.ap, ident.ap),
                       reads=[in_, ident], writes=[out], pe_chain=True)

    def act(self, out, in_, func, bias=None, scale=None, accum=None, extra_reads=()):
        kw = {}
        rd = [in_] + list(extra_reads)
        wr = [out]
        if bias is not None:
            if isinstance(bias, V):
                kw["bias"] = bias.ap; rd.append(bias)
            else:
                kw["bias"] = bias
        if scale is not None:
            if isinstance(scale, V):
                kw["scale"] = scale.ap; rd.append(scale)
            else:
                kw["scale"] = scale
        if accum is not None:
            kw["accum_out"] = accum.ap; wr.append(accum)
        return self.op("act", lambda: self.nc.scalar.activation(out.ap, in_.ap, func, **kw),
                       reads=rd, writes=wr)

    def _ve(self, e):
        return self.nc.vector if e == "dve" else self.nc.gpsimd

    def tt(self, out, in0, in1, op, e="dve"):
        return self.op(e, lambda: self._ve(e).tensor_tensor(out.ap, in0.ap, in1.ap, op),
                       reads=[in0, in1], writes=[out])

    def ts(self, out, in0, s1, s2, op0, op1=None, e="dve", accum=None):
        rd = [in0]; wr = [out]
        a1 = s1.ap if isinstance(s1, V) else s1
        a2 = s2.ap if isinstance(s2, V) else s2
        if isinstance(s1, V): rd.append(s1)
        if isinstance(s2, V): rd.append(s2)
        kw = {}
        if op1 is not None: kw["op1"] = op1
        if accum is not None:
            kw["accum_out"] = accum.ap; wr.append(accum)
        return self.op(e, lambda: self._ve(e).tensor_scalar(out.ap, in0.ap, a1, a2, op0, **kw),
                       reads=rd, writes=wr)

    def stt(self, out, in0, s, in1, op0, op1, e="dve", accum=None):
        rd = [in0, in1]; wr = [out]
        a = s.ap if isinstance(s, V) else s
        if isinstance(s, V): rd.append(s)
        kw = {}
        if accum is not None:
            kw["accum_out"] = accum.ap; wr.append(accum)
        return self.op(e, lambda: self._ve(e).scalar_tensor_tensor(out.ap, in0.ap, a, in1.ap, op0, op1, **kw),
                       reads=rd, writes=wr)

    def copy(self, out, in_, e="dve"):
        if e == "act":
            return self.op("act", lambda: self.nc.scalar.copy(out.ap, in_.ap), reads=[in_], writes=[out])
        return self.op(e, lambda: self._ve(e).tensor_copy(out.ap, in_.ap), reads=[in_], writes=[out])

    def recip(self, out, in_):
        return self.op("dve", lambda: self.nc.vector.reciprocal(out.ap, in_.ap), reads=[in_], writes=[out])

    def memset(self, out, val, e="dve"):
        return self.op(e, lambda: self._ve(e).memset(out.ap, val), reads=[], writes=[out])

    def finish(self, out_tensors):
        self.barrier()

D = 2048
NW = 8192
NOWN = 1024
KC = 16
C_AQ, C_AK, C_AV = 0, 1536, 3072
C_BQ = 4608
C_BKV = 5632
C_BG = 6400
C_MG = 6448
IN_COLS = 10544
EPS = 1e-6


class Rot:
    def __init__(self, tiles):
        self.tiles = tiles
        self.i = 0

    def next(self):
        t = self.tiles[self.i % len(self.tiles)]
        self.i += 1
        return t


def phase1(c, io, S):
    nc = c.nc
    top = c.stack
    with contextlib.ExitStack() as ph:
        c.stack = ph
        ones = c.sb([128, 128], BF16, "p1_ones")
        c.memset(ones[:], 1.0)
        gmix = c.sb([128, KC], F32, "p1_g")
        c.dma(gmix[:], io["g_mix"][:])
        with contextlib.ExitStack() as pa:
            c.stack = pa
            XT = [c.sb([128, KC, 512], F32, f"p1_xt{i}") for i in range(2)]
            XN = [c.sb([128, KC, 512], BF16, f"p1_xn{i}") for i in range(2)]
            SQ = Rot([c.sb([128, 512], BF16, f"p1_sq{i}") for i in range(3)])
            SSP = Rot([c.ps([128, 512], F32, f"p1_ss{i}") for i in range(2)])
            RB = Rot([c.sb([128, 512], F32, f"p1_rb{i}") for i in range(2)])
            for ch in range(16):
                xt = XT[ch % 2]
                xn = XN[ch % 2]
                c.dma(xt[:], V(io["xT"], io["xT"].ap[ch]))
                ss = SSP.next()
                for k in range(KC):
                    sq = SQ.next()
                    c.act(sq[:], xt[:, k, :], AF.Square)
                    c.mm(ss[:], ones[:], sq[:], start=(k == 0), stop=(k == KC - 1))
                rb = RB.next()
                c.ts(rb[:], ss[:], 1.0 / D, EPS, ALU.mult, ALU.add)
                c.act(rb[:], rb[:], AF.Sqrt)
                c.recip(rb[:], rb[:])
                for k in range(KC):
                    c.stt(xn[:, k, :], xt[:, k, :], gmix[:, k:k + 1], rb[:], ALU.mult, ALU.mult)
                c.dma(V(S["XN"], S["XN"].ap[ch]), xn[:])
        c.barrier()
        with contextlib.ExitStack() as pb:
            c.stack = pb
            WFs = [c.sb([128, KC, 512], F32, f"p1_wf{i}") for i in range(2)]
            XOWN = {}
            WB = [c.sb([128, KC, 512], BF16, f"p1_wb{i}") for i in range(2)]
            XNL = Rot([c.sb([128, KC, 512], BF16, f"p1_xl{i}") for i in range(2)])
            PS = Rot([c.ps([128, 512], F32, f"p1_ps{i}") for i in range(4)])
            STG = Rot([c.sb([128, 512], BF16, f"p1_st{i}") for i in range(4)])
            STGF = Rot([c.sb([128, 64], F32, f"p1_sf{i}") for i in range(2)])
            wv = io["w_in"].ap.rearrange("(k p) n -> p k n", p=128)
            slab_i = [0]

            def load_slab(col0, ncols):
                wb = WB[slab_i[0] % 2]
                WF = WFs[slab_i[0] % 2]
                slab_i[0] += 1
                c.dma(WF[:, :, 0:ncols], V(io["w_in"], wv[:, :, col0:col0 + ncols]))
                for k in range(KC):
                    e = ("act", "dve")[k % 2]
                    c.copy(wb[:, k, 0:ncols], WF[:, k, 0:ncols], e=e)
                return wb

            def load_xn(ch):
                if ch in XOWN:
                    return XOWN[ch]
                t = XNL.next()
                c.dma(t[:], V(S["XN"], S["XN"].ap[ch]))
                return t

            def fm(wb, xl, c0, M, evac):
                ps = PS.next()
                for k in range(KC):
                    c.mm(ps[0:M, :], wb[:, k, c0:c0 + M], xl[:, k, :], start=(k == 0), stop=(k == KC - 1))
                evac(ps)

            def tm(wb, xl, tt, c0, ncols, evac):
                ps = PS.next()
                for k in range(KC):
                    c.mm(ps[:, 0:ncols], xl[:, k, tt * 128:(tt + 1) * 128], wb[:, k, c0:c0 + ncols],
                         start=(k == 0), stop=(k == KC - 1))
                evac(ps)

            def ev_fm(dst_t, dst_ap, M, func=None, scale=None):
                def f(ps):
                    st = STG.next()
                    if func is None and scale is None:
                        c.copy(st[0:M, :], ps[0:M, :], e="dve")
                    else:
                        c.act(st[0:M, :], ps[0:M, :], func or AF.Copy, scale=scale)
                    c.dma(V(dst_t, dst_ap), st[0:M, :])
                return f

            def ev_tm(dst_t, dst_ap, ncols):
                def f(ps):
                    st = STG.next()
                    c.copy(st[:, 0:ncols], ps[:, 0:ncols], e="dve")
                    c.dma(V(dst_t, dst_ap), st[:, 0:ncols])
                return f

            for ch_ in (14, 15):
                t_ = c.sb([128, KC, 512], BF16, f"p1_xown{ch_}")
                c.dma(t_[:], V(S["XN"], S["XN"].ap[ch_]))
                XOWN[ch_] = t_
            for g in range(3):
                wb = load_slab(C_AQ + g * 512, 512)
                for ch in (14, 15):
                    xl = load_xn(ch)
                    o0 = (ch - 14) * 512
                    for h in range(4):
                        fm(wb, xl, h * 128, 128, ev_fm(S["AQT"], S["AQT"].ap[g, h, :, o0:o0 + 512], 128))
                chs = (13, 14, 15) if g < 2 else (10, 11, 12, 13, 14, 15)
                wb = load_slab(C_AK + g * 512, 512)
                for ch in chs:
                    xl = load_xn(ch)
                    o0 = (ch - 10) * 512
                    for h in range(4):
                        fm(wb, xl, h * 128, 128, ev_fm(S["AKT"], S["AKT"].ap[g, h, :, o0:o0 + 512], 128))
                wb = load_slab(C_AV + g * 512, 512)
                for ch in chs:
                    xl = load_xn(ch)
                    o0 = (ch - 10) * 512
                    for tt in range(4):
                        tm(wb, xl, tt, 0, 512, ev_tm(S["AV"], S["AV"].ap[g, o0 + tt * 128:o0 + (tt + 1) * 128, :], 512))
            for g in range(2):
                wb = load_slab(C_BQ + g * 512, 512)
                for ch in (14, 15):
                    xl = load_xn(ch)
                    for h in range(8):
                        dst = S["BQT"].ap[g, :, (ch - 14) * 4:(ch - 14) * 4 + 4, h, :]
                        def f(ps, dst=dst):
                            st = STG.next()
                            c.act(st[0:64, :], ps[0:64, :], AF.Copy, scale=0.125)
                            c.dma(V(S["BQT"], dst), V(st, st.ap[0:64, :].rearrange("p (i q) -> p i q", q=128)))
                        fm(wb, xl, h * 64, 64, f)
            wb = load_slab(C_BKV, 512)
            for ch in range(16):
                xl = load_xn(ch)
                o0 = ch * 512
                fm(wb, xl, 0, 128, ev_fm(S["KCT"], S["KCT"].ap[:, o0:o0 + 512], 128))
                fm(wb, xl, 128, 128, ev_fm(S["VCT"], S["VCT"].ap[:, o0:o0 + 512], 128))
                fm(wb, xl, 256, 128, ev_fm(S["KST"], S["KST"].ap.rearrange("g d w -> (g d) w")[:, o0:o0 + 512], 128))
                for tt in range(4):
                    tm(wb, xl, tt, 384, 128, ev_tm(S["VS"], S["VS"].ap[o0 + tt * 128:o0 + (tt + 1) * 128, :], 128))
            wb = load_slab(C_BKV + 512, 256 + 48)
            for ch in (13, 14, 15):
                xl = load_xn(ch)
                o0 = (ch - 13) * 512
                fm(wb, xl, 0, 128, ev_fm(S["KWT"], S["KWT"].ap.rearrange("g d w -> (g d) w")[:, o0:o0 + 512], 128))
                for tt in range(4):
                    tm(wb, xl, tt, 128, 128, ev_tm(S["VW"], S["VW"].ap[o0 + tt * 128:o0 + (tt + 1) * 128, :], 128))
                if ch >= 14:
                    for tt in range(4):
                        r0 = (ch - 14) * 512 + tt * 128
                        def f(ps, r0=r0):
                            sf = STGF.next()
                            c.act(sf[:, 0:48], ps[:, 0:48], AF.Sigmoid)
                            c.dma(S["BG"][r0:r0 + 128, :], sf[:, 0:48])
                        tm(wb, xl, tt, 256, 48, f)
            for sl in range(8):
                wb = load_slab(C_MG + sl * 512, 512)
                for ch in (14, 15):
                    xl = load_xn(ch)
                    o0 = (ch - 14) * 512
                    for j in range(4):
                        fm(wb, xl, j * 128, 128,
                           ev_fm(S["MG"], S["MG"].ap[sl * 4 + j, :, o0:o0 + 512], 128, func=AF.Sigmoid))
        c.barrier()
    c.stack = top

A_DIL = (1, 4, 16)
A_J = [128 * (d + 1) + 768 for d in A_DIL]
A_JOFF = []
_o = 0
for _g in range(3):
    for _h in range(4):
        A_JOFF.append(_o)
        _o += A_J[_g]
AB_COLS = _o
NEGM = -30000.0


def host_a_tables(core):
    slopes = (2.0 ** (-8.0 * (np.arange(12, dtype=np.float64) + 1.0) / 12)).reshape(3, 4)
    tab = np.empty((128, AB_COLS), np.float32)
    p = np.arange(128)[:, None]
    for g in range(3):
        d = A_DIL[g]
        jx = np.arange(A_J[g])[None, :]
        delta = (jx - 384) - p
        valid = (delta >= 0) & (delta <= 128 * d) & (delta % d == 0)
        for h in range(4):
            o = A_JOFF[g * 4 + h]
            tab[:, o:o + A_J[g]] = np.where(valid, -slopes[g, h] * delta, NEGM)
    wrel = np.arange(24)[None, :] * 128 + p
    kval = np.where(wrel + 1024 * core - 2048 >= 0, 0.0, NEGM).astype(np.float32)
    return tab, kval


def phase2(c, io, S):
    top = c.stack
    scale = float(128 ** -0.5)
    with contextlib.ExitStack() as ph:
        c.stack = ph
        ones = c.sb([128, 128], BF16, "p2_ones")
        c.memset(ones[:], 1.0)
        KV = c.sb([128, 24], F32, "p2_kv")
        c.dma(KV[:], io["a_kvalid"][:])
        KT = Rot([c.sb([128, 20 * 128], BF16, f"p2_kt{i}") for i in range(2)])
        VT = Rot([c.sb([128, 20, 128], BF16, f"p2_vt{i}") for i in range(2)])
        BT = Rot([c.sb([128, A_J[2]], F32, f"p2_bt{i}") for i in range(2)])
        QT = Rot([c.sb([128, 512], BF16, f"p2_qt{i}") for i in range(2)])
        PSS = Rot([c.ps([128, 512], F32, f"p2_ps{i}") for i in range(3)])
        PSN = Rot([c.ps([128, 512], F32, f"p2_pn{i}") for i in range(2)])
        PSD = Rot([c.ps([128, 512], F32, f"p2_pd{i}") for i in range(2)])
        SBF = Rot([c.sb([128, 512], F32, f"p2_sb{i}") for i in range(3)])
        PT = Rot([c.sb([128, 512], BF16, f"p2_pt{i}") for i in range(3)])
        RD = Rot([c.sb([128, 512], F32, f"p2_rd{i}") for i in range(2)])
        YT = Rot([c.sb([128, 512], BF16, f"p2_yt{i}") for i in range(2)])
        for quad in range(2):
            i0 = quad * 4
            for h in range(4):
                Np = PSN.next()
                Dp = PSD.next()
                st_ = [True]
                pend = []
                for g in range(3):
                    d = A_DIL[g]
                    kb_lo = 16 + i0 - d
                    kb_hi = 16 + i0 + 3
                    nb = kb_hi - kb_lo + 1
                    kt = KT.next()
                    c.dma(kt[:, 0:nb * 128], S["AKT"][g, h, :, kb_lo * 128:(kb_hi + 1) * 128])
                    vt = VT.next()
                    c.dma(vt[:, 0:nb, :],
                          V(S["AV"], S["AV"].ap[g, kb_lo * 128:(kb_hi + 1) * 128, h * 128:(h + 1) * 128]
                            .rearrange("(b p) d -> p b d", p=128)))
                    bt = BT.next()
                    o = A_JOFF[g * 4 + h]
                    c.dma(bt[:, 0:A_J[g]], io["a_bias"][:, o:o + A_J[g]])
                    qt = QT.next()
                    c.dma(qt[:], S["AQT"][g, h, :, i0 * 128:(i0 + 4) * 128])
                    def a_s(kb, kt=kt, qt=qt, kb_lo=kb_lo):
                        sp = PSS.next()
                        c.mm(sp[:], kt[:, (kb - kb_lo) * 128:(kb - kb_lo + 1) * 128], qt[:])
                        return (sp,)

                    def a_rest(kb, sp, g=g, bt=bt, vt=vt, kb_lo=kb_lo, kb_hi=kb_hi):
                        sb = SBF.next()
                        jx0 = 128 * (16 + i0 - kb) + 384
                        c.stt(sb[:], sp[:], scale, bt[:, jx0:jx0 + 512], ALU.mult, ALU.add)
                        pt = PT.next()
                        c.act(pt[:], sb[:], AF.Exp, bias=KV[:, kb:kb + 1])
                        last = (g == 2 and kb == kb_hi)
                        c.mm(Np[:], vt[:, kb - kb_lo, :], pt[:], start=st_[0], stop=last)
                        c.mm(Dp[:], ones[:], pt[:], start=st_[0], stop=last)
                        st_[0] = False

                    for kb in range(kb_lo, kb_hi + 1):
                        pend.append((a_rest, (kb,) + a_s(kb)))
                        if len(pend) > 1:
                            f_, a_ = pend.pop(0)
                            f_(*a_)
                while pend:
                    f_, a_ = pend.pop(0)
                    f_(*a_)
                rd = RD.next()
                c.recip(rd[:], Dp[:])
                yt = YT.next()
                c.tt(yt[:], Np[:], rd[:], ALU.mult)
                c.dma(S["YAT"][h, :, i0 * 128:(i0 + 4) * 128], yt[:])
        c.barrier()
    c.stack = top

import ml_dtypes
NPBF = ml_dtypes.bfloat16


def _split3(v):
    v = np.asarray(v, np.float64)
    a = v.astype(NPBF).astype(np.float64)
    b = (v - a).astype(NPBF).astype(np.float64)
    cc = (v - a - b).astype(NPBF).astype(np.float64)
    return a, b, cc


def host_b_static():
    T_ = {}
    slopes = 2.0 ** (-8.0 * (np.arange(16, dtype=np.float64) + 1.0) / 16)
    w = np.arange(8192)
    ka = np.zeros((8, 8192), np.float64)
    ka[0:3] = (w % 128)[None]
    ka[3:6] = (w - w % 128)[None]
    ka[6:8] = 1.0
    T_["kaug_sel"] = ka.astype(NPBF)
    cidx = np.arange(512)
    tau = 16 * cidx + 31
    kc = np.zeros((8, 512), np.float64)
    kc[0:3] = (tau % 128)[None]
    kc[3:6] = (tau - tau % 128)[None]
    kc[6:8] = 1.0
    T_["kaug_cmp"] = kc.astype(NPBF)
    qa = np.zeros((2, 8, 8, 8, 128), np.float64)
    for g in range(2):
        for h in range(8):
            s = slopes[g * 8 + h]
            s1, s2, s3 = _split3(s)
            for i in range(8):
                tq = 7168 + 128 * i + np.arange(128)
                cq = -s * tq
                c1 = cq.astype(NPBF).astype(np.float64)
                c2 = (cq - c1).astype(NPBF).astype(np.float64)
                qa[g, i, 0, h] = s1; qa[g, i, 1, h] = s2; qa[g, i, 2, h] = s3
                qa[g, i, 3, h] = s1; qa[g, i, 4, h] = s2; qa[g, i, 5, h] = s3
                qa[g, i, 6, h] = c1; qa[g, i, 7, h] = c2
    T_["qaug"] = qa.reshape(2, 8, 8, 1024).astype(NPBF)
    jj = np.arange(128)[:, None]
    T_["EE"] = (jj == (w // 64)[None, :]).astype(NPBF)
    kl = np.arange(128)[:, None]
    ql = np.arange(128)[None, :]
    tri = np.where(kl > ql, NEGM, 0.0)
    tri2 = np.where(kl <= ql, NEGM, 0.0)
    T_["TRI"] = np.tile(tri, (1, 8)).astype(NPBF)
    T_["TRI2"] = np.tile(tri2, (1, 8)).astype(NPBF)
    cm = np.zeros((8, 128, 128))
    for i in range(8):
        cm[i] = np.where(16 * (384 + kl) + 31 <= 7168 + 128 * i + ql, 0.0, NEGM)
    T_["CM"] = np.tile(cm, (1, 1, 8)).astype(NPBF)
    cs = 16 * cidx[:, None]
    ss = 64 * np.arange(128)[None, :]
    ov = np.clip(np.minimum(cs + 32, ss + 64) - np.maximum(cs, ss), 0, None) / 32.0
    ovx = np.zeros((512, 129))
    ovx[:, :128] = ov
    ovx[:, 128] = 1.0
    T_["OV"] = ovx.reshape(4, 128, 129).astype(NPBF)
    T_["ident"] = np.eye(128, dtype=np.float32)
    return T_


def host_b_core(core):
    T_ = {}
    p = np.arange(128)[:, None]
    c_ = 128 * np.arange(4)[None, :] + p
    T_["cval"] = np.where((16 * c_ >= 7168 - 1024 * core) & (c_ < 511), 0.0, NEGM).astype(np.float32)
    ww = 6656 + 128 * np.arange(12)[None, :] + p
    T_["wval"] = np.where(ww >= 7168 - 1024 * core, 0.0, NEGM).astype(np.float32)
    j0 = 112 - 16 * core
    jj = np.arange(128)[None, :]
    mul = np.zeros((8, 128, 128), np.float32)
    add = np.zeros((8, 128, 128), np.float32)
    for i in range(8):
        cur = (7168 + 128 * i + np.arange(128)[:, None]) // 64
        forced = (jj == cur) | (jj == cur - 1) | (jj == j0)
        ok = (jj <= cur) & (jj >= j0)
        forced = forced & (jj >= j0)
        mul[i] = np.where(ok & ~forced, 1.0, 0.0)
        add[i] = np.where(forced, 1e4, np.where(ok, 0.0, -1e30))
    T_["smul"] = mul
    T_["sadd"] = add
    return T_


NSA_PRUNE = 24


def bcast_ap(ap, dims):
    return bass.AP(tensor=ap.tensor, offset=ap.offset, ap=[list(ap.ap[0])] + [list(d) for d in dims])


def gelu_tanh(c, out, xin, t1, t2):
    c.tt(t1, xin, xin, ALU.mult)
    c.ts(t1, t1, 0.044715, 1.0, ALU.mult, ALU.add)
    c.tt(t1, t1, xin, ALU.mult)
    c.act(t2, t1, AF.Sigmoid, scale=1.5957691216057308)
    c.tt(out, xin, t2, ALU.mult)


def phase3(c, io, S, after_loads=None):
    top = c.stack
    with contextlib.ExitStack() as ph:
        c.stack = ph
        ident = c.sb([128, 128], F32, "p3_id")
        c.dma(ident[:], io["ident"][:])
        KS = [c.sb([72, NW], BF16, f"p3_ks{g}") for g in range(2)]
        KW = [c.sb([72, 1536], BF16, f"p3_kw{g}") for g in range(2)]
        for g in range(2):
            c.dma(KS[g][0:64, :], S["KST"][g, :, :])
            c.dma(KS[g][64:72, :], io["kaug_sel"][:, :])
            c.dma(KW[g][0:64, :], S["KWT"][g, :, :])
            c.dma(KW[g][64:72, :], io["kaug_sel"][:, 6656:8192])
        VSA = c.sb([128, 64, 2, 65], BF16, "p3_vs")
        c.memset(VSA[:, :, :, 64:65], 1.0, e="pool")
        for g in range(2):
            c.dma(VSA[:, :, g, 0:64], V(S["VS"], S["VS"].ap[:, g * 64:(g + 1) * 64].rearrange("(m p) d -> p m d", p=128)))
        VWA = c.sb([128, 12, 2, 65], BF16, "p3_vw")
        c.memset(VWA[:, :, :, 64:65], 1.0, e="pool")
        for g in range(2):
            c.dma(VWA[:, :, g, 0:64], V(S["VW"], S["VW"].ap[:, g * 64:(g + 1) * 64].rearrange("(m p) d -> p m d", p=128)))
        EE = c.sb([128, NW], BF16, "p3_ee")
        c.dma(EE[:], io["EE"][:])
        TRI = c.sb([128, 1024], BF16, "p3_tri")
        c.dma(TRI[:], io["TRI"][:])
        TRI2 = c.sb([128, 1024], BF16, "p3_tri2")
        c.dma(TRI2[:], io["TRI2"][:])
        IDB = c.sb([128, 128], BF16, "p3_idb")
        c.copy(IDB[:], ident[:])
        OV = c.sb([128, 4, 129], BF16, "p3_ov")
        c.dma(OV[:], V(io["OV"], io["OV"].ap.rearrange("t p n -> p t n")))
        CVAL = c.sb([128, 4], F32, "p3_cval")
        c.dma(CVAL[:], io["cval"][:])
        WVAL = c.sb([128, 12], F32, "p3_wval")
        c.dma(WVAL[:], io["wval"][:])
        KCA = [c.sb([72, 512], BF16, f"p3_kca{g}") for g in range(2)]
        VCA = c.sb([128, 4, 2, 65], BF16, "p3_vca")
        c.memset(VCA[:, :, :, 64:65], 1.0, e="pool")
        PSS = Rot([c.ps([128, 1024], F32, f"p3_pss{i}") for i in range(2)])
        PSO = c.ps([128, 1024], F32, "p3_pso")
        PSM = Rot([c.ps([128, 512], F32, f"p3_psm{i}") for i in range(2)])

        with contextlib.ExitStack() as p1:
            c.stack = p1
            KC = c.sb([128, NW], BF16, "p3_kc")
            W1F = c.sb([128, 32, 128], F32, "p3_w1f")
            W1 = c.sb([128, 32, 128], BF16, "p3_w1")
            W2F = c.sb([128, 64], F32, "p3_w2f")
            W2 = c.sb([128, 64], BF16, "p3_w2")
            PEF = c.sb([128, 32], F32, "p3_pef")
            PEB = c.sb([128, 32], BF16, "p3_peb")
            HB = c.sb([128, 1], F32, "p3_hb")
            HX = c.sb([128, 512], F32, "p3_hx")
            H1 = c.sb([128, 512], F32, "p3_h1")
            H2 = c.sb([128, 512], F32, "p3_h2")
            HT = c.sb([128, 512], BF16, "p3_ht")
            for kv in range(2):
                src = S["KCT"] if kv == 0 else S["VCT"]
                c.dma(KC[:], src[:, :])
                w1 = io["w_cmp_k1"] if kv == 0 else io["w_cmp_v1"]
                w2 = io["w_cmp_k2"] if kv == 0 else io["w_cmp_v2"]
                pe = io["pe_k_T"] if kv == 0 else io["pe_v_T"]
                w1v = w1.ap.rearrange("(p d) h -> d p h", d=64)
                for half in range(2):
                    c.dma(W1F[half * 64:(half + 1) * 64, :, :], V(w1, w1v))
                    c.dma(PEF[half * 64:(half + 1) * 64, :], pe[:, :])
                c.copy(W1[:], W1F[:], e="act")
                c.copy(PEB[:], PEF[:])
                c.dma(W2F[:], w2[:, :])
                c.copy(W2[:], W2F[:])
                bp = PSM.next()
                for p_ in range(32):
                    c.mm(bp[:, 0:1], W1[0:64, p_, :], PEB[0:64, p_:p_ + 1], start=(p_ == 0), stop=(p_ == 31))
                c.copy(HB[:], bp[:, 0:1])
                for g in range(2):
                    hp = PSM.next()
                    for p_ in range(32):
                        c.mm(hp[:, 0:511], W1[g * 64:(g + 1) * 64, p_, :],
                             KC[g * 64:(g + 1) * 64, p_:p_ + 8161:16], start=(p_ == 0), stop=(p_ == 31))
                    c.act(HX[:, 0:511], hp[:, 0:511], AF.Identity, bias=HB[:, 0:1])
                    c.memset(HX[:, 511:512], 0.0)
                    gelu_tanh(c, HT[:], HX[:], H1[:], H2[:])
                    if kv == 0:
                        op_ = PSM.next()
                        c.mm(op_[0:64, :], W2[:], HT[:])
                        c.copy(KCA[g][0:64, :], op_[0:64, :])
                        c.dma(KCA[g][64:72, :], io["kaug_cmp"][:, :])
                    else:
                        for ct in range(4):
                            op_ = PSM.next()
                            c.mm(op_[:, 0:64], HT[:, ct * 128:(ct + 1) * 128], W2[:])
                            c.copy(VCA[:, ct, g, 0:64], op_[:, 0:64])
        c.stack = ph
        c.barrier()

        QAall = [[c.sb([72, 1024], BF16, f"p3_qa{i}_{g}") for g in range(2)] for i in range(8)]
        PT = Rot([c.sb([128, 1024], BF16, f"p3_pt{i}") for i in range(3)])
        PC = [c.sb([128, 1024], BF16, f"p3_pc{i}") for i in range(4)]
        CMa = c.sb([128, 8, 1024], BF16, "p3_cma")
        SMULa = c.sb([128, 8, 128], F32, "p3_smula")
        SADDa = c.sb([128, 8, 128], F32, "p3_sadda")
        IMP = c.sb([128, 128], F32, "p3_imp")
        SCO = c.sb([128, 128], F32, "p3_sco")
        SWK = c.sb([128, 128], F32, "p3_swk")
        M8 = c.sb([128, 16], F32, "p3_m8")
        RDN = c.sb([128, 8], F32, "p3_rdn")
        SEL = c.sb([128, 128], F32, "p3_sel")
        SELV = c.sb([128, 128], F32, "p3_selv")
        SLT = c.sb([128, 128], BF16, "p3_slt")
        PTH = Rot([c.sb([128, 512], BF16, f"p3_pth{i}") for i in range(4)])
        PMH = Rot([c.sb([128, 512], BF16, f"p3_pmh{i}") for i in range(4)])
        HROT = Rot([T(f"p3_hv{i}", PSS.tiles[i // 2].ap[:, (i % 2) * 512:(i % 2 + 1) * 512]) for i in range(4)])
        OS = c.sb([65, 1024], F32, "p3_os")
        BGa = c.sb([128, 8, 48], F32, "p3_bga")
        BGh = [None]
        YB = c.sb([128, 512], F32, "p3_yb")
        COEF = c.sb([128, 8], F32, "p3_coef")
        YBT = Rot([c.sb([128, 128], BF16, f"p3_ybt{i}") for i in range(2)])

        TMP4 = c.sb([128, 256], F32, "p3_tmp4")

        def epilogue(b, g, first):
            c.copy(OS[:, :], PSO[0:65, :], e="act")
            for h0 in (0, 4):
                tp = PSM.next()
                for hh in range(4):
                    h = h0 + hh
                    c.transpose(tp[:, hh * 65:(hh + 1) * 65], OS[0:65, h * 128:(h + 1) * 128], ident[0:65, 0:65])
                cf = V(COEF, COEF.ap[:, h0:h0 + 4])
                den = V(tp, bcast_ap(tp.ap[:, 64:65], [[65, 4]]))
                c.ts(cf, den, 1e-30, None, ALU.max)
                c.recip(cf, cf)
                col0 = 3 * (g * 8 + h0) + b
                c.tt(cf, cf, V(BGa, bcast_ap(BGa.ap[:, BGh[0], col0:col0 + 1], [[3, 4]])), ALU.mult)
                ov = V(tp, bcast_ap(tp.ap[:, 0:1], [[65, 4], [1, 64]]))
                cb = V(COEF, bcast_ap(COEF.ap[:, h0:h0 + 1], [[1, 4], [0, 64]]))
                ybv = V(YB, YB.ap[:, h0 * 64:(h0 + 4) * 64].rearrange("p (h d) -> p h d", d=64))
                if first:
                    c.tt(ybv, ov, cb, ALU.mult)
                else:
                    tv = V(TMP4, TMP4.ap.rearrange("p (h d) -> p h d", d=64))
                    c.tt(tv, ov, cb, ALU.mult)
                    c.tt(ybv, ybv, tv, ALU.add)

        c.dma(BGa[:], V(S["BG"], S["BG"].ap.rearrange("(i p) n -> p i n", p=128)))
        c.dma(SMULa[:], V(io["smul"], io["smul"].ap.rearrange("i p n -> p i n")))
        c.dma(SADDa[:], V(io["sadd"], io["sadd"].ap.rearrange("i p n -> p i n")))
        c.dma(CMa[:], V(io["CM"], io["CM"].ap.rearrange("i p n -> p i n")))
        for i in range(8):
            for g in range(2):
                c.dma(QAall[i][g][0:64, :], V(S["BQT"], S["BQT"].ap[g, :, i, :, :].rearrange("p h q -> p (h q)")))
                c.dma(QAall[i][g][64:72, :], io["qaug"][g, i, :, :])
        c.barrier()
        if after_loads is not None:
            after_loads()
        for i in range(8):
            BGh[0] = i
            SMUL = V(SMULa, SMULa.ap[:, i, :])
            SADD = V(SADDa, SADDa.ap[:, i, :])
            cm = V(CMa, CMa.ap[:, i, :])
            for g in range(2):
                qa = QAall[i][g]
                for ct in range(4):
                    sp = PSS.next()
                    for hf in range(2):
                        cs = slice(hf * 512, (hf + 1) * 512)
                        c.mm(sp[:, cs], KCA[g][0:72, ct * 128:(ct + 1) * 128], qa[0:72, cs],
                             start=True, stop=(ct != 3))
                        if ct == 3:
                            c.mm(sp[:, cs], IDB[:], cm[:, cs], start=False, stop=True)
                    c.act(PC[ct][:], sp[:], AF.Exp, bias=CVAL[:, ct:ct + 1])
                for hf in range(2):
                    cs = slice(hf * 512, (hf + 1) * 512)
                    for ct in range(4):
                        c.mm(PSO[0:65, cs], VCA[:, ct, g, :], PC[ct][:, cs], start=(ct == 0), stop=(ct == 3))
                for h in range(8):
                    ip = PSM.next()
                    for ct in range(4):
                        c.mm(ip[:, 0:129], PC[ct][:, h * 128:(h + 1) * 128], OV[:, ct, :], start=(ct == 0), stop=(ct == 3))
                    c.ts(RDN[:, h:h + 1], ip[:, 128:129], 1e-30, None, ALU.max)
                    c.recip(RDN[:, h:h + 1], RDN[:, h:h + 1])
                    if h == 0:
                        c.ts(IMP[:], ip[:, 0:128], RDN[:, h:h + 1], None, ALU.mult)
                    else:
                        c.stt(IMP[:], ip[:, 0:128], RDN[:, h:h + 1], IMP[:], ALU.mult, ALU.add)
                epilogue(0, g, True)
                c.tt(SCO[:], IMP[:], SMUL[:], ALU.mult)
                c.tt(SCO[:], SCO[:], SADD[:], ALU.add)
                c.op("dve", lambda: c.nc.vector.max(out=M8.ap[:, 0:8], in_=SCO.ap[:]), reads=[SCO], writes=[M8])
                c.op("dve", lambda: c.nc.vector.match_replace(out=SWK.ap[:], in_to_replace=M8.ap[:, 0:8],
                                                              in_values=SCO.ap[:], imm_value=-3.0e38),
                     reads=[SCO, M8], writes=[SWK])
                c.op("dve", lambda: c.nc.vector.max(out=M8.ap[:, 8:16], in_=SWK.ap[:]), reads=[SWK], writes=[M8])
                c.ts(SEL[:], SCO[:], M8[:, 15:16], None, ALU.is_ge)
                c.ts(SELV[:], SCO[:], -1.0e29, None, ALU.is_gt)
                c.tt(SEL[:], SEL[:], SELV[:], ALU.mult)
                def win_s(mw):
                    sp = PSS.next()
                    for hf in range(2):
                        cs = slice(hf * 512, (hf + 1) * 512)
                        edge = (mw == i) or (mw == i + 4)
                        c.mm(sp[:, cs], KW[g][0:72, mw * 128:(mw + 1) * 128], qa[0:72, cs], start=True, stop=(not edge))
                        if mw == i:
                            c.mm(sp[:, cs], IDB[:], TRI2[:, cs], start=False, stop=True)
                        if mw == i + 4:
                            c.mm(sp[:, cs], IDB[:], TRI[:, cs], start=False, stop=True)
                    return (sp,)

                def win_rest(mw, sp):
                    pt = PT.next()
                    c.act(pt[:], sp[:], AF.Exp, bias=WVAL[:, mw:mw + 1])
                    for hf in range(2):
                        cs = slice(hf * 512, (hf + 1) * 512)
                        c.mm(PSO[0:65, cs], VWA[:, mw, g, :], pt[:, cs], start=(mw == i), stop=(mw == i + 4))

                pend = []
                for mw in range(i, i + 5):
                    pend.append((mw,) + win_s(mw))
                    if len(pend) > 1:
                        win_rest(*pend.pop(0))
                while pend:
                    win_rest(*pend.pop(0))
                epilogue(2, g, False)
                tp = PSM.next()
                c.transpose(tp[:, 0:128], SEL[:], ident[:])
                c.copy(SLT[:], tp[:, 0:128], e="act")
                mlast = 56 + i
                mfirst = max(0, mlast - NSA_PRUNE) if g == 0 else 0
                def sel_s(m, hf):
                    mk = mk_of.get(m)
                    if mk is None:
                        mk = PSM.next()
                        c.mm(mk[:, 0:128], EE[:, m * 128:(m + 1) * 128], SLT[:])
                        mk_of.clear()
                        mk_of[m] = mk
                    sp = HROT.next()
                    cs = slice(hf * 512, (hf + 1) * 512)
                    c.mm(sp[:], KS[g][0:72, m * 128:(m + 1) * 128], qa[0:72, cs], start=True, stop=(m != mlast))
                    if m == mlast:
                        c.mm(sp[:], IDB[:], TRI[:, cs], start=False, stop=True)
                    return sp, mk

                def sel_rest(m, hf, sp, mk):
                    cs = slice(hf * 512, (hf + 1) * 512)
                    pt = PTH.next()
                    c.act(pt[:], sp[:], AF.Exp)
                    pm = PMH.next()
                    c.tt(V(pm, pm.ap.rearrange("p (h q) -> p h q", q=128)),
                         V(pt, pt.ap.rearrange("p (h q) -> p h q", q=128)),
                         V(mk, bcast_ap(mk.ap[:, 0:128], [[0, 4], [1, 128]])), ALU.mult)
                    c.mm(PSO[0:65, cs], VSA[:, m, g, :], pm[:], start=(m == mfirst), stop=(m == mlast))

                mk_of = {}
                pend = []
                for m in range(mfirst, mlast + 1):
                    for hf in range(2):
                        pend.append((m, hf) + sel_s(m, hf))
                        if len(pend) > 2:
                            sel_rest(*pend.pop(0))
                while pend:
                    sel_rest(*pend.pop(0))
                epilogue(1, g, False)
                for j in range(4):
                    tp = PSM.next()
                    c.transpose(tp[:, 0:128], YB[:, j * 128:(j + 1) * 128], ident[:])
                    yt = YBT.next()
                    c.copy(yt[:], tp[:, 0:128], e="act")
                    c.dma(S["YBT"][g * 4 + j, :, i * 128:(i + 1) * 128], yt[:])
        c.barrier()
    c.stack = top

def load_w_bf16(c, dst, src_t, src_ap, stg_rot, nk, ncols):
    v = src_ap.rearrange("(k p) n -> p k n", p=128)
    for k in range(nk):
        st = stg_rot.next()
        c.dma(st[:, 0:ncols], V(src_t, v[:, k, :]))
        c.copy(dst[:, k, 0:ncols], st[:, 0:ncols], e=("act", "dve", "pool")[k % 3])


def rms_rows(c, r_out, xin, sq_scratch):
    c.act(sq_scratch, xin, AF.Square, accum=r_out)
    c.ts(r_out, r_out, 1.0 / D, EPS, ALU.mult, ALU.add)
    c.act(r_out, r_out, AF.Sqrt)
    c.recip(r_out, r_out)


def phase4(c, io, S):
    top = c.stack
    with contextlib.ExitStack() as ph:
        c.stack = ph
        ident = c.sb([128, 128], F32, "p4_id")
        c.dma(ident[:], io["ident"][:])
        IDB = c.sb([128, 128], BF16, "p4_idb")
        c.copy(IDB[:], ident[:])
        ones = c.sb([128, 128], BF16, "p4_ones")
        c.memset(ones[:], 1.0)
        STGW = Rot([c.sb([128, 2048], F32, f"p4_stg{i}") for i in range(2)])
        PS = Rot([c.ps([128, 512], F32, f"p4_ps{i}") for i in range(6)])
        PSB = Rot([c.ps([128, 512], BF16, f"p4_psb{i}") for i in range(2)])
        pmix = contextlib.ExitStack()
        c.stack = pmix
        MIXT = c.sb([128, 16, NOWN], BF16, "p4_mixt")
        c.stack = ph
        with contextlib.ExitStack() as pa:
            c.stack = pa
            WUA = c.sb([128, 4, 2048], BF16, "p4_wua")
            WUB = c.sb([128, 8, 2048], BF16, "p4_wub")
            load_w_bf16(c, WUA, io["w_up_a"], io["w_up_a"].ap, STGW, 4, 2048)
            load_w_bf16(c, WUB, io["w_up_b"], io["w_up_b"].ap, STGW, 8, 2048)
            YA = c.sb([128, 4, NOWN], BF16, "p4_ya")
            YBt = c.sb([128, 8, NOWN], BF16, "p4_yb")
            c.dma(YA[:], V(S["YAT"], S["YAT"].ap.rearrange("k p t -> p k t")))
            c.dma(YBt[:], V(S["YBT"], S["YBT"].ap.rearrange("k p t -> p k t")))
            MGA = Rot([c.sb([128, NOWN], BF16, f"p4_mga{i}") for i in range(2)])
            MGB = Rot([c.sb([128, NOWN], BF16, f"p4_mgb{i}") for i in range(2)])
            T1 = Rot([c.sb([128, 512], F32, f"p4_t1{i}") for i in range(2)])
            T2 = Rot([c.sb([128, 512], F32, f"p4_t2{i}") for i in range(2)])
            for j in range(16):
                ga = MGA.next(); gb = MGB.next()
                c.dma(ga[:], S["MG"][j, :, :])
                c.dma(gb[:], S["MG"][16 + j, :, :])
                for hf in range(2):
                    cs = slice(hf * 512, (hf + 1) * 512)
                    pa_ = PS.next()
                    for k in range(4):
                        c.mm(pa_[:], WUA[:, k, j * 128:(j + 1) * 128], YA[:, k, cs], start=(k == 0), stop=(k == 3))
                    pb_ = PS.next()
                    for k in range(8):
                        c.mm(pb_[:], WUB[:, k, j * 128:(j + 1) * 128], YBt[:, k, cs], start=(k == 0), stop=(k == 7))
                    t1 = T1.next(); t2 = T2.next()
                    c.tt(t1[:], pa_[:], ga[:, cs], ALU.mult)
                    c.tt(t2[:], pb_[:], gb[:, cs], ALU.mult)
                    c.tt(MIXT[:, j, cs], t1[:], t2[:], ALU.add, e="pool")
        c.stack = ph
        c.barrier()
        with contextlib.ExitStack() as pb:
            c.stack = pb
            WO = c.sb([128, 16, 2048], BF16, "p4_wo")
            load_w_bf16(c, WO, io["w_out"], io["w_out"].ap, STGW, 16, 2048)
            XO = Rot([c.sb([128, 2048], F32, f"p4_xo{i}") for i in range(2)])
            for tt in range(8):
                xo = XO.next()
                c.dma(xo[:], io["x_own"][tt * 128:(tt + 1) * 128, :])
                for cn in range(4):
                    ps = PS.next()
                    for j in range(16):
                        c.mm(ps[:], MIXT[:, j, tt * 128:(tt + 1) * 128], WO[:, j, cn * 512:(cn + 1) * 512],
                             start=(j == 0), stop=(j == 15))
                    c.tt(xo[:, cn * 512:(cn + 1) * 512], xo[:, cn * 512:(cn + 1) * 512], ps[:], ALU.add)
                c.dma(S["H"][tt * 128:(tt + 1) * 128, :], xo[:])
        c.stack = ph
        c.barrier()
        pmix.close()
        with contextlib.ExitStack() as pc:
            c.stack = pc
            gx = c.sb([128, KC], F32, "p4_gx")
            gm = c.sb([128, KC], F32, "p4_gm")
            c.dma(gx[:], io["g_x"][:])
            c.dma(gm[:], io["g_mem"][:])
            WQ = c.sb([128, 16, 512], BF16, "p4_wq")
            WKV = c.sb([128, 16, 1024], BF16, "p4_wkv")
            WXO = c.sb([128, 4, 2048], BF16, "p4_wxo")
            load_w_bf16(c, WQ, io["w_xq"], io["w_xq"].ap, STGW, 16, 512)
            load_w_bf16(c, WKV, io["w_xkv"], io["w_xkv"].ap, STGW, 16, 1024)
            load_w_bf16(c, WXO, io["w_xo"], io["w_xo"].ap, STGW, 4, 2048)
            HT_ = Rot([c.sb([128, 2048], F32, f"p4_h{i}") for i in range(2)])
            SQ = c.sb([128, 2048], BF16, "p4_sq")
            HB = Rot([c.sb([128, 2048], BF16, f"p4_hb{i}") for i in range(2)])
            RR = Rot([c.sb([128, 1], F32, f"p4_rr{i}") for i in range(4)])
            MNT = c.sb([128, 16, 256], BF16, "p4_mnt")
            KT = c.sb([128, 4, 256], BF16, "p4_kt")
            VM = c.sb([128, 2, 512], BF16, "p4_vm")

            def norm_T(src_tile, dst, tok0, gvec):
                rr = RR.next()
                rms_rows(c, rr[:], src_tile[:], SQ[:])
                hb = HB.next()
                c.act(hb[:], src_tile[:], AF.Copy, scale=rr[:, 0:1])
                for k4 in range(4):
                    tp = PSB.next()
                    for kk in range(4):
                        k = k4 * 4 + kk
                        c.transpose(tp[:, kk * 128:(kk + 1) * 128], hb[:, k * 128:(k + 1) * 128], IDB[:])
                    for kk in range(4):
                        k = k4 * 4 + kk
                        c.ts(dst[:, k, tok0:tok0 + 128], tp[:, kk * 128:(kk + 1) * 128], gvec[:, k:k + 1], None, ALU.mult)

            for mt in range(2):
                m_ = HT_.next()
                c.dma(m_[:], io["mem"][mt * 128:(mt + 1) * 128, :])
                norm_T(m_, MNT, mt * 128, gm)
            for h in range(4):
                ps = PS.next()
                for k in range(16):
                    c.mm(ps[:, 0:256], WKV[:, k, h * 128:(h + 1) * 128], MNT[:, k, :], start=(k == 0), stop=(k == 15))
                c.copy(KT[:, h, :], ps[:, 0:256])
            for mt in range(2):
                ps = PS.next()
                for k in range(16):
                    c.mm(ps[:], MNT[:, k, mt * 128:(mt + 1) * 128], WKV[:, k, 512:1024], start=(k == 0), stop=(k == 15))
                c.copy(VM[:, mt, :], ps[:])
            HNT = Rot([c.sb([128, 16, 512], BF16, f"p4_hnt{i}") for i in range(1)])
            HQ = [c.sb([128, 2048], F32, f"p4_hq{i}") for i in range(4)]
            QT = Rot([c.sb([128, 512], BF16, f"p4_qt{i}") for i in range(2)])
            PTm = Rot([c.sb([128, 512], BF16, f"p4_pt{i}") for i in range(4)])
            OT = c.sb([128, 4, 512], BF16, "p4_ot")
            RD = Rot([c.sb([128, 512], F32, f"p4_rd{i}") for i in range(2)])
            xs = float(128 ** -0.5)
            for quad in range(2):
                hnt = HNT.next()
                for t4 in range(4):
                    tt = quad * 4 + t4
                    c.dma(HQ[t4][:], S["H"][tt * 128:(tt + 1) * 128, :])
                    norm_T(HQ[t4], hnt, t4 * 128, gx)
                for h in range(4):
                    ps = PS.next()
                    for k in range(16):
                        c.mm(ps[:], WQ[:, k, h * 128:(h + 1) * 128], hnt[:, k, :], start=(k == 0), stop=(k == 15))
                    qt = QT.next()
                    c.copy(qt[:], ps[:])
                    pts = []
                    for mt in range(2):
                        sp = PS.next()
                        c.mm(sp[:], KT[:, h, mt * 128:(mt + 1) * 128], qt[:])
                        pt = PTm.next()
                        c.act(pt[:], sp[:], AF.Exp, scale=xs)
                        pts.append(pt)
                    op_ = PS.next(); dp_ = PS.next()
                    for mt in range(2):
                        c.mm(op_[:], VM[:, mt, h * 128:(h + 1) * 128], pts[mt][:], start=(mt == 0), stop=(mt == 1))
                    for mt in range(2):
                        c.mm(dp_[:], ones[:], pts[mt][:], start=(mt == 0), stop=(mt == 1))
                    rd = RD.next()
                    c.recip(rd[:], dp_[:])
                    c.tt(OT[:, h, :], op_[:], rd[:], ALU.mult)
                for t4 in range(4):
                    tt = quad * 4 + t4
                    for cn in range(4):
                        ps = PS.next()
                        for h in range(4):
                            c.mm(ps[:], OT[:, h, t4 * 128:(t4 + 1) * 128], WXO[:, h, cn * 512:(cn + 1) * 512],
                                 start=(h == 0), stop=(h == 3))
                        c.tt(HQ[t4][:, cn * 512:(cn + 1) * 512], HQ[t4][:, cn * 512:(cn + 1) * 512], ps[:], ALU.add)
                    c.dma(S["H2"][tt * 128:(tt + 1) * 128, :], HQ[t4][:])
        c.barrier()
    c.stack = top

def bcast_ap(ap, dims):
    return bass.AP(tensor=ap.tensor, offset=ap.offset, ap=[list(ap.ap[0])] + [list(d) for d in dims])


class ExpertConv:
    def __init__(self, c, io, S):
        self.c, self.io, self.S = c, io, S

    def emit(self):
        c, io, S = self.c, self.io, self.S
        for r in range(16):
            for (src, dst) in (("expert_u", "UB"), ("expert_v", "VB")):
                c.dma(S[dst][r * 1024:(r + 1) * 1024, :], io[src][r * 1024:(r + 1) * 1024, :], eng="pool")

    def close(self):
        pass


def phase5(c, io, S, out_t):
    nc = c.nc
    top = c.stack
    with contextlib.ExitStack() as ph:
        c.stack = ph
        ident = c.sb([128, 128], F32, "p5_id")
        c.dma(ident[:], io["ident"][:])
        IDB = c.sb([128, 128], BF16, "p5_idb")
        c.copy(IDB[:], ident[:])
        WPQ = c.sb([128, 16, 2048], BF16, "p5_wpq")
        with contextlib.ExitStack() as pl:
            c.stack = pl
            STGW = Rot([c.sb([128, 2048], F32, f"p5_stg{i}") for i in range(2)])
            load_w_bf16(c, WPQ, io["w_pq"], io["w_pq"].ap, STGW, 16, 2048)
            c.barrier()
        c.stack = ph
        SKF = c.sb([128, 2, 128], F32, "p5_skf")
        SKT = c.sb([128, 2, 128], BF16, "p5_skt")
        c.dma(SKF[:, 0, :], io["sk1_T"][:, :])
        c.dma(SKF[:, 1, :], io["sk2_T"][:, :])
        c.copy(SKT[:], SKF[:])
        GBC = c.sb([128, D], F32, "p5_gbc")
        GFIN = c.sb([128, D], F32, "p5_gfin")
        c.dma(GBC[:], io["g_ffn_bc"][:, :])
        c.dma(GFIN[:], io["g_fin_bc"][:, :])
        HT2 = [c.sb([128, D], F32, f"p5_ht{i}") for i in range(2)]
        XN32 = [c.sb([128, D], F32, f"p5_xn3{i}") for i in range(2)]
        XB = c.sb([128, D], BF16, "p5_xb")
        XT3 = c.sb([128, 16, 128], BF16, "p5_xt3")
        QT = c.sb([128, 16, 128], BF16, "p5_qt")
        SC = c.sb([128, 16, 128], F32, "p5_sc")
        WK = c.sb([128, 256], F32, "p5_wk")
        V16 = c.sb([128, 16, 16], F32, "p5_v16")
        I16 = c.sb([128, 16, 16], U32, "p5_i16")
        IF = c.sb([128, 16, 16], F32, "p5_if")
        CA = c.sb([128, 256], F32, "p5_ca")
        EI = c.sb([128, 256], F32, "p5_ei")
        JK2 = c.sb([128, 256], F32, "p5_jk2")
        VS = c.sb([128, 16], F32, "p5_vs")
        NB = c.sb([128, 1], F32, "p5_nb")
        ZS = c.sb([128, 1], F32, "p5_zs")
        EIDS = c.sb([128, 128], F32, "p5_eids")
        EI322 = [c.sb([128, 128], I32, f"p5_ei32{i}") for i in range(2)]
        GW2 = [c.sb([128, 128], F32, f"p5_gw{i}") for i in range(2)]
        AA2 = [c.sb([128, 128], F32, f"p5_aa{i}") for i in range(2)]
        G1 = c.sb([128, 128], F32, "p5_g1")
        G2 = c.sb([128, 128], F32, "p5_g2")
        WT2 = [c.sb([128, 128], F32, f"p5_wt{i}") for i in range(2)]
        RR = c.sb([128, 1], F32, "p5_rr")
        RR2 = c.sb([128, 1], F32, "p5_rr2")
        UG = Rot([c.sb([128, D], BF16, f"p5_ug{i}") for i in range(5)])
        VG = Rot([c.sb([128, D], BF16, f"p5_vg{i}") for i in range(5)])
        TB = Rot([c.sb([128, D], BF16, f"p5_tb{i}") for i in range(2)])
        JK = c.sb([128, D], BF16, "p5_jk")
        JKA = c.sb([128, D], BF16, "p5_jka")
        YP = c.ps([128, D], F32, "p5_yp")
        PS = Rot([c.ps([128, 512], F32, f"p5_ps{i}") for i in range(2)])
        PSB = c.ps([128, 1024], BF16, "p5_psb")

        def top16(src, ncol, mv, iu):
            c.op("dve", lambda: nc.vector.max(out=mv.ap[:, 0:8], in_=src.ap), reads=[src], writes=[mv])
            if iu is not None:
                c.op("dve", lambda: nc.vector.max_index(out=iu.ap[:, 0:8], in_max=mv.ap[:, 0:8], in_values=src.ap),
                     reads=[src, mv], writes=[iu])
            c.op("dve", lambda: nc.vector.match_replace(out=WK.ap[:, 0:ncol], in_to_replace=mv.ap[:, 0:8],
                                                        in_values=src.ap, imm_value=-3.0e38),
                 reads=[src, mv], writes=[WK])
            c.op("dve", lambda: nc.vector.max(out=mv.ap[:, 8:16], in_=WK.ap[:, 0:ncol]), reads=[WK], writes=[mv])
            if iu is not None:
                c.op("dve", lambda: nc.vector.max_index(out=iu.ap[:, 8:16], in_max=mv.ap[:, 8:16],
                                                        in_values=WK.ap[:, 0:ncol]),
                     reads=[WK, mv], writes=[iu])

        def stage_a(tt):
            HT, XN3, EI32, GW = HT2[tt % 2], XN32[tt % 2], EI322[tt % 2], GW2[tt % 2]
            c.dma(HT[:], S["H2"][tt * 128:(tt + 1) * 128, :])
            rms_rows(c, RR[:], HT[:], JKA[:])
            c.stt(XN3[:], HT[:], RR[:, 0:1], GBC[:], ALU.mult, ALU.mult)
            c.copy(XB[:], XN3[:], e="act")
            for k4 in range(4):
                for kk in range(4):
                    k = k4 * 4 + kk
                    c.transpose(PSB[:, kk * 128:(kk + 1) * 128], XB[:, k * 128:(k + 1) * 128], IDB[:])
                c.copy(V(XT3, XT3.ap[:, k4 * 4:(k4 + 1) * 4, :].rearrange("p a b -> p (a b)")), PSB[:, 0:512])
            for c4 in range(4):
                ps = PS.next()
                for cc_ in range(4):
                    cq = c4 * 4 + cc_
                    for k in range(16):
                        c.mm(ps[:, cc_ * 128:(cc_ + 1) * 128], WPQ[:, k, cq * 128:(cq + 1) * 128], XT3[:, k, :],
                             start=(k == 0), stop=(k == 15))
                c.copy(V(QT, QT.ap[:, c4 * 4:(c4 + 1) * 4, :].rearrange("p a b -> p (a b)")), ps[:], e="act")
            for c4 in range(4):
                ps = PS.next()
                for cc_ in range(4):
                    cq = c4 * 4 + cc_
                    c.mm(ps[:, cc_ * 128:(cc_ + 1) * 128], QT[:, cq, :], SKT[:, cq % 2, :])
                c.copy(V(SC, SC.ap[:, c4 * 4:(c4 + 1) * 4, :].rearrange("p a b -> p (a b)")), ps[:], e="act")
            for cq in range(16):
                top16(SC[:, cq, :], 128, V(V16, V16.ap[:, cq, :]), V(I16, I16.ap[:, cq, :]))
            c.copy(IF[:], I16[:])
            c.memset(EIDS[:], 0.0)
            for h in range(8):
                a0 = V(V16, bcast_ap(V16.ap[:, 2 * h, :], [[1, 16], [0, 16]]))
                a1 = V(V16, bcast_ap(V16.ap[:, 2 * h + 1, :], [[0, 16], [1, 16]]))
                c.tt(V(CA, CA.ap.rearrange("p (a b) -> p a b", b=16)), a0, a1, ALU.add)
                e0 = V(IF, bcast_ap(IF.ap[:, 2 * h, :], [[1, 16], [0, 16]]))
                e1 = V(IF, bcast_ap(IF.ap[:, 2 * h + 1, :], [[0, 16], [1, 16]]))
                c.stt(V(EI, EI.ap.rearrange("p (a b) -> p a b", b=16)), e0, 128.0, e1, ALU.mult, ALU.add)
                top16(CA[:], 256, VS[:], None)
                for k in range(16):
                    c.stt(JK2[:], CA[:], VS[:, k:k + 1], EI[:], ALU.is_equal, ALU.mult,
                          accum=EIDS[:, h * 16 + k:h * 16 + k + 1])
                c.ts(NB[:], VS[:, 0:1], -1.0, None, ALU.mult)
                c.memset(ZS[:], 0.0)
                c.act(GW[:, h * 16:(h + 1) * 16], VS[:], AF.Exp, bias=NB[:, 0:1], accum=ZS[:])
                c.recip(ZS[:], ZS[:])
                c.ts(GW[:, h * 16:(h + 1) * 16], GW[:, h * 16:(h + 1) * 16], ZS[:, 0:1], None, ALU.mult)
            c.ts(EIDS[:], EIDS[:], 0.0, float(128 * 128 - 1), ALU.max, ALU.min)
            c.copy(EI32[:], EIDS[:])
            c.memset(AA2[tt % 2][:], 0.0)

        def step_u(tt, j):
            EI32, XN3, AA = EI322[tt % 2], XN32[tt % 2], AA2[tt % 2]
            ug = UG.next()
            c.op("pool", lambda ug=ug, j=j: nc.gpsimd.indirect_dma_start(
                out=ug.ap[:, :], out_offset=None, in_=S["UB"].ap[:, :],
                in_offset=bass.IndirectOffsetOnAxis(ap=EI32.ap[:, j:j + 1], axis=0)),
                reads=[EI32, S["UB"]], writes=[ug], dma=True)
            c.stt(JK[:], ug[:], 1.0, XN3[:], ALU.mult, ALU.mult, accum=AA[:, j:j + 1])

        def stage_g(tt):
            WT = WT2[tt % 2]
            gelu_tanh(c, WT[:], AA2[tt % 2][:], G1[:], G2[:])
            c.tt(WT[:], WT[:], GW2[tt % 2][:], ALU.mult)

        def step_v(tt, j):
            EI32, WT = EI322[tt % 2], WT2[tt % 2]
            vg = VG.next()
            c.op("pool", lambda vg=vg, j=j: nc.gpsimd.indirect_dma_start(
                out=vg.ap[:, :], out_offset=None, in_=S["VB"].ap[:, :],
                in_offset=bass.IndirectOffsetOnAxis(ap=EI32.ap[:, j:j + 1], axis=0)),
                reads=[EI32, S["VB"]], writes=[vg], dma=True)
            tb = TB.next()
            c.act(tb[:], vg[:], AF.Copy, scale=WT[:, j:j + 1])
            for cn in range(4):
                c.mm(YP[:, cn * 512:(cn + 1) * 512], IDB[:], tb[:, cn * 512:(cn + 1) * 512],
                     start=(j == 0), stop=(j == 127))

        def stage_f(tt):
            HT = HT2[tt % 2]
            for cn in range(4):
                c.tt(HT[:, cn * 512:(cn + 1) * 512], HT[:, cn * 512:(cn + 1) * 512], YP[:, cn * 512:(cn + 1) * 512], ALU.add)
            rms_rows(c, RR2[:], HT[:], JKA[:])
            OUTB = XN32[tt % 2]
            c.stt(OUTB[:], HT[:], RR2[:, 0:1], GFIN[:], ALU.mult, ALU.mult)
            c.dma(out_t[tt * 128:(tt + 1) * 128, :], OUTB[:])

        stage_a(0)
        for j in range(128):
            step_u(0, j)
        stage_g(0)
        for tt in range(8):
            if tt < 7:
                stage_a(tt + 1)
            for j in range(128):
                step_v(tt, j)
                if tt < 7:
                    step_u(tt + 1, j)
            stage_f(tt)
            if tt < 7:
                stage_g(tt + 1)
        c.barrier()
    c.stack = top

def scratch_spec():
    return {
        "XN": ([16, 128, KC, 512], BF16),
        "AQT": ([3, 4, 128, NOWN], BF16),
        "AKT": ([3, 4, 128, 3072], BF16),
        "AV": ([3, 3072, 512], BF16),
        "BQT": ([2, 64, 8, 8, 128], BF16),
        "KCT": ([128, NW], BF16),
        "VCT": ([128, NW], BF16),
        "KST": ([2, 64, NW], BF16),
        "VS": ([NW, 128], BF16),
        "KWT": ([2, 64, 1536], BF16),
        "VW": ([1536, 128], BF16),
        "BG": ([NOWN, 48], F32),
        "MG": ([32, 128, NOWN], BF16),
        "YAT": ([4, 128, NOWN], BF16),
        "YBT": ([8, 128, NOWN], BF16),
        "H": ([NOWN, D], F32),
        "H2": ([NOWN, D], F32),
        "UB": ([16384, D], BF16),
        "VB": ([16384, D], BF16),
    }


def input_spec():
    return {
        "xT": ([16, 128, KC, 512], F32),
        "w_in": ([D, IN_COLS], F32),
        "g_mix": ([128, KC], F32),
        "a_bias": ([128, AB_COLS], F32),
        "a_kvalid": ([128, 24], F32),
        "kaug_sel": ([8, NW], BF16), "kaug_cmp": ([8, 512], BF16), "qaug": ([2, 8, 8, 1024], BF16),
        "EE": ([128, NW], BF16), "TRI": ([128, 1024], BF16), "TRI2": ([128, 1024], BF16),
        "CM": ([8, 128, 1024], BF16), "OV": ([4, 128, 129], BF16), "ident": ([128, 128], F32),
        "cval": ([128, 4], F32), "wval": ([128, 12], F32),
        "smul": ([8, 128, 128], F32), "sadd": ([8, 128, 128], F32),
        "x_own": ([NOWN, D], F32), "mem": ([256, D], F32),
        "w_up_a": ([512, D], F32), "w_up_b": ([1024, D], F32), "w_out": ([D, D], F32),
        "w_xq": ([D, 512], F32), "w_xkv": ([D, 1024], F32), "w_xo": ([512, D], F32),
        "g_x": ([128, KC], F32), "g_mem": ([128, KC], F32),
        "w_pq": ([D, D], F32), "sk1_T": ([128, 128], F32), "sk2_T": ([128, 128], F32),
        "g_ffn_bc": ([128, D], F32), "g_fin_bc": ([128, D], F32),
        "expert_u": ([16384, D], F32), "expert_v": ([16384, D], F32),
        "w_cmp_k1": ([2048, 128], F32), "w_cmp_k2": ([128, 64], F32), "pe_k_T": ([64, 32], F32),
        "w_cmp_v1": ([2048, 128], F32), "w_cmp_v2": ([128, 64], F32), "pe_v_T": ([64, 32], F32),
    }


def build(debug_out=(), upto=99, skip=()):
    nc = bass.Bass("TRN2", target_bir_lowering=False)
    with contextlib.ExitStack() as st:
        c = Ctx(nc, st)
        io = {k: c.dram(k, shp, dt, kind="ExternalInput") for k, (shp, dt) in input_spec().items()}
        S = {}
        for k, (shp, dt) in scratch_spec().items():
            S[k] = c.dram("s_" + k, shp, dt, kind=("ExternalOutput" if k in debug_out else "Internal"))
        conv = ExpertConv(c, io, S)
        phase1(c, io, S)
        if upto >= 2 and 2 not in skip:
            phase2(c, io, S)
        if upto >= 3 and 3 not in skip:
            phase3(c, io, S, after_loads=conv.emit)
        else:
            conv.emit()
        c.barrier()
        conv.close()
        if upto >= 4 and 4 not in skip:
            phase4(c, io, S)
        out_t = c.dram("out", [NOWN, D], F32, kind="ExternalOutput")
        if upto >= 5:
            phase5(c, io, S, out_t)
        c.finish([])
    return nc, c


_CACHE = {}


def _gl(v):
    return np.ascontiguousarray(np.asarray(v, np.float32).reshape(16, 128).T)


def make_inputs(inp, cores=range(8)):
    x = np.asarray(inp["x"], np.float32)[0]
    xT = np.ascontiguousarray(x.T)
    st = host_b_static()
    shared = {
        "w_in": np.asarray(inp["w_in"], np.float32)[0],
        "g_mix": _gl(inp["norm_mix"][0]),
        "mem": np.asarray(inp["mem"], np.float32)[0],
        "g_x": _gl(inp["norm_x"][0]), "g_mem": _gl(inp["norm_mem"][0]),
        "pe_k_T": np.ascontiguousarray(np.asarray(inp["pe_cmp_k"], np.float32)[0].T),
        "pe_v_T": np.ascontiguousarray(np.asarray(inp["pe_cmp_v"], np.float32)[0].T),
        "sk1_T": np.ascontiguousarray(np.asarray(inp["sub_keys1"], np.float32)[0].T),
        "sk2_T": np.ascontiguousarray(np.asarray(inp["sub_keys2"], np.float32)[0].T),
        "g_ffn_bc": np.ascontiguousarray(np.broadcast_to(np.asarray(inp["norm_ffn"], np.float32)[0][None, :], (128, D))),
        "g_fin_bc": np.ascontiguousarray(np.broadcast_to(np.asarray(inp["norm_final"], np.float32)[None, :], (128, D))),
        "expert_u": np.asarray(inp["expert_u"], np.float32)[0],
        "expert_v": np.asarray(inp["expert_v"], np.float32)[0],
    }
    for k in ("w_cmp_k1", "w_cmp_k2", "w_cmp_v1", "w_cmp_v2", "w_up_a", "w_up_b", "w_out", "w_xq", "w_xkv", "w_xo", "w_pq"):
        shared[k] = np.asarray(inp[k], np.float32)[0]
    shared.update(st)
    maps = []
    for cc in cores:
        lo = 1024 * cc - 7168
        w = np.zeros((D, NW), np.float32)
        if lo >= 0:
            w[:] = xT[:, lo:lo + NW]
        else:
            w[:, -lo:] = xT[:, :NW + lo]
        tab, kval = host_a_tables(cc)
        d = dict(shared)
        w = np.ascontiguousarray(w.reshape(KC, 128, 16, 512).transpose(2, 1, 0, 3))
        d.update({"xT": w, "x_own": np.ascontiguousarray(x[1024 * cc:1024 * cc + 1024]), "a_bias": tab, "a_kvalid": kval})
        d.update(host_b_core(cc))
        maps.append(d)
    return maps


def kernel(**inputs):
    if "nc" not in _CACHE:
        _CACHE["nc"] = build()[0]
    nc = _CACHE["nc"]
    maps = make_inputs(inputs)
    res = run_bass_kernel_spmd(nc, maps, core_ids=list(range(8)))
    out = np.concatenate([np.asarray(r["out"], np.float32) for r in res.results], axis=0)
    return out.reshape(1, 8192, D)
```

```python
import contextlib
from concourse.bass_utils import run_bass_kernel_spmd
import numpy as np
import concourse.bass as bass
import concourse.mybir as mybir

F32 = mybir.dt.float32
BF16 = mybir.dt.bfloat16
I32 = mybir.dt.int32
U32 = mybir.dt.uint32
ALU = mybir.AluOpType
AF = mybir.ActivationFunctionType
AX = mybir.AxisListType


class T:
    def __init__(self, name, ap):
        self.name = name
        self.ap = ap
        self.w = None
        self.r = []

    def __getitem__(self, k):
        return V(self, self.ap[k])


class V:
    def __init__(self, t, ap):
        self.t = t
        self.ap = ap

    def __getitem__(self, k):
        return V(self.t, self.ap[k])


class Ctx:
    ENG = ("pe", "act", "dve", "pool", "sp")

    def __init__(self, nc, stack):
        self.nc = nc
        self.stack = stack
        self.eng = {"pe": nc.tensor, "act": nc.scalar, "dve": nc.vector,
                    "pool": nc.gpsimd, "sp": nc.sync}
        self.CE = ("pe", "act", "dve", "pool")
        self.sem = {e: stack.enter_context(nc.semaphore("s_" + e)) for e in self.CE}
        self.cnt = {e: 0 for e in self.CE}
        self.NS = 12
        self.dsem = {q: [stack.enter_context(nc.semaphore(f"d_{q}{i}")) for i in range(self.NS)]
                     for q in ("sp", "pool")}
        self.dcnt = {q: 0 for q in ("sp", "pool")}
        self.waited = {e: {} for e in self.ENG}
        self.n_inst = 0
        self.uid = 0

    def sb(self, shape, dt, name=None):
        self.uid += 1
        name = name or f"sb{self.uid}"
        h = self.stack.enter_context(self.nc.sbuf_tensor(name, list(shape), dt))
        return T(name, h)

    def ps(self, shape, dt=F32, name=None):
        self.uid += 1
        name = name or f"ps{self.uid}"
        h = self.stack.enter_context(self.nc.psum_tensor(name, list(shape), dt))
        return T(name, h)

    def dram(self, name, shape, dt, kind="Internal"):
        h = self.nc.dram_tensor(name, list(shape), dt, kind=kind)
        return T(name, h.ap())

    def _semof(self, key):
        if isinstance(key, tuple):
            return self.dsem[key[0]][key[1]]
        return self.sem[key]

    def _need(self, e, dep):
        if dep is None:
            return
        key, val = dep
        if self.waited[e].get(key, 0) >= val:
            return
        self.eng[e].wait_ge(self._semof(key), val)
        self.waited[e][key] = val

    def op(self, e, fn, reads=(), writes=(), pe_chain=False, dma=False):
        rt = [v.t if isinstance(v, V) else v for v in reads]
        wt = [v.t if isinstance(v, V) else v for v in writes]
        for t in rt:
            self._need(e, t.w)
        for t in wt:
            if not (pe_chain and t.w is not None and t.w[0] == e):
                self._need(e, t.w)
            for d in t.r:
                self._need(e, d)
        if dma:
            n = self.dcnt[e]
            slot = n % self.NS
            val = 16 * (n // self.NS + 1)
            key = (e, slot)
            if n >= self.NS:
                self._need(e, (key, val - 16))
            ins = fn()
            ins.then_inc(self.dsem[e][slot], 16)
            self.dcnt[e] += 1
            tok = (key, val)
        else:
            ins = fn()
            self.cnt[e] += 1
            ins.then_inc(self.sem[e], 1)
            tok = (e, self.cnt[e])
        for t in rt:
            t.r.append(tok)
            if len(t.r) > 48:
                last = {}
                for (x, s_) in t.r:
                    last[x] = max(last.get(x, 0), s_)
                t.r = list(last.items())
        for t in wt:
            t.w = tok
            t.r = []
        self.n_inst += 1
        return ins

    def barrier(self):
        for e in self.ENG:
            for x in self.CE:
                if x != e and self.cnt[x] > 0:
                    self._need(e, (x, self.cnt[x]))
            for q in ("sp", "pool"):
                n = self.dcnt[q]
                for slot in range(min(n, self.NS)):
                    k = (n - 1 - slot) // self.NS + 1 if n - 1 >= slot else 0
                    cnt_slot = (n - slot + self.NS - 1) // self.NS
                    if cnt_slot > 0:
                        self._need(e, ((q, slot), 16 * cnt_slot))

    def dma(self, out, in_, eng="sp", **kw):
        return self.op(eng, lambda: self.eng[eng].dma_start(out=out.ap, in_=in_.ap, **kw),
                       reads=[in_], writes=[out], dma=True)

    def mm(self, out, lhsT, rhs, start=True, stop=True):
        return self.op("pe", lambda: self.nc.tensor.matmul(out.ap, lhsT.ap, rhs.ap, start=start, stop=stop),
                       reads=[lhsT, rhs], writes=[out], pe_chain=True)

    def transpose(self, out, in_, ident):
        return self.op("pe", lambda: self.nc.tensor.transpose(out.ap, in_.ap, ident.ap),
                       reads=[in_, ident], writes=[out], pe_chain=True)

    def act(self, out, in_, func, bias=None, scale=None, accum=None, extra_reads=()):
        kw = {}
        rd = [in_] + list(extra_reads)
        wr = [out]
        if bias is not None:
            if isinstance(bias, V):
                kw["bias"] = bias.ap; rd.append(bias)
            else:
                kw["bias"] = bias
        if scale is not None:
            if isinstance(scale, V):
                kw["scale"] = scale.ap; rd.append(scale)
            else:
                kw["scale"] = scale
        if accum is not None:
            kw["accum_out"] = accum.ap; wr.append(accum)
        return self.op("act", lambda: self.nc.scalar.activation(out.ap, in_.ap, func, **kw),
                       reads=rd, writes=wr)

    def _ve(self, e):
        return self.nc.vector if e == "dve" else self.nc.gpsimd

    def tt(self, out, in0, in1, op, e="dve"):
        return self.op(e, lambda: self._ve(e).tensor_tensor(out.ap, in0.ap, in1.ap, op),
                       reads=[in0, in1], writes=[out])

    def ts(self, out, in0, s1, s2, op0, op1=None, e="dve", accum=None):
        rd = [in0]; wr = [out]
        a1 = s1.ap if isinstance(s1, V) else s1
        a2 = s2.ap if isinstance(s2, V) else s2
        if isinstance(s1, V): rd.append(s1)
        if isinstance(s2, V): rd.append(s2)
        kw = {}
        if op1 is not None: kw["op1"] = op1
        if accum is not None:
            kw["accum_out"] = accum.ap; wr.append(accum)
        return self.op(e, lambda: self._ve(e).tensor_scalar(out.ap, in0.ap, a1, a2, op0, **kw),
                       reads=rd, writes=wr)

    def stt(self, out, in0, s, in1, op0, op1, e="dve", accum=None):
        rd = [in0, in1]; wr = [out]
        a = s.ap if isinstance(s, V) else s
        if isinstance(s, V): rd.append(s)
        kw = {}
        if accum is not None:
            kw["accum_out"] = accum.ap; wr.append(accum)
        return self.op(e, lambda: self._ve(e).scalar_tensor_tensor(out.ap, in0.ap, a, in1.ap, op0, op1, **kw),
                       reads=rd, writes=wr)

    def copy(self, out, in_, e="dve"):
        if e == "act":
            return self.op("act", lambda: self.nc.scalar.copy(out.ap, in_.ap), reads=[in_], writes=[out])
        return self.op(e, lambda: self._ve(e).tensor_copy(out.ap, in_.ap), reads=[in_], writes=[out])

    def recip(self, out, in_):
        return self.op("dve", lambda: self.nc.vector.reciprocal(out.ap, in_.ap), reads=[in_], writes=[out])

    def memset(self, out, val, e="dve"):
        return self.op(e, lambda: self._ve(e).memset(out.ap, val), reads=[], writes=[out])

    def finish(self, out_tensors):
        self.barrier()

D = 2048
NW = 8192
NOWN = 1024
KC = 16
C_AQ, C_AK, C_AV = 0, 1536, 3072
C_BQ = 4608
C_BKV = 5632
C_BG = 6400
C_MG = 6448
IN_COLS = 10544
EPS = 1e-6


class Rot:
    def __init__(self, tiles):
        self.tiles = tiles
        self.i = 0

    def next(self):
        t = self.tiles[self.i % len(self.tiles)]
        self.i += 1
        return t


def phase1(c, io, S):
    nc = c.nc
    top = c.stack
    with contextlib.ExitStack() as ph:
        c.stack = ph
        ones = c.sb([128, 128], BF16, "p1_ones")
        c.memset(ones[:], 1.0)
        gmix = c.sb([128, KC], F32, "p1_g")
        c.dma(gmix[:], io["g_mix"][:])
        with contextlib.ExitStack() as pa:
            c.stack = pa
            XT = [c.sb([128, KC, 512], F32, f"p1_xt{i}") for i in range(2)]
            XN = [c.sb([128, KC, 512], BF16, f"p1_xn{i}") for i in range(2)]
            SQ = Rot([c.sb([128, 512], BF16, f"p1_sq{i}") for i in range(3)])
            SSP = Rot([c.ps([128, 512], F32, f"p1_ss{i}") for i in range(2)])
            RB = Rot([c.sb([128, 512], F32, f"p1_rb{i}") for i in range(2)])
            for ch in range(16):
                xt = XT[ch % 2]
                xn = XN[ch % 2]
                c.dma(xt[:], V(io["xT"], io["xT"].ap[ch]))
                ss = SSP.next()
                for k in range(KC):
                    sq = SQ.next()
                    c.act(sq[:], xt[:, k, :], AF.Square)
                    c.mm(ss[:], ones[:], sq[:], start=(k == 0), stop=(k == KC - 1))
                rb = RB.next()
                c.ts(rb[:], ss[:], 1.0 / D, EPS, ALU.mult, ALU.add)
                c.act(rb[:], rb[:], AF.Sqrt)
                c.recip(rb[:], rb[:])
                for k in range(KC):
                    c.stt(xn[:, k, :], xt[:, k, :], gmix[:, k:k + 1], rb[:], ALU.mult, ALU.mult)
                c.dma(V(S["XN"], S["XN"].ap[ch]), xn[:], eng="pool")
        c.barrier()
        with contextlib.ExitStack() as pb:
            c.stack = pb
            WFs = [c.sb([128, KC, 512], F32, f"p1_wf{i}") for i in range(2)]
            XOWN = {}
            WB = [c.sb([128, KC, 512], BF16, f"p1_wb{i}") for i in range(2)]
            XNL = Rot([c.sb([128, KC, 512], BF16, f"p1_xl{i}") for i in range(2)])
            PS = Rot([c.ps([128, 512], F32, f"p1_ps{i}") for i in range(4)])
            STG = Rot([c.sb([128, 512], BF16, f"p1_st{i}") for i in range(4)])
            STGF = Rot([c.sb([128, 64], F32, f"p1_sf{i}") for i in range(2)])
            wv = io["w_in"].ap.rearrange("(k p) n -> p k n", p=128)
            slab_i = [0]

            def load_slab(col0, ncols):
                wb = WB[slab_i[0] % 2]
                WF = WFs[slab_i[0] % 2]
                slab_i[0] += 1
                c.dma(WF[:, :, 0:ncols], V(io["w_in"], wv[:, :, col0:col0 + ncols]))
                for k in range(KC):
                    e = ("act", "dve")[k % 2]
                    c.copy(wb[:, k, 0:ncols], WF[:, k, 0:ncols], e=e)
                return wb

            def load_xn(ch):
                if ch in XOWN:
                    return XOWN[ch]
                t = XNL.next()
                c.dma(t[:], V(S["XN"], S["XN"].ap[ch]))
                return t

            def fm(wb, xl, c0, M, evac):
                ps = PS.next()
                for k in range(KC):
                    c.mm(ps[0:M, :], wb[:, k, c0:c0 + M], xl[:, k, :], start=(k == 0), stop=(k == KC - 1))
                evac(ps)

            def tm(wb, xl, tt, c0, ncols, evac):
                ps = PS.next()
                for k in range(KC):
                    c.mm(ps[:, 0:ncols], xl[:, k, tt * 128:(tt + 1) * 128], wb[:, k, c0:c0 + ncols],
                         start=(k == 0), stop=(k == KC - 1))
                evac(ps)

            def ev_fm(dst_t, dst_ap, M, func=None, scale=None):
                def f(ps):
                    st = STG.next()
                    if func is None and scale is None:
                        c.copy(st[0:M, :], ps[0:M, :], e="dve")
                    else:
                        c.act(st[0:M, :], ps[0:M, :], func or AF.Copy, scale=scale)
                    c.dma(V(dst_t, dst_ap), st[0:M, :], eng="pool")
                return f

            def ev_tm(dst_t, dst_ap, ncols):
                def f(ps):
                    st = STG.next()
                    c.copy(st[:, 0:ncols], ps[:, 0:ncols], e="dve")
                    c.dma(V(dst_t, dst_ap), st[:, 0:ncols], eng="pool")
                return f

            for ch_ in (14, 15):
                t_ = c.sb([128, KC, 512], BF16, f"p1_xown{ch_}")
                c.dma(t_[:], V(S["XN"], S["XN"].ap[ch_]))
                XOWN[ch_] = t_
            for g in range(3):
                wb = load_slab(C_AQ + g * 512, 512)
                for ch in (14, 15):
                    xl = load_xn(ch)
                    o0 = (ch - 14) * 512
                    for h in range(4):
                        fm(wb, xl, h * 128, 128, ev_fm(S["AQT"], S["AQT"].ap[g, h, :, o0:o0 + 512], 128))
                chs = (13, 14, 15) if g < 2 else (10, 11, 12, 13, 14, 15)
                wb = load_slab(C_AK + g * 512, 512)
                for ch in chs:
                    xl = load_xn(ch)
                    o0 = (ch - 10) * 512
                    for h in range(4):
                        fm(wb, xl, h * 128, 128, ev_fm(S["AKT"], S["AKT"].ap[g, h, :, o0:o0 + 512], 128))
                wb = load_slab(C_AV + g * 512, 512)
                for ch in chs:
                    xl = load_xn(ch)
                    o0 = (ch - 10) * 512
                    for tt in range(4):
                        tm(wb, xl, tt, 0, 512, ev_tm(S["AV"], S["AV"].ap[g, o0 + tt * 128:o0 + (tt + 1) * 128, :], 512))
            for g in range(2):
                wb = load_slab(C_BQ + g * 512, 512)
                for ch in (14, 15):
                    xl = load_xn(ch)
                    for h in range(8):
                        dst = S["BQT"].ap[g, :, (ch - 14) * 4:(ch - 14) * 4 + 4, h, :]
                        def f(ps, dst=dst):
                            st = STG.next()
                            c.act(st[0:64, :], ps[0:64, :], AF.Copy, scale=0.125)
                            c.dma(V(S["BQT"], dst), V(st, st.ap[0:64, :].rearrange("p (i q) -> p i q", q=128)), eng="pool")
                        fm(wb, xl, h * 64, 64, f)
            wb = load_slab(C_BKV, 512)
            for ch in range(16):
                xl = load_xn(ch)
                o0 = ch * 512
                fm(wb, xl, 0, 128, ev_fm(S["KCT"], S["KCT"].ap[:, o0:o0 + 512], 128))
                fm(wb, xl, 128, 128, ev_fm(S["VCT"], S["VCT"].ap[:, o0:o0 + 512], 128))
                fm(wb, xl, 256, 128, ev_fm(S["KST"], S["KST"].ap.rearrange("g d w -> (g d) w")[:, o0:o0 + 512], 128))
                for tt in range(4):
                    tm(wb, xl, tt, 384, 128, ev_tm(S["VS"], S["VS"].ap[o0 + tt * 128:o0 + (tt + 1) * 128, :], 128))
            wb = load_slab(C_BKV + 512, 256 + 48)
            for ch in (13, 14, 15):
                xl = load_xn(ch)
                o0 = (ch - 13) * 512
                fm(wb, xl, 0, 128, ev_fm(S["KWT"], S["KWT"].ap.rearrange("g d w -> (g d) w")[:, o0:o0 + 512], 128))
                for tt in range(4):
                    tm(wb, xl, tt, 128, 128, ev_tm(S["VW"], S["VW"].ap[o0 + tt * 128:o0 + (tt + 1) * 128, :], 128))
                if ch >= 14:
                    for tt in range(4):
                        r0 = (ch - 14) * 512 + tt * 128
                        def f(ps, r0=r0):
                            sf = STGF.next()
                            c.act(sf[:, 0:48], ps[:, 0:48], AF.Sigmoid)
                            c.dma(S["BG"][r0:r0 + 128, :], sf[:, 0:48], eng="pool")
                        tm(wb, xl, tt, 256, 48, f)
            for sl in range(8):
                wb = load_slab(C_MG + sl * 512, 512)
                for ch in (14, 15):
                    xl = load_xn(ch)
                    o0 = (ch - 14) * 512
                    for j in range(4):
                        fm(wb, xl, j * 128, 128,
                           ev_fm(S["MG"], S["MG"].ap[sl * 4 + j, :, o0:o0 + 512], 128, func=AF.Sigmoid))
        c.barrier()
    c.stack = top

A_DIL = (1, 4, 16)
A_J = [128 * (d + 1) + 768 for d in A_DIL]
A_JOFF = []
_o = 0
for _g in range(3):
    for _h in range(4):
        A_JOFF.append(_o)
        _o += A_J[_g]
AB_COLS = _o
NEGM = -30000.0


def host_a_tables(core):
    slopes = (2.0 ** (-8.0 * (np.arange(12, dtype=np.float64) + 1.0) / 12)).reshape(3, 4)
    tab = np.empty((128, AB_COLS), np.float32)
    p = np.arange(128)[:, None]
    for g in range(3):
        d = A_DIL[g]
        jx = np.arange(A_J[g])[None, :]
        delta = (jx - 384) - p
        valid = (delta >= 0) & (delta <= 128 * d) & (delta % d == 0)
        for h in range(4):
            o = A_JOFF[g * 4 + h]
            tab[:, o:o + A_J[g]] = np.where(valid, -slopes[g, h] * delta, NEGM)
    wrel = np.arange(24)[None, :] * 128 + p
    kval = np.where(wrel + 1024 * core - 2048 >= 0, 0.0, NEGM).astype(np.float32)
    return tab, kval


def phase2(c, io, S):
    top = c.stack
    scale = float(128 ** -0.5)
    with contextlib.ExitStack() as ph:
        c.stack = ph
        ones = c.sb([128, 128], BF16, "p2_ones")
        c.memset(ones[:], 1.0)
        KV = c.sb([128, 24], F32, "p2_kv")
        c.dma(KV[:], io["a_kvalid"][:])
        KT = Rot([c.sb([128, 20 * 128], BF16, f"p2_kt{i}") for i in range(2)])
        VT = Rot([c.sb([128, 20, 128], BF16, f"p2_vt{i}") for i in range(2)])
        BT = Rot([c.sb([128, A_J[2]], F32, f"p2_bt{i}") for i in range(2)])
        QT = Rot([c.sb([128, 512], BF16, f"p2_qt{i}") for i in range(2)])
        PSS = Rot([c.ps([128, 512], F32, f"p2_ps{i}") for i in range(3)])
        PSN = Rot([c.ps([128, 512], F32, f"p2_pn{i}") for i in range(2)])
        PSD = Rot([c.ps([128, 512], F32, f"p2_pd{i}") for i in range(2)])
        SBF = Rot([c.sb([128, 512], F32, f"p2_sb{i}") for i in range(3)])
        PT = Rot([c.sb([128, 512], BF16, f"p2_pt{i}") for i in range(3)])
        RD = Rot([c.sb([128, 512], F32, f"p2_rd{i}") for i in range(2)])
        YT = Rot([c.sb([128, 512], BF16, f"p2_yt{i}") for i in range(2)])
        for quad in range(2):
            i0 = quad * 4
            for h in range(4):
                Np = PSN.next()
                Dp = PSD.next()
                st_ = [True]
                pend = []
                for g in range(3):
                    d = A_DIL[g]
                    kb_lo = 16 + i0 - d
                    kb_hi = 16 + i0 + 3
                    nb = kb_hi - kb_lo + 1
                    kt = KT.next()
                    c.dma(kt[:, 0:nb * 128], S["AKT"][g, h, :, kb_lo * 128:(kb_hi + 1) * 128])
                    vt = VT.next()
                    c.dma(vt[:, 0:nb, :],
                          V(S["AV"], S["AV"].ap[g, kb_lo * 128:(kb_hi + 1) * 128, h * 128:(h + 1) * 128]
                            .rearrange("(b p) d -> p b d", p=128)))
                    bt = BT.next()
                    o = A_JOFF[g * 4 + h]
                    c.dma(bt[:, 0:A_J[g]], io["a_bias"][:, o:o + A_J[g]])
                    qt = QT.next()
                    c.dma(qt[:], S["AQT"][g, h, :, i0 * 128:(i0 + 4) * 128])
                    def a_s(kb, kt=kt, qt=qt, kb_lo=kb_lo):
                        sp = PSS.next()
                        c.mm(sp[:], kt[:, (kb - kb_lo) * 128:(kb - kb_lo + 1) * 128], qt[:])
                        return (sp,)

                    def a_rest(kb, sp, g=g, bt=bt, vt=vt, kb_lo=kb_lo, kb_hi=kb_hi):
                        sb = SBF.next()
                        jx0 = 128 * (16 + i0 - kb) + 384
                        c.stt(sb[:], sp[:], scale, bt[:, jx0:jx0 + 512], ALU.mult, ALU.add)
                        pt = PT.next()
                        c.act(pt[:], sb[:], AF.Exp, bias=KV[:, kb:kb + 1])
                        last = (g == 2 and kb == kb_hi)
                        c.mm(Np[:], vt[:, kb - kb_lo, :], pt[:], start=st_[0], stop=last)
                        c.mm(Dp[:], ones[:], pt[:], start=st_[0], stop=last)
                        st_[0] = False

                    for kb in range(kb_lo, kb_hi + 1):
                        pend.append((a_rest, (kb,) + a_s(kb)))
                        if len(pend) > 1:
                            f_, a_ = pend.pop(0)
                            f_(*a_)
                while pend:
                    f_, a_ = pend.pop(0)
                    f_(*a_)
                rd = RD.next()
                c.recip(rd[:], Dp[:])
                yt = YT.next()
                c.tt(yt[:], Np[:], rd[:], ALU.mult)
                c.dma(S["YAT"][h, :, i0 * 128:(i0 + 4) * 128], yt[:])
        c.barrier()
    c.stack = top

import ml_dtypes
NPBF = ml_dtypes.bfloat16


def _split3(v):
    v = np.asarray(v, np.float64)
    a = v.astype(NPBF).astype(np.float64)
    b = (v - a).astype(NPBF).astype(np.float64)
    cc = (v - a - b).astype(NPBF).astype(np.float64)
    return a, b, cc


def host_b_static():
    T_ = {}
    slopes = 2.0 ** (-8.0 * (np.arange(16, dtype=np.float64) + 1.0) / 16)
    w = np.arange(8192)
    ka = np.zeros((8, 8192), np.float64)
    ka[0:3] = (w % 128)[None]
    ka[3:6] = (w - w % 128)[None]
    ka[6:8] = 1.0
    T_["kaug_sel"] = ka.astype(NPBF)
    cidx = np.arange(512)
    tau = 16 * cidx + 31
    kc = np.zeros((8, 512), np.float64)
    kc[0:3] = (tau % 128)[None]
    kc[3:6] = (tau - tau % 128)[None]
    kc[6:8] = 1.0
    T_["kaug_cmp"] = kc.astype(NPBF)
    qa = np.zeros((2, 8, 8, 8, 128), np.float64)
    for g in range(2):
        for h in range(8):
            s = slopes[g * 8 + h]
            s1, s2, s3 = _split3(s)
            for i in range(8):
                tq = 7168 + 128 * i + np.arange(128)
                cq = -s * tq
                c1 = cq.astype(NPBF).astype(np.float64)
                c2 = (cq - c1).astype(NPBF).astype(np.float64)
                qa[g, i, 0, h] = s1; qa[g, i, 1, h] = s2; qa[g, i, 2, h] = s3
                qa[g, i, 3, h] = s1; qa[g, i, 4, h] = s2; qa[g, i, 5, h] = s3
                qa[g, i, 6, h] = c1; qa[g, i, 7, h] = c2
    T_["qaug"] = qa.reshape(2, 8, 8, 1024).astype(NPBF)
    jj = np.arange(128)[:, None]
    T_["EE"] = (jj == (w // 64)[None, :]).astype(NPBF)
    kl = np.arange(128)[:, None]
    ql = np.arange(128)[None, :]
    tri = np.where(kl > ql, NEGM, 0.0)
    tri2 = np.where(kl <= ql, NEGM, 0.0)
    T_["TRI"] = np.tile(tri, (1, 8)).astype(NPBF)
    T_["TRI2"] = np.tile(tri2, (1, 8)).astype(NPBF)
    cm = np.zeros((8, 128, 128))
    for i in range(8):
        cm[i] = np.where(16 * (384 + kl) + 31 <= 7168 + 128 * i + ql, 0.0, NEGM)
    T_["CM"] = np.tile(cm, (1, 1, 8)).astype(NPBF)
    cs = 16 * cidx[:, None]
    ss = 64 * np.arange(128)[None, :]
    ov = np.clip(np.minimum(cs + 32, ss + 64) - np.maximum(cs, ss), 0, None) / 32.0
    ovx = np.zeros((512, 129))
    ovx[:, :128] = ov
    ovx[:, 128] = 1.0
    T_["OV"] = ovx.reshape(4, 128, 129).astype(NPBF)
    T_["ident"] = np.eye(128, dtype=np.float32)
    return T_


def host_b_core(core):
    T_ = {}
    p = np.arange(128)[:, None]
    c_ = 128 * np.arange(4)[None, :] + p
    T_["cval"] = np.where((16 * c_ >= 7168 - 1024 * core) & (c_ < 511), 0.0, NEGM).astype(np.float32)
    ww = 6656 + 128 * np.arange(12)[None, :] + p
    T_["wval"] = np.where(ww >= 7168 - 1024 * core, 0.0, NEGM).astype(np.float32)
    j0 = 112 - 16 * core
    jj = np.arange(128)[None, :]
    mul = np.zeros((8, 128, 128), np.float32)
    add = np.zeros((8, 128, 128), np.float32)
    for i in range(8):
        cur = (7168 + 128 * i + np.arange(128)[:, None]) // 64
        forced = (jj == cur) | (jj == cur - 1) | (jj == j0)
        ok = (jj <= cur) & (jj >= j0)
        forced = forced & (jj >= j0)
        mul[i] = np.where(ok & ~forced, 1.0, 0.0)
        add[i] = np.where(forced, 1e4, np.where(ok, 0.0, -1e30))
    T_["smul"] = mul
    T_["sadd"] = add
    return T_


NSA_PRUNE = 24


def bcast_ap(ap, dims):
    return bass.AP(tensor=ap.tensor, offset=ap.offset, ap=[list(ap.ap[0])] + [list(d) for d in dims])


def gelu_tanh(c, out, xin, t1, t2):
    c.tt(t1, xin, xin, ALU.mult)
    c.ts(t1, t1, 0.044715, 1.0, ALU.mult, ALU.add)
    c.tt(t1, t1, xin, ALU.mult)
    c.act(t2, t1, AF.Sigmoid, scale=1.5957691216057308)
    c.tt(out, xin, t2, ALU.mult)


def phase3(c, io, S, after_loads=None):
    top = c.stack
    with contextlib.ExitStack() as ph:
        c.stack = ph
        ident = c.sb([128, 128], F32, "p3_id")
        c.dma(ident[:], io["ident"][:])
        KS = [c.sb([72, NW], BF16, f"p3_ks{g}") for g in range(2)]
        KW = [c.sb([72, 1536], BF16, f"p3_kw{g}") for g in range(2)]
        for g in range(2):
            c.dma(KS[g][0:64, :], S["KST"][g, :, :])
            c.dma(KS[g][64:72, :], io["kaug_sel"][:, :])
            c.dma(KW[g][0:64, :], S["KWT"][g, :, :])
            c.dma(KW[g][64:72, :], io["kaug_sel"][:, 6656:8192])
        VSA = c.sb([128, 64, 2, 65], BF16, "p3_vs")
        c.memset(VSA[:, :, :, 64:65], 1.0, e="pool")
        for g in range(2):
            c.dma(VSA[:, :, g, 0:64], V(S["VS"], S["VS"].ap[:, g * 64:(g + 1) * 64].rearrange("(m p) d -> p m d", p=128)))
        VWA = c.sb([128, 12, 2, 65], BF16, "p3_vw")
        c.memset(VWA[:, :, :, 64:65], 1.0, e="pool")
        for g in range(2):
            c.dma(VWA[:, :, g, 0:64], V(S["VW"], S["VW"].ap[:, g * 64:(g + 1) * 64].rearrange("(m p) d -> p m d", p=128)))
        EE = c.sb([128, NW], BF16, "p3_ee")
        c.dma(EE[:], io["EE"][:])
        TRI = c.sb([128, 1024], BF16, "p3_tri")
        c.dma(TRI[:], io["TRI"][:])
        TRI2 = c.sb([128, 1024], BF16, "p3_tri2")
        c.dma(TRI2[:], io["TRI2"][:])
        IDB = c.sb([128, 128], BF16, "p3_idb")
        c.copy(IDB[:], ident[:])
        OV = c.sb([128, 4, 129], BF16, "p3_ov")
        c.dma(OV[:], V(io["OV"], io["OV"].ap.rearrange("t p n -> p t n")))
        CVAL = c.sb([128, 4], F32, "p3_cval")
        c.dma(CVAL[:], io["cval"][:])
        WVAL = c.sb([128, 12], F32, "p3_wval")
        c.dma(WVAL[:], io["wval"][:])
        KCA = [c.sb([72, 512], BF16, f"p3_kca{g}") for g in range(2)]
        VCA = c.sb([128, 4, 2, 65], BF16, "p3_vca")
        c.memset(VCA[:, :, :, 64:65], 1.0, e="pool")
        PSS = Rot([c.ps([128, 1024], F32, f"p3_pss{i}") for i in range(2)])
        PSO = c.ps([128, 1024], F32, "p3_pso")
        PSM = Rot([c.ps([128, 512], F32, f"p3_psm{i}") for i in range(2)])

        with contextlib.ExitStack() as p1:
            c.stack = p1
            KC = c.sb([128, NW], BF16, "p3_kc")
            W1F = c.sb([128, 32, 128], F32, "p3_w1f")
            W1 = c.sb([128, 32, 128], BF16, "p3_w1")
            W2F = c.sb([128, 64], F32, "p3_w2f")
            W2 = c.sb([128, 64], BF16, "p3_w2")
            PEF = c.sb([128, 32], F32, "p3_pef")
            PEB = c.sb([128, 32], BF16, "p3_peb")
            HB = c.sb([128, 1], F32, "p3_hb")
            HX = c.sb([128, 512], F32, "p3_hx")
            H1 = c.sb([128, 512], F32, "p3_h1")
            H2 = c.sb([128, 512], F32, "p3_h2")
            HT = c.sb([128, 512], BF16, "p3_ht")
            for kv in range(2):
                src = S["KCT"] if kv == 0 else S["VCT"]
                c.dma(KC[:], src[:, :])
                w1 = io["w_cmp_k1"] if kv == 0 else io["w_cmp_v1"]
                w2 = io["w_cmp_k2"] if kv == 0 else io["w_cmp_v2"]
                pe = io["pe_k_T"] if kv == 0 else io["pe_v_T"]
                w1v = w1.ap.rearrange("(p d) h -> d p h", d=64)
                for half in range(2):
                    c.dma(W1F[half * 64:(half + 1) * 64, :, :], V(w1, w1v))
                    c.dma(PEF[half * 64:(half + 1) * 64, :], pe[:, :])
                c.copy(W1[:], W1F[:], e="act")
                c.copy(PEB[:], PEF[:])
                c.dma(W2F[:], w2[:, :])
                c.copy(W2[:], W2F[:])
                bp = PSM.next()
                for p_ in range(32):
                    c.mm(bp[:, 0:1], W1[0:64, p_, :], PEB[0:64, p_:p_ + 1], start=(p_ == 0), stop=(p_ == 31))
                c.copy(HB[:], bp[:, 0:1])
                for g in range(2):
                    hp = PSM.next()
                    for p_ in range(32):
                        c.mm(hp[:, 0:511], W1[g * 64:(g + 1) * 64, p_, :],
                             KC[g * 64:(g + 1) * 64, p_:p_ + 8161:16], start=(p_ == 0), stop=(p_ == 31))
                    c.act(HX[:, 0:511], hp[:, 0:511], AF.Identity, bias=HB[:, 0:1])
                    c.memset(HX[:, 511:512], 0.0)
                    gelu_tanh(c, HT[:], HX[:], H1[:], H2[:])
                    if kv == 0:
                        op_ = PSM.next()
                        c.mm(op_[0:64, :], W2[:], HT[:])
                        c.copy(KCA[g][0:64, :], op_[0:64, :])
                        c.dma(KCA[g][64:72, :], io["kaug_cmp"][:, :])
                    else:
                        for ct in range(4):
                            op_ = PSM.next()
                            c.mm(op_[:, 0:64], HT[:, ct * 128:(ct + 1) * 128], W2[:])
                            c.copy(VCA[:, ct, g, 0:64], op_[:, 0:64])
        c.stack = ph
        c.barrier()

        QAall = [[c.sb([72, 1024], BF16, f"p3_qa{i}_{g}") for g in range(2)] for i in range(8)]
        PT = Rot([c.sb([128, 1024], BF16, f"p3_pt{i}") for i in range(3)])
        PC = [c.sb([128, 1024], BF16, f"p3_pc{i}") for i in range(4)]
        CMa = c.sb([128, 8, 1024], BF16, "p3_cma")
        SMULa = c.sb([128, 8, 128], F32, "p3_smula")
        SADDa = c.sb([128, 8, 128], F32, "p3_sadda")
        IMP = c.sb([128, 128], F32, "p3_imp")
        SCO = c.sb([128, 128], F32, "p3_sco")
        SWK = c.sb([128, 128], F32, "p3_swk")
        M8 = c.sb([128, 16], F32, "p3_m8")
        RDN = c.sb([128, 8], F32, "p3_rdn")
        SEL = c.sb([128, 128], F32, "p3_sel")
        SELV = c.sb([128, 128], F32, "p3_selv")
        SLT = c.sb([128, 128], BF16, "p3_slt")
        PTH = Rot([c.sb([128, 512], BF16, f"p3_pth{i}") for i in range(4)])
        PMH = Rot([c.sb([128, 512], BF16, f"p3_pmh{i}") for i in range(4)])
        HROT = Rot([T(f"p3_hv{i}", PSS.tiles[i // 2].ap[:, (i % 2) * 512:(i % 2 + 1) * 512]) for i in range(4)])
        OS = c.sb([65, 1024], F32, "p3_os")
        BGa = c.sb([128, 8, 48], F32, "p3_bga")
        BGh = [None]
        YB = c.sb([128, 512], F32, "p3_yb")
        COEF = c.sb([128, 8], F32, "p3_coef")
        YBT = Rot([c.sb([128, 128], BF16, f"p3_ybt{i}") for i in range(2)])

        TMP4 = c.sb([128, 256], F32, "p3_tmp4")

        def epilogue(b, g, first):
            c.copy(OS[:, :], PSO[0:65, :], e="act")
            for h0 in (0, 4):
                tp = PSM.next()
                for hh in range(4):
                    h = h0 + hh
                    c.transpose(tp[:, hh * 65:(hh + 1) * 65], OS[0:65, h * 128:(h + 1) * 128], ident[0:65, 0:65])
                cf = V(COEF, COEF.ap[:, h0:h0 + 4])
                den = V(tp, bcast_ap(tp.ap[:, 64:65], [[65, 4]]))
                c.ts(cf, den, 1e-30, None, ALU.max)
                c.recip(cf, cf)
                col0 = 3 * (g * 8 + h0) + b
                c.tt(cf, cf, V(BGa, bcast_ap(BGa.ap[:, BGh[0], col0:col0 + 1], [[3, 4]])), ALU.mult)
                ov = V(tp, bcast_ap(tp.ap[:, 0:1], [[65, 4], [1, 64]]))
                cb = V(COEF, bcast_ap(COEF.ap[:, h0:h0 + 1], [[1, 4], [0, 64]]))
                ybv = V(YB, YB.ap[:, h0 * 64:(h0 + 4) * 64].rearrange("p (h d) -> p h d", d=64))
                if first:
                    c.tt(ybv, ov, cb, ALU.mult)
                else:
                    tv = V(TMP4, TMP4.ap.rearrange("p (h d) -> p h d", d=64))
                    c.tt(tv, ov, cb, ALU.mult)
                    c.tt(ybv, ybv, tv, ALU.add)

        c.dma(BGa[:], V(S["BG"], S["BG"].ap.rearrange("(i p) n -> p i n", p=128)))
        c.dma(SMULa[:], V(io["smul"], io["smul"].ap.rearrange("i p n -> p i n")))
        c.dma(SADDa[:], V(io["sadd"], io["sadd"].ap.rearrange("i p n -> p i n")))
        c.dma(CMa[:], V(io["CM"], io["CM"].ap.rearrange("i p n -> p i n")))
        for i in range(8):
            for g in range(2):
                c.dma(QAall[i][g][0:64, :], V(S["BQT"], S["BQT"].ap[g, :, i, :, :].rearrange("p h q -> p (h q)")))
                c.dma(QAall[i][g][64:72, :], io["qaug"][g, i, :, :])
        c.barrier()
        if after_loads is not None:
            after_loads()
        for i in range(8):
            BGh[0] = i
            SMUL = V(SMULa, SMULa.ap[:, i, :])
            SADD = V(SADDa, SADDa.ap[:, i, :])
            cm = V(CMa, CMa.ap[:, i, :])
            for g in range(2):
                qa = QAall[i][g]
                for ct in range(4):
                    sp = PSS.next()
                    for hf in range(2):
                        cs = slice(hf * 512, (hf + 1) * 512)
                        c.mm(sp[:, cs], KCA[g][0:72, ct * 128:(ct + 1) * 128], qa[0:72, cs],
                             start=True, stop=(ct != 3))
                        if ct == 3:
                            c.mm(sp[:, cs], IDB[:], cm[:, cs], start=False, stop=True)
                    c.act(PC[ct][:], sp[:], AF.Exp, bias=CVAL[:, ct:ct + 1])
                for hf in range(2):
                    cs = slice(hf * 512, (hf + 1) * 512)
                    for ct in range(4):
                        c.mm(PSO[0:65, cs], VCA[:, ct, g, :], PC[ct][:, cs], start=(ct == 0), stop=(ct == 3))
                for h in range(8):
                    ip = PSM.next()
                    for ct in range(4):
                        c.mm(ip[:, 0:129], PC[ct][:, h * 128:(h + 1) * 128], OV[:, ct, :], start=(ct == 0), stop=(ct == 3))
                    c.ts(RDN[:, h:h + 1], ip[:, 128:129], 1e-30, None, ALU.max)
                    c.recip(RDN[:, h:h + 1], RDN[:, h:h + 1])
                    if h == 0:
                        c.ts(IMP[:], ip[:, 0:128], RDN[:, h:h + 1], None, ALU.mult)
                    else:
                        c.stt(IMP[:], ip[:, 0:128], RDN[:, h:h + 1], IMP[:], ALU.mult, ALU.add)
                epilogue(0, g, True)
                c.tt(SCO[:], IMP[:], SMUL[:], ALU.mult)
                c.tt(SCO[:], SCO[:], SADD[:], ALU.add)
                c.op("dve", lambda: c.nc.vector.max(out=M8.ap[:, 0:8], in_=SCO.ap[:]), reads=[SCO], writes=[M8])
                c.op("dve", lambda: c.nc.vector.match_replace(out=SWK.ap[:], in_to_replace=M8.ap[:, 0:8],
                                                              in_values=SCO.ap[:], imm_value=-3.0e38),
                     reads=[SCO, M8], writes=[SWK])
                c.op("dve", lambda: c.nc.vector.max(out=M8.ap[:, 8:16], in_=SWK.ap[:]), reads=[SWK], writes=[M8])
                c.ts(SEL[:], SCO[:], M8[:, 15:16], None, ALU.is_ge)
                c.ts(SELV[:], SCO[:], -1.0e29, None, ALU.is_gt)
                c.tt(SEL[:], SEL[:], SELV[:], ALU.mult)
                def win_s(mw):
                    sp = PSS.next()
                    for hf in range(2):
                        cs = slice(hf * 512, (hf + 1) * 512)
                        edge = (mw == i) or (mw == i + 4)
                        c.mm(sp[:, cs], KW[g][0:72, mw * 128:(mw + 1) * 128], qa[0:72, cs], start=True, stop=(not edge))
                        if mw == i:
                            c.mm(sp[:, cs], IDB[:], TRI2[:, cs], start=False, stop=True)
                        if mw == i + 4:
                            c.mm(sp[:, cs], IDB[:], TRI[:, cs], start=False, stop=True)
                    return (sp,)

                def win_rest(mw, sp):
                    pt = PT.next()
                    c.act(pt[:], sp[:], AF.Exp, bias=WVAL[:, mw:mw + 1])
                    for hf in range(2):
                        cs = slice(hf * 512, (hf + 1) * 512)
                        c.mm(PSO[0:65, cs], VWA[:, mw, g, :], pt[:, cs], start=(mw == i), stop=(mw == i + 4))

                pend = []
                for mw in range(i, i + 5):
                    pend.append((mw,) + win_s(mw))
                    if len(pend) > 1:
                        win_rest(*pend.pop(0))
                while pend:
                    win_rest(*pend.pop(0))
                epilogue(2, g, False)
                tp = PSM.next()
                c.transpose(tp[:, 0:128], SEL[:], ident[:])
                c.copy(SLT[:], tp[:, 0:128], e="act")
                mlast = 56 + i
                mfirst = max(0, mlast - NSA_PRUNE) if g == 0 else 0
                def sel_s(m, hf):
                    mk = mk_of.get(m)
                    if mk is None:
                        mk = PSM.next()
                        c.mm(mk[:, 0:128], EE[:, m * 128:(m + 1) * 128], SLT[:])
                        mk_of.clear()
                        mk_of[m] = mk
                    sp = HROT.next()
                    cs = slice(hf * 512, (hf + 1) * 512)
                    c.mm(sp[:], KS[g][0:72, m * 128:(m + 1) * 128], qa[0:72, cs], start=True, stop=(m != mlast))
                    if m == mlast:
                        c.mm(sp[:], IDB[:], TRI[:, cs], start=False, stop=True)
                    return sp, mk

                def sel_rest(m, hf, sp, mk):
                    cs = slice(hf * 512, (hf + 1) * 512)
                    pt = PTH.next()
                    c.act(pt[:], sp[:], AF.Exp)
                    pm = PMH.next()
                    c.tt(V(pm, pm.ap.rearrange("p (h q) -> p h q", q=128)),
                         V(pt, pt.ap.rearrange("p (h q) -> p h q", q=128)),
                         V(mk, bcast_ap(mk.ap[:, 0:128], [[0, 4], [1, 128]])), ALU.mult)
                    c.mm(PSO[0:65, cs], VSA[:, m, g, :], pm[:], start=(m == mfirst), stop=(m == mlast))

                mk_of = {}
                pend = []
                for m in range(mfirst, mlast + 1):
                    for hf in range(2):
                        pend.append((m, hf) + sel_s(m, hf))
                        if len(pend) > 2:
                            sel_rest(*pend.pop(0))
                while pend:
                    sel_rest(*pend.pop(0))
                epilogue(1, g, False)
                for j in range(4):
                    tp = PSM.next()
                    c.transpose(tp[:, 0:128], YB[:, j * 128:(j + 1) * 128], ident[:])
                    yt = YBT.next()
                    c.copy(yt[:], tp[:, 0:128], e="act")
                    c.dma(S["YBT"][g * 4 + j, :, i * 128:(i + 1) * 128], yt[:])
        c.barrier()
    c.stack = top

def load_w_bf16(c, dst, src_t, src_ap, stg_rot, nk, ncols):
    v = src_ap.rearrange("(k p) n -> p k n", p=128)
    for k in range(nk):
        st = stg_rot.next()
        c.dma(st[:, 0:ncols], V(src_t, v[:, k, :]))
        c.copy(dst[:, k, 0:ncols], st[:, 0:ncols], e=("act", "dve", "pool")[k % 3])


def rms_rows(c, r_out, xin, sq_scratch):
    c.act(sq_scratch, xin, AF.Square, accum=r_out)
    c.ts(r_out, r_out, 1.0 / D, EPS, ALU.mult, ALU.add)
    c.act(r_out, r_out, AF.Sqrt)
    c.recip(r_out, r_out)


def phase4(c, io, S):
    top = c.stack
    with contextlib.ExitStack() as ph:
        c.stack = ph
        ident = c.sb([128, 128], F32, "p4_id")
        c.dma(ident[:], io["ident"][:])
        IDB = c.sb([128, 128], BF16, "p4_idb")
        c.copy(IDB[:], ident[:])
        ones = c.sb([128, 128], BF16, "p4_ones")
        c.memset(ones[:], 1.0)
        STGW = Rot([c.sb([128, 2048], F32, f"p4_stg{i}") for i in range(2)])
        PS = Rot([c.ps([128, 512], F32, f"p4_ps{i}") for i in range(6)])
        PSB = Rot([c.ps([128, 512], BF16, f"p4_psb{i}") for i in range(2)])
        pmix = contextlib.ExitStack()
        c.stack = pmix
        MIXT = c.sb([128, 16, NOWN], BF16, "p4_mixt")
        c.stack = ph
        with contextlib.ExitStack() as pa:
            c.stack = pa
            WUA = c.sb([128, 4, 2048], BF16, "p4_wua")
            WUB = c.sb([128, 8, 2048], BF16, "p4_wub")
            load_w_bf16(c, WUA, io["w_up_a"], io["w_up_a"].ap, STGW, 4, 2048)
            load_w_bf16(c, WUB, io["w_up_b"], io["w_up_b"].ap, STGW, 8, 2048)
            YA = c.sb([128, 4, NOWN], BF16, "p4_ya")
            YBt = c.sb([128, 8, NOWN], BF16, "p4_yb")
            c.dma(YA[:], V(S["YAT"], S["YAT"].ap.rearrange("k p t -> p k t")))
            c.dma(YBt[:], V(S["YBT"], S["YBT"].ap.rearrange("k p t -> p k t")))
            MGA = Rot([c.sb([128, NOWN], BF16, f"p4_mga{i}") for i in range(2)])
            MGB = Rot([c.sb([128, NOWN], BF16, f"p4_mgb{i}") for i in range(2)])
            T1 = Rot([c.sb([128, 512], F32, f"p4_t1{i}") for i in range(2)])
            T2 = Rot([c.sb([128, 512], F32, f"p4_t2{i}") for i in range(2)])
            for j in range(16):
                ga = MGA.next(); gb = MGB.next()
                c.dma(ga[:], S["MG"][j, :, :])
                c.dma(gb[:], S["MG"][16 + j, :, :])
                for hf in range(2):
                    cs = slice(hf * 512, (hf + 1) * 512)
                    pa_ = PS.next()
                    for k in range(4):
                        c.mm(pa_[:], WUA[:, k, j * 128:(j + 1) * 128], YA[:, k, cs], start=(k == 0), stop=(k == 3))
                    pb_ = PS.next()
                    for k in range(8):
                        c.mm(pb_[:], WUB[:, k, j * 128:(j + 1) * 128], YBt[:, k, cs], start=(k == 0), stop=(k == 7))
                    t1 = T1.next(); t2 = T2.next()
                    c.tt(t1[:], pa_[:], ga[:, cs], ALU.mult)
                    c.tt(t2[:], pb_[:], gb[:, cs], ALU.mult)
                    c.tt(MIXT[:, j, cs], t1[:], t2[:], ALU.add, e="pool")
        c.stack = ph
        c.barrier()
        with contextlib.ExitStack() as pb:
            c.stack = pb
            WO = c.sb([128, 16, 2048], BF16, "p4_wo")
            load_w_bf16(c, WO, io["w_out"], io["w_out"].ap, STGW, 16, 2048)
            XO = Rot([c.sb([128, 2048], F32, f"p4_xo{i}") for i in range(2)])
            for tt in range(8):
                xo = XO.next()
                c.dma(xo[:], io["x_own"][tt * 128:(tt + 1) * 128, :])
                for cn in range(4):
                    ps = PS.next()
                    for j in range(16):
                        c.mm(ps[:], MIXT[:, j, tt * 128:(tt + 1) * 128], WO[:, j, cn * 512:(cn + 1) * 512],
                             start=(j == 0), stop=(j == 15))
                    c.tt(xo[:, cn * 512:(cn + 1) * 512], xo[:, cn * 512:(cn + 1) * 512], ps[:], ALU.add)
                c.dma(S["H"][tt * 128:(tt + 1) * 128, :], xo[:])
        c.stack = ph
        c.barrier()
        pmix.close()
        with contextlib.ExitStack() as pc:
            c.stack = pc
            gx = c.sb([128, KC], F32, "p4_gx")
            gm = c.sb([128, KC], F32, "p4_gm")
            c.dma(gx[:], io["g_x"][:])
            c.dma(gm[:], io["g_mem"][:])
            WQ = c.sb([128, 16, 512], BF16, "p4_wq")
            WKV = c.sb([128, 16, 1024], BF16, "p4_wkv")
            WXO = c.sb([128, 4, 2048], BF16, "p4_wxo")
            load_w_bf16(c, WQ, io["w_xq"], io["w_xq"].ap, STGW, 16, 512)
            load_w_bf16(c, WKV, io["w_xkv"], io["w_xkv"].ap, STGW, 16, 1024)
            load_w_bf16(c, WXO, io["w_xo"], io["w_xo"].ap, STGW, 4, 2048)
            HT_ = Rot([c.sb([128, 2048], F32, f"p4_h{i}") for i in range(2)])
            SQ = c.sb([128, 2048], BF16, "p4_sq")
            HB = Rot([c.sb([128, 2048], BF16, f"p4_hb{i}") for i in range(2)])
            RR = Rot([c.sb([128, 1], F32, f"p4_rr{i}") for i in range(4)])
            MNT = c.sb([128, 16, 256], BF16, "p4_mnt")
            KT = c.sb([128, 4, 256], BF16, "p4_kt")
            VM = c.sb([128, 2, 512], BF16, "p4_vm")

            def norm_T(src_tile, dst, tok0, gvec):
                rr = RR.next()
                rms_rows(c, rr[:], src_tile[:], SQ[:])
                hb = HB.next()
                c.act(hb[:], src_tile[:], AF.Copy, scale=rr[:, 0:1])
                for k4 in range(4):
                    tp = PSB.next()
                    for kk in range(4):
                        k = k4 * 4 + kk
                        c.transpose(tp[:, kk * 128:(kk + 1) * 128], hb[:, k * 128:(k + 1) * 128], IDB[:])
                    for kk in range(4):
                        k = k4 * 4 + kk
                        c.ts(dst[:, k, tok0:tok0 + 128], tp[:, kk * 128:(kk + 1) * 128], gvec[:, k:k + 1], None, ALU.mult)

            for mt in range(2):
                m_ = HT_.next()
                c.dma(m_[:], io["mem"][mt * 128:(mt + 1) * 128, :])
                norm_T(m_, MNT, mt * 128, gm)
            for h in range(4):
                ps = PS.next()
                for k in range(16):
                    c.mm(ps[:, 0:256], WKV[:, k, h * 128:(h + 1) * 128], MNT[:, k, :], start=(k == 0), stop=(k == 15))
                c.copy(KT[:, h, :], ps[:, 0:256])
            for mt in range(2):
                ps = PS.next()
                for k in range(16):
                    c.mm(ps[:], MNT[:, k, mt * 128:(mt + 1) * 128], WKV[:, k, 512:1024], start=(k == 0), stop=(k == 15))
                c.copy(VM[:, mt, :], ps[:])
            HNT = Rot([c.sb([128, 16, 512], BF16, f"p4_hnt{i}") for i in range(1)])
            HQ = [c.sb([128, 2048], F32, f"p4_hq{i}") for i in range(4)]
            QT = Rot([c.sb([128, 512], BF16, f"p4_qt{i}") for i in range(2)])
            PTm = Rot([c.sb([128, 512], BF16, f"p4_pt{i}") for i in range(4)])
            OT = c.sb([128, 4, 512], BF16, "p4_ot")
            RD = Rot([c.sb([128, 512], F32, f"p4_rd{i}") for i in range(2)])
            xs = float(128 ** -0.5)
            for quad in range(2):
                hnt = HNT.next()
                for t4 in range(4):
                    tt = quad * 4 + t4
                    c.dma(HQ[t4][:], S["H"][tt * 128:(tt + 1) * 128, :])
                    norm_T(HQ[t4], hnt, t4 * 128, gx)
                for h in range(4):
                    ps = PS.next()
                    for k in range(16):
                        c.mm(ps[:], WQ[:, k, h * 128:(h + 1) * 128], hnt[:, k, :], start=(k == 0), stop=(k == 15))
                    qt = QT.next()
                    c.copy(qt[:], ps[:])
                    pts = []
                    for mt in range(2):
                        sp = PS.next()
                        c.mm(sp[:], KT[:, h, mt * 128:(mt + 1) * 128], qt[:])
                        pt = PTm.next()
                        c.act(pt[:], sp[:], AF.Exp, scale=xs)
                        pts.append(pt)
                    op_ = PS.next(); dp_ = PS.next()
                    for mt in range(2):
                        c.mm(op_[:], VM[:, mt, h * 128:(h + 1) * 128], pts[mt][:], start=(mt == 0), stop=(mt == 1))
                    for mt in range(2):
                        c.mm(dp_[:], ones[:], pts[mt][:], start=(mt == 0), stop=(mt == 1))
                    rd = RD.next()
                    c.recip(rd[:], dp_[:])
                    c.tt(OT[:, h, :], op_[:], rd[:], ALU.mult)
                for t4 in range(4):
                    tt = quad * 4 + t4
                    for cn in range(4):
                        ps = PS.next()
                        for h in range(4):
                            c.mm(ps[:], OT[:, h, t4 * 128:(t4 + 1) * 128], WXO[:, h, cn * 512:(cn + 1) * 512],
                                 start=(h == 0), stop=(h == 3))
                        c.tt(HQ[t4][:, cn * 512:(cn + 1) * 512], HQ[t4][:, cn * 512:(cn + 1) * 512], ps[:], ALU.add)
                    c.dma(S["H2"][tt * 128:(tt + 1) * 128, :], HQ[t4][:])
        c.barrier()
    c.stack = top

def bcast_ap(ap, dims):
    return bass.AP(tensor=ap.tensor, offset=ap.offset, ap=[list(ap.ap[0])] + [list(d) for d in dims])


class ExpertConv:
    def __init__(self, c, io, S):
        self.c, self.io, self.S = c, io, S

    def emit(self):
        c, io, S = self.c, self.io, self.S
        for r in range(16):
            for (src, dst) in (("expert_u", "UB"), ("expert_v", "VB")):
                c.dma(S[dst][r * 1024:(r + 1) * 1024, :], io[src][r * 1024:(r + 1) * 1024, :], eng="pool")

    def close(self):
        pass


def phase5(c, io, S, out_t):
    nc = c.nc
    top = c.stack
    with contextlib.ExitStack() as ph:
        c.stack = ph
        ident = c.sb([128, 128], F32, "p5_id")
        c.dma(ident[:], io["ident"][:])
        IDB = c.sb([128, 128], BF16, "p5_idb")
        c.copy(IDB[:], ident[:])
        WPQ = c.sb([128, 16, 2048], BF16, "p5_wpq")
        with contextlib.ExitStack() as pl:
            c.stack = pl
            STGW = Rot([c.sb([128, 2048], F32, f"p5_stg{i}") for i in range(2)])
            load_w_bf16(c, WPQ, io["w_pq"], io["w_pq"].ap, STGW, 16, 2048)
            c.barrier()
        c.stack = ph
        SKF = c.sb([128, 2, 128], F32, "p5_skf")
        SKT = c.sb([128, 2, 128], BF16, "p5_skt")
        c.dma(SKF[:, 0, :], io["sk1_T"][:, :])
        c.dma(SKF[:, 1, :], io["sk2_T"][:, :])
        c.copy(SKT[:], SKF[:])
        GBC = c.sb([128, D], F32, "p5_gbc")
        GFIN = c.sb([128, D], F32, "p5_gfin")
        c.dma(GBC[:], io["g_ffn_bc"][:, :])
        c.dma(GFIN[:], io["g_fin_bc"][:, :])
        HT2 = [c.sb([128, D], F32, f"p5_ht{i}") for i in range(2)]
        XN32 = [c.sb([128, D], F32, f"p5_xn3{i}") for i in range(2)]
        OUTB = c.sb([128, D], F32, "p5_outb")
        XB = c.sb([128, D], BF16, "p5_xb")
        XT3 = c.sb([128, 16, 128], BF16, "p5_xt3")
        QT = c.sb([128, 16, 128], BF16, "p5_qt")
        SC = c.sb([128, 16, 128], F32, "p5_sc")
        WK = c.sb([128, 256], F32, "p5_wk")
        V16 = c.sb([128, 16, 16], F32, "p5_v16")
        I16 = c.sb([128, 16, 16], U32, "p5_i16")
        IF = c.sb([128, 16, 16], F32, "p5_if")
        CA = c.sb([128, 256], F32, "p5_ca")
        EI = c.sb([128, 256], F32, "p5_ei")
        JK2 = c.sb([128, 256], F32, "p5_jk2")
        VS = c.sb([128, 16], F32, "p5_vs")
        NB = c.sb([128, 1], F32, "p5_nb")
        ZS = c.sb([128, 1], F32, "p5_zs")
        EIDS = c.sb([128, 128], F32, "p5_eids")
        EI322 = [c.sb([128, 128], I32, f"p5_ei32{i}") for i in range(2)]
        GW2 = [c.sb([128, 128], F32, f"p5_gw{i}") for i in range(2)]
        AA2 = [c.sb([128, 128], F32, f"p5_aa{i}") for i in range(2)]
        G1 = c.sb([128, 128], F32, "p5_g1")
        G2 = c.sb([128, 128], F32, "p5_g2")
        WT2 = [c.sb([128, 128], F32, f"p5_wt{i}") for i in range(2)]
        RR = c.sb([128, 1], F32, "p5_rr")
        RR2 = c.sb([128, 1], F32, "p5_rr2")
        UG = Rot([c.sb([128, D], BF16, f"p5_ug{i}") for i in range(4)])
        VG = Rot([c.sb([128, D], BF16, f"p5_vg{i}") for i in range(4)])
        TB = Rot([c.sb([128, D], BF16, f"p5_tb{i}") for i in range(2)])
        JK = c.sb([128, D], BF16, "p5_jk")
        JKA = c.sb([128, D], BF16, "p5_jka")
        YP = c.ps([128, D], F32, "p5_yp")
        PS = Rot([c.ps([128, 512], F32, f"p5_ps{i}") for i in range(2)])
        PSB = c.ps([128, 1024], BF16, "p5_psb")

        def top16(src, ncol, mv, iu):
            c.op("dve", lambda: nc.vector.max(out=mv.ap[:, 0:8], in_=src.ap), reads=[src], writes=[mv])
            if iu is not None:
                c.op("dve", lambda: nc.vector.max_index(out=iu.ap[:, 0:8], in_max=mv.ap[:, 0:8], in_values=src.ap),
                     reads=[src, mv], writes=[iu])
            c.op("dve", lambda: nc.vector.match_replace(out=WK.ap[:, 0:ncol], in_to_replace=mv.ap[:, 0:8],
                                                        in_values=src.ap, imm_value=-3.0e38),
                 reads=[src, mv], writes=[WK])
            c.op("dve", lambda: nc.vector.max(out=mv.ap[:, 8:16], in_=WK.ap[:, 0:ncol]), reads=[WK], writes=[mv])
            if iu is not None:
                c.op("dve", lambda: nc.vector.max_index(out=iu.ap[:, 8:16], in_max=mv.ap[:, 8:16],
                                                        in_values=WK.ap[:, 0:ncol]),
                     reads=[WK, mv], writes=[iu])

        def stage_a(tt):
            HT, XN3, EI32, GW = HT2[tt % 2], XN32[tt % 2], EI322[tt % 2], GW2[tt % 2]
            c.dma(HT[:], S["H2"][tt * 128:(tt + 1) * 128, :])
            rms_rows(c, RR[:], HT[:], JKA[:])
            c.stt(XN3[:], HT[:], RR[:, 0:1], GBC[:], ALU.mult, ALU.mult)
            c.copy(XB[:], XN3[:], e="act")
            for k4 in range(4):
                for kk in range(4):
                    k = k4 * 4 + kk
                    c.transpose(PSB[:, kk * 128:(kk + 1) * 128], XB[:, k * 128:(k + 1) * 128], IDB[:])
                c.copy(V(XT3, XT3.ap[:, k4 * 4:(k4 + 1) * 4, :].rearrange("p a b -> p (a b)")), PSB[:, 0:512])
            for c4 in range(4):
                ps = PS.next()
                for cc_ in range(4):
                    cq = c4 * 4 + cc_
                    for k in range(16):
                        c.mm(ps[:, cc_ * 128:(cc_ + 1) * 128], WPQ[:, k, cq * 128:(cq + 1) * 128], XT3[:, k, :],
                             start=(k == 0), stop=(k == 15))
                c.copy(V(QT, QT.ap[:, c4 * 4:(c4 + 1) * 4, :].rearrange("p a b -> p (a b)")), ps[:], e="act")
            for c4 in range(4):
                ps = PS.next()
                for cc_ in range(4):
                    cq = c4 * 4 + cc_
                    c.mm(ps[:, cc_ * 128:(cc_ + 1) * 128], QT[:, cq, :], SKT[:, cq % 2, :])
                c.copy(V(SC, SC.ap[:, c4 * 4:(c4 + 1) * 4, :].rearrange("p a b -> p (a b)")), ps[:], e="act")
            for cq in range(16):
                top16(SC[:, cq, :], 128, V(V16, V16.ap[:, cq, :]), V(I16, I16.ap[:, cq, :]))
            c.copy(IF[:], I16[:])
            c.memset(EIDS[:], 0.0)
            for h in range(8):
                a0 = V(V16, bcast_ap(V16.ap[:, 2 * h, :], [[1, 16], [0, 16]]))
                a1 = V(V16, bcast_ap(V16.ap[:, 2 * h + 1, :], [[0, 16], [1, 16]]))
                c.tt(V(CA, CA.ap.rearrange("p (a b) -> p a b", b=16)), a0, a1, ALU.add)
                e0 = V(IF, bcast_ap(IF.ap[:, 2 * h, :], [[1, 16], [0, 16]]))
                e1 = V(IF, bcast_ap(IF.ap[:, 2 * h + 1, :], [[0, 16], [1, 16]]))
                c.stt(V(EI, EI.ap.rearrange("p (a b) -> p a b", b=16)), e0, 128.0, e1, ALU.mult, ALU.add)
                top16(CA[:], 256, VS[:], None)
                for k in range(16):
                    c.stt(JK2[:], CA[:], VS[:, k:k + 1], EI[:], ALU.is_equal, ALU.mult,
                          accum=EIDS[:, h * 16 + k:h * 16 + k + 1])
                c.ts(NB[:], VS[:, 0:1], -1.0, None, ALU.mult)
                c.memset(ZS[:], 0.0)
                c.act(GW[:, h * 16:(h + 1) * 16], VS[:], AF.Exp, bias=NB[:, 0:1], accum=ZS[:])
                c.recip(ZS[:], ZS[:])
                c.ts(GW[:, h * 16:(h + 1) * 16], GW[:, h * 16:(h + 1) * 16], ZS[:, 0:1], None, ALU.mult)
            c.ts(EIDS[:], EIDS[:], 0.0, float(128 * 128 - 1), ALU.max, ALU.min)
            c.copy(EI32[:], EIDS[:])
            c.memset(AA2[tt % 2][:], 0.0)

        def step_u(tt, j):
            EI32, XN3, AA = EI322[tt % 2], XN32[tt % 2], AA2[tt % 2]
            ug = UG.next()
            c.op("pool", lambda ug=ug, j=j: nc.gpsimd.indirect_dma_start(
                out=ug.ap[:, :], out_offset=None, in_=S["UB"].ap[:, :],
                in_offset=bass.IndirectOffsetOnAxis(ap=EI32.ap[:, j:j + 1], axis=0)),
                reads=[EI32, S["UB"]], writes=[ug], dma=True)
            c.stt(JK[:], ug[:], 1.0, XN3[:], ALU.mult, ALU.mult, accum=AA[:, j:j + 1])

        def stage_g(tt):
            WT = WT2[tt % 2]
            gelu_tanh(c, WT[:], AA2[tt % 2][:], G1[:], G2[:])
            c.tt(WT[:], WT[:], GW2[tt % 2][:], ALU.mult)

        def step_v(tt, j):
            EI32, WT = EI322[tt % 2], WT2[tt % 2]
            vg = VG.next()
            c.op("pool", lambda vg=vg, j=j: nc.gpsimd.indirect_dma_start(
                out=vg.ap[:, :], out_offset=None, in_=S["VB"].ap[:, :],
                in_offset=bass.IndirectOffsetOnAxis(ap=EI32.ap[:, j:j + 1], axis=0)),
                reads=[EI32, S["VB"]], writes=[vg], dma=True)
            tb = TB.next()
            c.act(tb[:], vg[:], AF.Copy, scale=WT[:, j:j + 1])
            for cn in range(4):
                c.mm(YP[:, cn * 512:(cn + 1) * 512], IDB[:], tb[:, cn * 512:(cn + 1) * 512],
                     start=(j == 0), stop=(j == 127))

        def stage_f(tt):
            HT = HT2[tt % 2]
            for cn in range(4):
                c.tt(HT[:, cn * 512:(cn + 1) * 512], HT[:, cn * 512:(cn + 1) * 512], YP[:, cn * 512:(cn + 1) * 512], ALU.add)
            rms_rows(c, RR2[:], HT[:], JKA[:])
            c.stt(OUTB[:], HT[:], RR2[:, 0:1], GFIN[:], ALU.mult, ALU.mult)
            c.dma(out_t[tt * 128:(tt + 1) * 128, :], OUTB[:])

        stage_a(0)
        for j in range(128):
            step_u(0, j)
        stage_g(0)
        for tt in range(8):
            if tt < 7:
                stage_a(tt + 1)
            for j in range(128):
                step_v(tt, j)
                if tt < 7:
                    step_u(tt + 1, j)
            stage_f(tt)
            if tt < 7:
                stage_g(tt + 1)
        c.barrier()
    c.stack = top

def scratch_spec():
    return {
        "XN": ([16, 128, KC, 512], BF16),
        "AQT": ([3, 4, 128, NOWN], BF16),
        "AKT": ([3, 4, 128, 3072], BF16),
        "AV": ([3, 3072, 512], BF16),
        "BQT": ([2, 64, 8, 8, 128], BF16),
        "KCT": ([128, NW], BF16),
        "VCT": ([128, NW], BF16),
        "KST": ([2, 64, NW], BF16),
        "VS": ([NW, 128], BF16),
        "KWT": ([2, 64, 1536], BF16),
        "VW": ([1536, 128], BF16),
        "BG": ([NOWN, 48], F32),
        "MG": ([32, 128, NOWN], BF16),
        "YAT": ([4, 128, NOWN], BF16),
        "YBT": ([8, 128, NOWN], BF16),
        "H": ([NOWN, D], F32),
        "H2": ([NOWN, D], F32),
        "UB": ([16384, D], BF16),
        "VB": ([16384, D], BF16),
    }


def input_spec():
    return {
        "xT": ([16, 128, KC, 512], F32),
        "w_in": ([D, IN_COLS], F32),
        "g_mix": ([128, KC], F32),
        "a_bias": ([128, AB_COLS], F32),
        "a_kvalid": ([128, 24], F32),
        "kaug_sel": ([8, NW], BF16), "kaug_cmp": ([8, 512], BF16), "qaug": ([2, 8, 8, 1024], BF16),
        "EE": ([128, NW], BF16), "TRI": ([128, 1024], BF16), "TRI2": ([128, 1024], BF16),
        "CM": ([8, 128, 1024], BF16), "OV": ([4, 128, 129], BF16), "ident": ([128, 128], F32),
        "cval": ([128, 4], F32), "wval": ([128, 12], F32),
        "smul": ([8, 128, 128], F32), "sadd": ([8, 128, 128], F32),
        "x_own": ([NOWN, D], F32), "mem": ([256, D], F32),
        "w_up_a": ([512, D], F32), "w_up_b": ([1024, D], F32), "w_out": ([D, D], F32),
        "w_xq": ([D, 512], F32), "w_xkv": ([D, 1024], F32), "w_xo": ([512, D], F32),
        "g_x": ([128, KC], F32), "g_mem": ([128, KC], F32),
        "w_pq": ([D, D], F32), "sk1_T": ([128, 128], F32), "sk2_T": ([128, 128], F32),
        "g_ffn_bc": ([128, D], F32), "g_fin_bc": ([128, D], F32),
        "expert_u": ([16384, D], F32), "expert_v": ([16384, D], F32),
        "w_cmp_k1": ([2048, 128], F32), "w_cmp_k2": ([128, 64], F32), "pe_k_T": ([64, 32], F32),
        "w_cmp_v1": ([2048, 128], F32), "w_cmp_v2": ([128, 64], F32), "pe_v_T": ([64, 32], F32),
    }


def build(debug_out=(), upto=99, skip=()):
    nc = bass.Bass("TRN2", target_bir_lowering=False)
    with contextlib.ExitStack() as st:
        c = Ctx(nc, st)
        io = {k: c.dram(k, shp, dt, kind="ExternalInput") for k, (shp, dt) in input_spec().items()}
        S = {}
        for k, (shp, dt) in scratch_spec().items():
            S[k] = c.dram("s_" + k, shp, dt, kind=("ExternalOutput" if k in debug_out else "Internal"))
        conv = ExpertConv(c, io, S)
        phase1(c, io, S)
        if upto >= 2 and 2 not in skip:
            phase2(c, io, S)
        if upto >= 3 and 3 not in skip:
            phase3(c, io, S, after_loads=conv.emit)
        else:
            conv.emit()
        c.barrier()
        conv.close()
        if upto >= 4 and 4 not in skip:
            phase4(c, io, S)
        out_t = c.dram("out", [NOWN, D], F32, kind="ExternalOutput")
        if upto >= 5:
            phase5(c, io, S, out_t)
        c.finish([])
    return nc, c


_CACHE = {}


def _gl(v):
    return np.ascontiguousarray(np.asarray(v, np.float32).reshape(16, 128).T)


def make_inputs(inp, cores=range(8)):
    x = np.asarray(inp["x"], np.float32)[0]
    xT = np.ascontiguousarray(x.T)
    st = host_b_static()
    shared = {
        "w_in": np.asarray(inp["w_in"], np.float32)[0],
        "g_mix": _gl(inp["norm_mix"][0]),
        "mem": np.asarray(inp["mem"], np.float32)[0],
        "g_x": _gl(inp["norm_x"][0]), "g_mem": _gl(inp["norm_mem"][0]),
        "pe_k_T": np.ascontiguousarray(np.asarray(inp["pe_cmp_k"], np.float32)[0].T),
        "pe_v_T": np.ascontiguousarray(np.asarray(inp["pe_cmp_v"], np.float32)[0].T),
        "sk1_T": np.ascontiguousarray(np.asarray(inp["sub_keys1"], np.float32)[0].T),
        "sk2_T": np.ascontiguousarray(np.asarray(inp["sub_keys2"], np.float32)[0].T),
        "g_ffn_bc": np.ascontiguousarray(np.broadcast_to(np.asarray(inp["norm_ffn"], np.float32)[0][None, :], (128, D))),
        "g_fin_bc": np.ascontiguousarray(np.broadcast_to(np.asarray(inp["norm_final"], np.float32)[None, :], (128, D))),
        "expert_u": np.asarray(inp["expert_u"], np.float32)[0],
        "expert_v": np.asarray(inp["expert_v"], np.float32)[0],
    }
    for k in ("w_cmp_k1", "w_cmp_k2", "w_cmp_v1", "w_cmp_v2", "w_up_a", "w_up_b", "w_out", "w_xq", "w_xkv", "w_xo", "w_pq"):
        shared[k] = np.asarray(inp[k], np.float32)[0]
    shared.update(st)
    maps = []
    for cc in cores:
        lo = 1024 * cc - 7168
        w = np.zeros((D, NW), np.float32)
        if lo >= 0:
            w[:] = xT[:, lo:lo + NW]
        else:
            w[:, -lo:] = xT[:, :NW + lo]
        tab, kval = host_a_tables(cc)
        d = dict(shared)
        w = np.ascontiguousarray(w.reshape(KC, 128, 16, 512).transpose(2, 1, 0, 3))
        d.update({"xT": w, "x_own": np.ascontiguousarray(x[1024 * cc:1024 * cc + 1024]), "a_bias": tab, "a_kvalid": kval})
        d.update(host_b_core(cc))
        maps.append(d)
    return maps


def kernel(**inputs):
    if "nc" not in _CACHE:
        _CACHE["nc"] = build()[0]
    nc = _CACHE["nc"]
    maps = make_inputs(inputs)
    res = run_bass_kernel_spmd(nc, maps, core_ids=list(range(8)))
    out = np.concatenate([np.asarray(r["out"], np.float32) for r in res.results], axis=0)
    return out.reshape(1, 8192, D)
```

```python
import contextlib
from concourse.bass_utils import run_bass_kernel_spmd
import numpy as np
import concourse.bass as bass
import concourse.mybir as mybir

F32 = mybir.dt.float32
BF16 = mybir.dt.bfloat16
I32 = mybir.dt.int32
U32 = mybir.dt.uint32
ALU = mybir.AluOpType
AF = mybir.ActivationFunctionType
AX = mybir.AxisListType


class T:
    def __init__(self, name, ap):
        self.name = name
        self.ap = ap
        self.w = None
        self.r = []

    def __getitem__(self, k):
        return V(self, self.ap[k])


class V:
    def __init__(self, t, ap):
        self.t = t
        self.ap = ap

    def __getitem__(self, k):
        return V(self.t, self.ap[k])


class Ctx:
    ENG = ("pe", "act", "dve", "pool", "sp")

    def __init__(self, nc, stack):
        self.nc = nc
        self.stack = stack
        self.eng = {"pe": nc.tensor, "act": nc.scalar, "dve": nc.vector,
                    "pool": nc.gpsimd, "sp": nc.sync}
        self.CE = ("pe", "act", "dve", "pool")
        self.sem = {e: stack.enter_context(nc.semaphore("s_" + e)) for e in self.CE}
        self.cnt = {e: 0 for e in self.CE}
        self.NS = 12
        self.dsem = {q: [stack.enter_context(nc.semaphore(f"d_{q}{i}")) for i in range(self.NS)]
                     for q in ("sp", "pool")}
        self.dcnt = {q: 0 for q in ("sp", "pool")}
        self.waited = {e: {} for e in self.ENG}
        self.n_inst = 0
        self.uid = 0

    def sb(self, shape, dt, name=None):
        self.uid += 1
        name = name or f"sb{self.uid}"
        h = self.stack.enter_context(self.nc.sbuf_tensor(name, list(shape), dt))
        return T(name, h)

    def ps(self, shape, dt=F32, name=None):
        self.uid += 1
        name = name or f"ps{self.uid}"
        h = self.stack.enter_context(self.nc.psum_tensor(name, list(shape), dt))
        return T(name, h)

    def dram(self, name, shape, dt, kind="Internal"):
        h = self.nc.dram_tensor(name, list(shape), dt, kind=kind)
        return T(name, h.ap())

    def _semof(self, key):
        if isinstance(key, tuple):
            return self.dsem[key[0]][key[1]]
        return self.sem[key]

    def _need(self, e, dep):
        if dep is None:
            return
        key, val = dep
        if self.waited[e].get(key, 0) >= val:
            return
        self.eng[e].wait_ge(self._semof(key), val)
        self.waited[e][key] = val

    def op(self, e, fn, reads=(), writes=(), pe_chain=False, dma=False):
        rt = [v.t if isinstance(v, V) else v for v in reads]
        wt = [v.t if isinstance(v, V) else v for v in writes]
        for t in rt:
            self._need(e, t.w)
        for t in wt:
            if not (pe_chain and t.w is not None and t.w[0] == e):
                self._need(e, t.w)
            for d in t.r:
                self._need(e, d)
        if dma:
            n = self.dcnt[e]
            slot = n % self.NS
            val = 16 * (n // self.NS + 1)
            key = (e, slot)
            if n >= self.NS:
                self._need(e, (key, val - 16))
            ins = fn()
            ins.then_inc(self.dsem[e][slot], 16)
            self.dcnt[e] += 1
            tok = (key, val)
        else:
            ins = fn()
            self.cnt[e] += 1
            ins.then_inc(self.sem[e], 1)
            tok = (e, self.cnt[e])
        for t in rt:
            t.r.append(tok)
            if len(t.r) > 48:
                last = {}
                for (x, s_) in t.r:
                    last[x] = max(last.get(x, 0), s_)
                t.r = list(last.items())
        for t in wt:
            t.w = tok
            t.r = []
        self.n_inst += 1
        return ins

    def barrier(self):
        for e in self.ENG:
            for x in self.CE:
                if x != e and self.cnt[x] > 0:
                    self._need(e, (x, self.cnt[x]))
            for q in ("sp", "pool"):
                n = self.dcnt[q]
                for slot in range(min(n, self.NS)):
                    k = (n - 1 - slot) // self.NS + 1 if n - 1 >= slot else 0
                    cnt_slot = (n - slot + self.NS - 1) // self.NS
                    if cnt_slot > 0:
                        self._need(e, ((q, slot), 16 * cnt_slot))

    def dma(self, out, in_, eng="sp", **kw):
        return self.op(eng, lambda: self.eng[eng].dma_start(out=out.ap, in_=in_.ap, **kw),
                       reads=[in_], writes=[out], dma=True)

    def mm(self, out, lhsT, rhs, start=True, stop=True):
        return self.op("pe", lambda: self.nc.tensor.matmul(out.ap, lhsT.ap, rhs.ap, start=start, stop=stop),
                       reads=[lhsT, rhs], writes=[out], pe_chain=True)

    def transpose(self, out, in_, ident):
        return self.op("pe", lambda: self.nc.tensor.transpose(out.ap, in_.ap, ident.ap),
                       reads=[in_, ident], writes=[out], pe_chain=True)

    def act(self, out, in_, func, bias=None, scale=None, accum=None, extra_reads=()):
        kw = {}
        rd = [in_] + list(extra_reads)
        wr = [out]
        if bias is not None:
            if isinstance(bias, V):
                kw["bias"] = bias.ap; rd.append(bias)
            else:
                kw["bias"] = bias
        if scale is not None:
            if isinstance(scale, V):
                kw["scale"] = scale.ap; rd.append(scale)
            else:
                kw["scale"] = scale
        if accum is not None:
            kw["accum_out"] = accum.ap; wr.append(accum)
        return self.op("act", lambda: self.nc.scalar.activation(out.ap, in_.ap, func, **kw),
                       reads=rd, writes=wr)

    def _ve(self, e):
        return self.nc.vector if e == "dve" else self.nc.gpsimd

    def tt(self, out, in0, in1, op, e="dve"):
        return self.op(e, lambda: self._ve(e).tensor_tensor(out.ap, in0.ap, in1.ap, op),
                       reads=[in0, in1], writes=[out])

    def ts(self, out, in0, s1, s2, op0, op1=None, e="dve", accum=None):
        rd = [in0]; wr = [out]
        a1 = s1.ap if isinstance(s1, V) else s1
        a2 = s2.ap if isinstance(s2, V) else s2
        if isinstance(s1, V): rd.append(s1)
        if isinstance(s2, V): rd.append(s2)
        kw = {}
        if op1 is not None: kw["op1"] = op1
        if accum is not None:
            kw["accum_out"] = accum.ap; wr.append(accum)
        return self.op(e, lambda: self._ve(e).tensor_scalar(out.ap, in0.ap, a1, a2, op0, **kw),
                       reads=rd, writes=wr)

    def stt(self, out, in0, s, in1, op0, op1, e="dve", accum=None):
        rd = [in0, in1]; wr = [out]
        a = s.ap if isinstance(s, V) else s
        if isinstance(s, V): rd.append(s)
        kw = {}
        if accum is not None:
            kw["accum_out"] = accum.ap; wr.append(accum)
        return self.op(e, lambda: self._ve(e).scalar_tensor_tensor(out.ap, in0.ap, a, in1.ap, op0, op1, **kw),
                       reads=rd, writes=wr)

    def copy(self, out, in_, e="dve"):
        if e == "act":
            return self.op("act", lambda: self.nc.scalar.copy(out.ap, in_.ap), reads=[in_], writes=[out])
        return self.op(e, lambda: self._ve(e).tensor_copy(out.ap, in_.ap), reads=[in_], writes=[out])

    def recip(self, out, in_):
        return self.op("dve", lambda: self.nc.vector.reciprocal(out.ap, in_.ap), reads=[in_], writes=[out])

    def memset(self, out, val, e="dve"):
        return self.op(e, lambda: self._ve(e).memset(out.ap, val), reads=[], writes=[out])

    def finish(self, out_tensors):
        self.barrier()

D = 2048
NW = 8192
NOWN = 1024
KC = 16
C_AQ, C_AK, C_AV = 0, 1536, 3072
C_BQ = 4608
C_BKV = 5632
C_BG = 6400
C_MG = 6448
IN_COLS = 10544
EPS = 1e-6


class Rot:
    def __init__(self, tiles):
        self.tiles = tiles
        self.i = 0

    def next(self):
        t = self.tiles[self.i % len(self.tiles)]
        self.i += 1
        return t


def phase1(c, io, S):
    nc = c.nc
    top = c.stack
    with contextlib.ExitStack() as ph:
        c.stack = ph
        ones = c.sb([128, 128], BF16, "p1_ones")
        c.memset(ones[:], 1.0)
        gmix = c.sb([128, KC], F32, "p1_g")
        c.dma(gmix[:], io["g_mix"][:])
        with contextlib.ExitStack() as pa:
            c.stack = pa
            XT = [c.sb([128, KC, 512], F32, f"p1_xt{i}") for i in range(2)]
            XN = [c.sb([128, KC, 512], BF16, f"p1_xn{i}") for i in range(2)]
            SQ = Rot([c.sb([128, 512], BF16, f"p1_sq{i}") for i in range(3)])
            SSP = Rot([c.ps([128, 512], F32, f"p1_ss{i}") for i in range(2)])
            RB = Rot([c.sb([128, 512], F32, f"p1_rb{i}") for i in range(2)])
            for ch in range(16):
                xt = XT[ch % 2]
                xn = XN[ch % 2]
                c.dma(xt[:], V(io["xT"], io["xT"].ap[ch]))
                ss = SSP.next()
                for k in range(KC):
                    sq = SQ.next()
                    c.act(sq[:], xt[:, k, :], AF.Square)
                    c.mm(ss[:], ones[:], sq[:], start=(k == 0), stop=(k == KC - 1))
                rb = RB.next()
                c.ts(rb[:], ss[:], 1.0 / D, EPS, ALU.mult, ALU.add)
                c.act(rb[:], rb[:], AF.Sqrt)
                c.recip(rb[:], rb[:])
                for k in range(KC):
                    c.stt(xn[:, k, :], xt[:, k, :], gmix[:, k:k + 1], rb[:], ALU.mult, ALU.mult)
                c.dma(V(S["XN"], S["XN"].ap[ch]), xn[:], eng="pool")
        c.barrier()
        with contextlib.ExitStack() as pb:
            c.stack = pb
            WFs = [c.sb([128, KC, 512], F32, f"p1_wf{i}") for i in range(2)]
            XOWN = {}
            WB = [c.sb([128, KC, 512], BF16, f"p1_wb{i}") for i in range(2)]
            XNL = Rot([c.sb([128, KC, 512], BF16, f"p1_xl{i}") for i in range(2)])
            PS = Rot([c.ps([128, 512], F32, f"p1_ps{i}") for i in range(4)])
            STG = Rot([c.sb([128, 512], BF16, f"p1_st{i}") for i in range(4)])
            STGF = Rot([c.sb([128, 64], F32, f"p1_sf{i}") for i in range(2)])
            wv = io["w_in"].ap.rearrange("(k p) n -> p k n", p=128)
            slab_i = [0]

            def load_slab(col0, ncols):
                wb = WB[slab_i[0] % 2]
                WF = WFs[slab_i[0] % 2]
                slab_i[0] += 1
                c.dma(WF[:, :, 0:ncols], V(io["w_in"], wv[:, :, col0:col0 + ncols]))
                for k in range(KC):
                    e = ("act", "dve")[k % 2]
                    c.copy(wb[:, k, 0:ncols], WF[:, k, 0:ncols], e=e)
                return wb

            def load_xn(ch):
                if ch in XOWN:
                    return XOWN[ch]
                t = XNL.next()
                c.dma(t[:], V(S["XN"], S["XN"].ap[ch]))
                return t

            def fm(wb, xl, c0, M, evac):
                ps = PS.next()
                for k in range(KC):
                    c.mm(ps[0:M, :], wb[:, k, c0:c0 + M], xl[:, k, :], start=(k == 0), stop=(k == KC - 1))
                evac(ps)

            def tm(wb, xl, tt, c0, ncols, evac):
                ps = PS.next()
                for k in range(KC):
                    c.mm(ps[:, 0:ncols], xl[:, k, tt * 128:(tt + 1) * 128], wb[:, k, c0:c0 + ncols],
                         start=(k == 0), stop=(k == KC - 1))
                evac(ps)

            def ev_fm(dst_t, dst_ap, M, func=None, scale=None):
                def f(ps):
                    st = STG.next()
                    if func is None and scale is None:
                        c.copy(st[0:M, :], ps[0:M, :], e="dve")
                    else:
                        c.act(st[0:M, :], ps[0:M, :], func or AF.Copy, scale=scale)
                    c.dma(V(dst_t, dst_ap), st[0:M, :], eng="pool")
                return f

            def ev_tm(dst_t, dst_ap, ncols):
                def f(ps):
                    st = STG.next()
                    c.copy(st[:, 0:ncols], ps[:, 0:ncols], e="dve")
                    c.dma(V(dst_t, dst_ap), st[:, 0:ncols], eng="pool")
                return f

            for ch_ in (14, 15):
                t_ = c.sb([128, KC, 512], BF16, f"p1_xown{ch_}")
                c.dma(t_[:], V(S["XN"], S["XN"].ap[ch_]))
                XOWN[ch_] = t_
            for g in range(3):
                wb = load_slab(C_AQ + g * 512, 512)
                for ch in (14, 15):
                    xl = load_xn(ch)
                    o0 = (ch - 14) * 512
                    for h in range(4):
                        fm(wb, xl, h * 128, 128, ev_fm(S["AQT"], S["AQT"].ap[g, h, :, o0:o0 + 512], 128))
                chs = (13, 14, 15) if g < 2 else (10, 11, 12, 13, 14, 15)
                wb = load_slab(C_AK + g * 512, 512)
                for ch in chs:
                    xl = load_xn(ch)
                    o0 = (ch - 10) * 512
                    for h in range(4):
                        fm(wb, xl, h * 128, 128, ev_fm(S["AKT"], S["AKT"].ap[g, h, :, o0:o0 + 512], 128))
                wb = load_slab(C_AV + g * 512, 512)
                for ch in chs:
                    xl = load_xn(ch)
                    o0 = (ch - 10) * 512
                    for tt in range(4):
                        tm(wb, xl, tt, 0, 512, ev_tm(S["AV"], S["AV"].ap[g, o0 + tt * 128:o0 + (tt + 1) * 128, :], 512))
            for g in range(2):
                wb = load_slab(C_BQ + g * 512, 512)
                for ch in (14, 15):
                    xl = load_xn(ch)
                    for h in range(8):
                        dst = S["BQT"].ap[g, :, (ch - 14) * 4:(ch - 14) * 4 + 4, h, :]
                        def f(ps, dst=dst):
                            st = STG.next()
                            c.act(st[0:64, :], ps[0:64, :], AF.Copy, scale=0.125)
                            c.dma(V(S["BQT"], dst), V(st, st.ap[0:64, :].rearrange("p (i q) -> p i q", q=128)), eng="pool")
                        fm(wb, xl, h * 64, 64, f)
            wb = load_slab(C_BKV, 512)
            for ch in range(16):
                xl = load_xn(ch)
                o0 = ch * 512
                fm(wb, xl, 0, 128, ev_fm(S["KCT"], S["KCT"].ap[:, o0:o0 + 512], 128))
                fm(wb, xl, 128, 128, ev_fm(S["VCT"], S["VCT"].ap[:, o0:o0 + 512], 128))
                fm(wb, xl, 256, 128, ev_fm(S["KST"], S["KST"].ap.rearrange("g d w -> (g d) w")[:, o0:o0 + 512], 128))
                for tt in range(4):
                    tm(wb, xl, tt, 384, 128, ev_tm(S["VS"], S["VS"].ap[o0 + tt * 128:o0 + (tt + 1) * 128, :], 128))
            wb = load_slab(C_BKV + 512, 256 + 48)
            for ch in (13, 14, 15):
                xl = load_xn(ch)
                o0 = (ch - 13) * 512
                fm(wb, xl, 0, 128, ev_fm(S["KWT"], S["KWT"].ap.rearrange("g d w -> (g d) w")[:, o0:o0 + 512], 128))
                for tt in range(4):
                    tm(wb, xl, tt, 128, 128, ev_tm(S["VW"], S["VW"].ap[o0 + tt * 128:o0 + (tt + 1) * 128, :], 128))
                if ch >= 14:
                    for tt in range(4):
                        r0 = (ch - 14) * 512 + tt * 128
                        def f(ps, r0=r0):
                            sf = STGF.next()
                            c.act(sf[:, 0:48], ps[:, 0:48], AF.Sigmoid)
                            c.dma(S["BG"][r0:r0 + 128, :], sf[:, 0:48], eng="pool")
                        tm(wb, xl, tt, 256, 48, f)
            for sl in range(8):
                wb = load_slab(C_MG + sl * 512, 512)
                for ch in (14, 15):
                    xl = load_xn(ch)
                    o0 = (ch - 14) * 512
                    for j in range(4):
                        fm(wb, xl, j * 128, 128,
                           ev_fm(S["MG"], S["MG"].ap[sl * 4 + j, :, o0:o0 + 512], 128, func=AF.Sigmoid))
        c.barrier()
    c.stack = top

A_DIL = (1, 4, 16)
A_J = [128 * (d + 1) + 768 for d in A_DIL]
A_JOFF = []
_o = 0
for _g in range(3):
    for _h in range(4):
        A_JOFF.append(_o)
        _o += A_J[_g]
AB_COLS = _o
NEGM = -30000.0


def host_a_tables(core):
    slopes = (2.0 ** (-8.0 * (np.arange(12, dtype=np.float64) + 1.0) / 12)).reshape(3, 4)
    tab = np.empty((128, AB_COLS), np.float32)
    p = np.arange(128)[:, None]
    for g in range(3):
        d = A_DIL[g]
        jx = np.arange(A_J[g])[None, :]
        delta = (jx - 384) - p
        valid = (delta >= 0) & (delta <= 128 * d) & (delta % d == 0)
        for h in range(4):
            o = A_JOFF[g * 4 + h]
            tab[:, o:o + A_J[g]] = np.where(valid, -slopes[g, h] * delta, NEGM)
    wrel = np.arange(24)[None, :] * 128 + p
    kval = np.where(wrel + 1024 * core - 2048 >= 0, 0.0, NEGM).astype(np.float32)
    return tab, kval


def phase2(c, io, S):
    top = c.stack
    scale = float(128 ** -0.5)
    with contextlib.ExitStack() as ph:
        c.stack = ph
        ones = c.sb([128, 128], BF16, "p2_ones")
        c.memset(ones[:], 1.0)
        KV = c.sb([128, 24], F32, "p2_kv")
        c.dma(KV[:], io["a_kvalid"][:])
        KT = Rot([c.sb([128, 20 * 128], BF16, f"p2_kt{i}") for i in range(2)])
        VT = Rot([c.sb([128, 20, 128], BF16, f"p2_vt{i}") for i in range(2)])
        BT = Rot([c.sb([128, A_J[2]], F32, f"p2_bt{i}") for i in range(2)])
        QT = Rot([c.sb([128, 512], BF16, f"p2_qt{i}") for i in range(2)])
        PSS = Rot([c.ps([128, 512], F32, f"p2_ps{i}") for i in range(3)])
        PSN = Rot([c.ps([128, 512], F32, f"p2_pn{i}") for i in range(2)])
        PSD = Rot([c.ps([128, 512], F32, f"p2_pd{i}") for i in range(2)])
        SBF = Rot([c.sb([128, 512], F32, f"p2_sb{i}") for i in range(3)])
        PT = Rot([c.sb([128, 512], BF16, f"p2_pt{i}") for i in range(3)])
        RD = Rot([c.sb([128, 512], F32, f"p2_rd{i}") for i in range(2)])
        YT = Rot([c.sb([128, 512], BF16, f"p2_yt{i}") for i in range(2)])
        for quad in range(2):
            i0 = quad * 4
            for h in range(4):
                Np = PSN.next()
                Dp = PSD.next()
                st_ = [True]
                pend = []
                for g in range(3):
                    d = A_DIL[g]
                    kb_lo = 16 + i0 - d
                    kb_hi = 16 + i0 + 3
                    nb = kb_hi - kb_lo + 1
                    kt = KT.next()
                    c.dma(kt[:, 0:nb * 128], S["AKT"][g, h, :, kb_lo * 128:(kb_hi + 1) * 128])
                    vt = VT.next()
                    c.dma(vt[:, 0:nb, :],
                          V(S["AV"], S["AV"].ap[g, kb_lo * 128:(kb_hi + 1) * 128, h * 128:(h + 1) * 128]
                            .rearrange("(b p) d -> p b d", p=128)))
                    bt = BT.next()
                    o = A_JOFF[g * 4 + h]
                    c.dma(bt[:, 0:A_J[g]], io["a_bias"][:, o:o + A_J[g]])
                    qt = QT.next()
                    c.dma(qt[:], S["AQT"][g, h, :, i0 * 128:(i0 + 4) * 128])
                    def a_s(kb, kt=kt, qt=qt, kb_lo=kb_lo):
                        sp = PSS.next()
                        c.mm(sp[:], kt[:, (kb - kb_lo) * 128:(kb - kb_lo + 1) * 128], qt[:])
                        return (sp,)

                    def a_rest(kb, sp, g=g, bt=bt, vt=vt, kb_lo=kb_lo, kb_hi=kb_hi):
                        sb = SBF.next()
                        jx0 = 128 * (16 + i0 - kb) + 384
                        c.stt(sb[:], sp[:], scale, bt[:, jx0:jx0 + 512], ALU.mult, ALU.add)
                        pt = PT.next()
                        c.act(pt[:], sb[:], AF.Exp, bias=KV[:, kb:kb + 1])
                        last = (g == 2 and kb == kb_hi)
                        c.mm(Np[:], vt[:, kb - kb_lo, :], pt[:], start=st_[0], stop=last)
                        c.mm(Dp[:], ones[:], pt[:], start=st_[0], stop=last)
                        st_[0] = False

                    for kb in range(kb_lo, kb_hi + 1):
                        pend.append((a_rest, (kb,) + a_s(kb)))
                        if len(pend) > 1:
                            f_, a_ = pend.pop(0)
                            f_(*a_)
                while pend:
                    f_, a_ = pend.pop(0)
                    f_(*a_)
                rd = RD.next()
                c.recip(rd[:], Dp[:])
                yt = YT.next()
                c.tt(yt[:], Np[:], rd[:], ALU.mult)
                c.dma(S["YAT"][h, :, i0 * 128:(i0 + 4) * 128], yt[:], eng="pool")
        c.barrier()
    c.stack = top

import ml_dtypes
NPBF = ml_dtypes.bfloat16


def _split3(v):
    v = np.asarray(v, np.float64)
    a = v.astype(NPBF).astype(np.float64)
    b = (v - a).astype(NPBF).astype(np.float64)
    cc = (v - a - b).astype(NPBF).astype(np.float64)
    return a, b, cc


def host_b_static():
    T_ = {}
    slopes = 2.0 ** (-8.0 * (np.arange(16, dtype=np.float64) + 1.0) / 16)
    w = np.arange(8192)
    ka = np.zeros((8, 8192), np.float64)
    ka[0:3] = (w % 128)[None]
    ka[3:6] = (w - w % 128)[None]
    ka[6:8] = 1.0
    T_["kaug_sel"] = ka.astype(NPBF)
    cidx = np.arange(512)
    tau = 16 * cidx + 31
    kc = np.zeros((8, 512), np.float64)
    kc[0:3] = (tau % 128)[None]
    kc[3:6] = (tau - tau % 128)[None]
    kc[6:8] = 1.0
    T_["kaug_cmp"] = kc.astype(NPBF)
    qa = np.zeros((2, 8, 8, 8, 128), np.float64)
    for g in range(2):
        for h in range(8):
            s = slopes[g * 8 + h]
            s1, s2, s3 = _split3(s)
            for i in range(8):
                tq = 7168 + 128 * i + np.arange(128)
                cq = -s * tq
                c1 = cq.astype(NPBF).astype(np.float64)
                c2 = (cq - c1).astype(NPBF).astype(np.float64)
                qa[g, i, 0, h] = s1; qa[g, i, 1, h] = s2; qa[g, i, 2, h] = s3
                qa[g, i, 3, h] = s1; qa[g, i, 4, h] = s2; qa[g, i, 5, h] = s3
                qa[g, i, 6, h] = c1; qa[g, i, 7, h] = c2
    T_["qaug"] = qa.reshape(2, 8, 8, 1024).astype(NPBF)
    jj = np.arange(128)[:, None]
    T_["EE"] = (jj == (w // 64)[None, :]).astype(NPBF)
    kl = np.arange(128)[:, None]
    ql = np.arange(128)[None, :]
    tri = np.where(kl > ql, NEGM, 0.0)
    tri2 = np.where(kl <= ql, NEGM, 0.0)
    T_["TRI"] = np.tile(tri, (1, 8)).astype(NPBF)
    T_["TRI2"] = np.tile(tri2, (1, 8)).astype(NPBF)
    cm = np.zeros((8, 128, 128))
    for i in range(8):
        cm[i] = np.where(16 * (384 + kl) + 31 <= 7168 + 128 * i + ql, 0.0, NEGM)
    T_["CM"] = np.tile(cm, (1, 1, 8)).astype(NPBF)
    cs = 16 * cidx[:, None]
    ss = 64 * np.arange(128)[None, :]
    ov = np.clip(np.minimum(cs + 32, ss + 64) - np.maximum(cs, ss), 0, None) / 32.0
    ovx = np.zeros((512, 129))
    ovx[:, :128] = ov
    ovx[:, 128] = 1.0
    T_["OV"] = ovx.reshape(4, 128, 129).astype(NPBF)
    T_["ident"] = np.eye(128, dtype=np.float32)
    return T_


def host_b_core(core):
    T_ = {}
    p = np.arange(128)[:, None]
    c_ = 128 * np.arange(4)[None, :] + p
    T_["cval"] = np.where((16 * c_ >= 7168 - 1024 * core) & (c_ < 511), 0.0, NEGM).astype(np.float32)
    ww = 6656 + 128 * np.arange(12)[None, :] + p
    T_["wval"] = np.where(ww >= 7168 - 1024 * core, 0.0, NEGM).astype(np.float32)
    j0 = 112 - 16 * core
    jj = np.arange(128)[None, :]
    mul = np.zeros((8, 128, 128), np.float32)
    add = np.zeros((8, 128, 128), np.float32)
    for i in range(8):
        cur = (7168 + 128 * i + np.arange(128)[:, None]) // 64
        forced = (jj == cur) | (jj == cur - 1) | (jj == j0)
        ok = (jj <= cur) & (jj >= j0)
        forced = forced & (jj >= j0)
        mul[i] = np.where(ok & ~forced, 1.0, 0.0)
        add[i] = np.where(forced, 1e4, np.where(ok, 0.0, -1e30))
    T_["smul"] = mul
    T_["sadd"] = add
    return T_


NSA_PRUNE = 24


def bcast_ap(ap, dims):
    return bass.AP(tensor=ap.tensor, offset=ap.offset, ap=[list(ap.ap[0])] + [list(d) for d in dims])


def gelu_tanh(c, out, xin, t1, t2):
    c.tt(t1, xin, xin, ALU.mult)
    c.ts(t1, t1, 0.044715, 1.0, ALU.mult, ALU.add)
    c.tt(t1, t1, xin, ALU.mult)
    c.act(t2, t1, AF.Sigmoid, scale=1.5957691216057308)
    c.tt(out, xin, t2, ALU.mult)


def phase3(c, io, S, after_loads=None):
    top = c.stack
    with contextlib.ExitStack() as ph:
        c.stack = ph
        ident = c.sb([128, 128], F32, "p3_id")
        c.dma(ident[:], io["ident"][:])
        KS = [c.sb([72, NW], BF16, f"p3_ks{g}") for g in range(2)]
        KW = [c.sb([72, 1536], BF16, f"p3_kw{g}") for g in range(2)]
        for g in range(2):
            c.dma(KS[g][0:64, :], S["KST"][g, :, :])
            c.dma(KS[g][64:72, :], io["kaug_sel"][:, :])
            c.dma(KW[g][0:64, :], S["KWT"][g, :, :])
            c.dma(KW[g][64:72, :], io["kaug_sel"][:, 6656:8192])
        VSA = c.sb([128, 64, 2, 65], BF16, "p3_vs")
        c.memset(VSA[:, :, :, 64:65], 1.0, e="pool")
        for g in range(2):
            c.dma(VSA[:, :, g, 0:64], V(S["VS"], S["VS"].ap[:, g * 64:(g + 1) * 64].rearrange("(m p) d -> p m d", p=128)))
        VWA = c.sb([128, 12, 2, 65], BF16, "p3_vw")
        c.memset(VWA[:, :, :, 64:65], 1.0, e="pool")
        for g in range(2):
            c.dma(VWA[:, :, g, 0:64], V(S["VW"], S["VW"].ap[:, g * 64:(g + 1) * 64].rearrange("(m p) d -> p m d", p=128)))
        EE = c.sb([128, NW], BF16, "p3_ee")
        c.dma(EE[:], io["EE"][:])
        TRI = c.sb([128, 1024], BF16, "p3_tri")
        c.dma(TRI[:], io["TRI"][:])
        TRI2 = c.sb([128, 1024], BF16, "p3_tri2")
        c.dma(TRI2[:], io["TRI2"][:])
        IDB = c.sb([128, 128], BF16, "p3_idb")
        c.copy(IDB[:], ident[:])
        OV = c.sb([128, 4, 129], BF16, "p3_ov")
        c.dma(OV[:], V(io["OV"], io["OV"].ap.rearrange("t p n -> p t n")))
        CVAL = c.sb([128, 4], F32, "p3_cval")
        c.dma(CVAL[:], io["cval"][:])
        WVAL = c.sb([128, 12], F32, "p3_wval")
        c.dma(WVAL[:], io["wval"][:])
        KCA = [c.sb([72, 512], BF16, f"p3_kca{g}") for g in range(2)]
        VCA = c.sb([128, 4, 2, 65], BF16, "p3_vca")
        c.memset(VCA[:, :, :, 64:65], 1.0, e="pool")
        PSS = Rot([c.ps([128, 1024], F32, f"p3_pss{i}") for i in range(2)])
        PSO = c.ps([128, 1024], F32, "p3_pso")
        PSM = Rot([c.ps([128, 512], F32, f"p3_psm{i}") for i in range(2)])

        with contextlib.ExitStack() as p1:
            c.stack = p1
            KC = c.sb([128, NW], BF16, "p3_kc")
            W1F = c.sb([128, 32, 128], F32, "p3_w1f")
            W1 = c.sb([128, 32, 128], BF16, "p3_w1")
            W2F = c.sb([128, 64], F32, "p3_w2f")
            W2 = c.sb([128, 64], BF16, "p3_w2")
            PEF = c.sb([128, 32], F32, "p3_pef")
            PEB = c.sb([128, 32], BF16, "p3_peb")
            HB = c.sb([128, 1], F32, "p3_hb")
            HX = c.sb([128, 512], F32, "p3_hx")
            H1 = c.sb([128, 512], F32, "p3_h1")
            H2 = c.sb([128, 512], F32, "p3_h2")
            HT = c.sb([128, 512], BF16, "p3_ht")
            for kv in range(2):
                src = S["KCT"] if kv == 0 else S["VCT"]
                c.dma(KC[:], src[:, :])
                w1 = io["w_cmp_k1"] if kv == 0 else io["w_cmp_v1"]
                w2 = io["w_cmp_k2"] if kv == 0 else io["w_cmp_v2"]
                pe = io["pe_k_T"] if kv == 0 else io["pe_v_T"]
                w1v = w1.ap.rearrange("(p d) h -> d p h", d=64)
                for half in range(2):
                    c.dma(W1F[half * 64:(half + 1) * 64, :, :], V(w1, w1v))
                    c.dma(PEF[half * 64:(half + 1) * 64, :], pe[:, :])
                c.copy(W1[:], W1F[:], e="act")
                c.copy(PEB[:], PEF[:])
                c.dma(W2F[:], w2[:, :])
                c.copy(W2[:], W2F[:])
                bp = PSM.next()
                for p_ in range(32):
                    c.mm(bp[:, 0:1], W1[0:64, p_, :], PEB[0:64, p_:p_ + 1], start=(p_ == 0), stop=(p_ == 31))
                c.copy(HB[:], bp[:, 0:1])
                for g in range(2):
                    hp = PSM.next()
                    for p_ in range(32):
                        c.mm(hp[:, 0:511], W1[g * 64:(g + 1) * 64, p_, :],
                             KC[g * 64:(g + 1) * 64, p_:p_ + 8161:16], start=(p_ == 0), stop=(p_ == 31))
                    c.act(HX[:, 0:511], hp[:, 0:511], AF.Identity, bias=HB[:, 0:1])
                    c.memset(HX[:, 511:512], 0.0)
                    gelu_tanh(c, HT[:], HX[:], H1[:], H2[:])
                    if kv == 0:
                        op_ = PSM.next()
                        c.mm(op_[0:64, :], W2[:], HT[:])
                        c.copy(KCA[g][0:64, :], op_[0:64, :])
                        c.dma(KCA[g][64:72, :], io["kaug_cmp"][:, :])
                    else:
                        for ct in range(4):
                            op_ = PSM.next()
                            c.mm(op_[:, 0:64], HT[:, ct * 128:(ct + 1) * 128], W2[:])
                            c.copy(VCA[:, ct, g, 0:64], op_[:, 0:64])
        c.stack = ph
        c.barrier()

        QAall = [[c.sb([72, 1024], BF16, f"p3_qa{i}_{g}") for g in range(2)] for i in range(8)]
        PT = Rot([c.sb([128, 1024], BF16, f"p3_pt{i}") for i in range(3)])
        PC = [c.sb([128, 1024], BF16, f"p3_pc{i}") for i in range(4)]
        CMa = c.sb([128, 8, 1024], BF16, "p3_cma")
        SMULa = c.sb([128, 8, 128], F32, "p3_smula")
        SADDa = c.sb([128, 8, 128], F32, "p3_sadda")
        IMP = c.sb([128, 128], F32, "p3_imp")
        SCO = c.sb([128, 128], F32, "p3_sco")
        SWK = c.sb([128, 128], F32, "p3_swk")
        M8 = c.sb([128, 16], F32, "p3_m8")
        RDN = c.sb([128, 8], F32, "p3_rdn")
        SEL = c.sb([128, 128], F32, "p3_sel")
        SELV = c.sb([128, 128], F32, "p3_selv")
        SLT = c.sb([128, 128], BF16, "p3_slt")
        PTH = Rot([c.sb([128, 512], BF16, f"p3_pth{i}") for i in range(4)])
        PMH = Rot([c.sb([128, 512], BF16, f"p3_pmh{i}") for i in range(4)])
        HROT = Rot([T(f"p3_hv{i}", PSS.tiles[i // 2].ap[:, (i % 2) * 512:(i % 2 + 1) * 512]) for i in range(4)])
        OS = c.sb([65, 1024], F32, "p3_os")
        BGa = c.sb([128, 8, 48], F32, "p3_bga")
        BGh = [None]
        YB = c.sb([128, 512], F32, "p3_yb")
        COEF = c.sb([128, 8], F32, "p3_coef")
        YBT = Rot([c.sb([128, 128], BF16, f"p3_ybt{i}") for i in range(2)])

        TMP4 = c.sb([128, 256], F32, "p3_tmp4")

        def epilogue(b, g, first):
            c.copy(OS[:, :], PSO[0:65, :], e="act")
            for h0 in (0, 4):
                tp = PSM.next()
                for hh in range(4):
                    h = h0 + hh
                    c.transpose(tp[:, hh * 65:(hh + 1) * 65], OS[0:65, h * 128:(h + 1) * 128], ident[0:65, 0:65])
                cf = V(COEF, COEF.ap[:, h0:h0 + 4])
                den = V(tp, bcast_ap(tp.ap[:, 64:65], [[65, 4]]))
                c.ts(cf, den, 1e-30, None, ALU.max)
                c.recip(cf, cf)
                col0 = 3 * (g * 8 + h0) + b
                c.tt(cf, cf, V(BGa, bcast_ap(BGa.ap[:, BGh[0], col0:col0 + 1], [[3, 4]])), ALU.mult)
                ov = V(tp, bcast_ap(tp.ap[:, 0:1], [[65, 4], [1, 64]]))
                cb = V(COEF, bcast_ap(COEF.ap[:, h0:h0 + 1], [[1, 4], [0, 64]]))
                ybv = V(YB, YB.ap[:, h0 * 64:(h0 + 4) * 64].rearrange("p (h d) -> p h d", d=64))
                if first:
                    c.tt(ybv, ov, cb, ALU.mult)
                else:
                    tv = V(TMP4, TMP4.ap.rearrange("p (h d) -> p h d", d=64))
                    c.tt(tv, ov, cb, ALU.mult)
                    c.tt(ybv, ybv, tv, ALU.add)

        c.dma(BGa[:], V(S["BG"], S["BG"].ap.rearrange("(i p) n -> p i n", p=128)))
        c.dma(SMULa[:], V(io["smul"], io["smul"].ap.rearrange("i p n -> p i n")))
        c.dma(SADDa[:], V(io["sadd"], io["sadd"].ap.rearrange("i p n -> p i n")))
        c.dma(CMa[:], V(io["CM"], io["CM"].ap.rearrange("i p n -> p i n")))
        for i in range(8):
            for g in range(2):
                c.dma(QAall[i][g][0:64, :], V(S["BQT"], S["BQT"].ap[g, :, i, :, :].rearrange("p h q -> p (h q)")))
                c.dma(QAall[i][g][64:72, :], io["qaug"][g, i, :, :])
        c.barrier()
        if after_loads is not None:
            after_loads()
        for i in range(8):
            BGh[0] = i
            SMUL = V(SMULa, SMULa.ap[:, i, :])
            SADD = V(SADDa, SADDa.ap[:, i, :])
            cm = V(CMa, CMa.ap[:, i, :])
            for g in range(2):
                qa = QAall[i][g]
                for ct in range(4):
                    sp = PSS.next()
                    for hf in range(2):
                        cs = slice(hf * 512, (hf + 1) * 512)
                        c.mm(sp[:, cs], KCA[g][0:72, ct * 128:(ct + 1) * 128], qa[0:72, cs],
                             start=True, stop=(ct != 3))
                        if ct == 3:
                            c.mm(sp[:, cs], IDB[:], cm[:, cs], start=False, stop=True)
                    c.act(PC[ct][:], sp[:], AF.Exp, bias=CVAL[:, ct:ct + 1])
                for hf in range(2):
                    cs = slice(hf * 512, (hf + 1) * 512)
                    for ct in range(4):
                        c.mm(PSO[0:65, cs], VCA[:, ct, g, :], PC[ct][:, cs], start=(ct == 0), stop=(ct == 3))
                for h in range(8):
                    ip = PSM.next()
                    for ct in range(4):
                        c.mm(ip[:, 0:129], PC[ct][:, h * 128:(h + 1) * 128], OV[:, ct, :], start=(ct == 0), stop=(ct == 3))
                    c.ts(RDN[:, h:h + 1], ip[:, 128:129], 1e-30, None, ALU.max)
                    c.recip(RDN[:, h:h + 1], RDN[:, h:h + 1])
                    if h == 0:
                        c.ts(IMP[:], ip[:, 0:128], RDN[:, h:h + 1], None, ALU.mult)
                    else:
                        c.stt(IMP[:], ip[:, 0:128], RDN[:, h:h + 1], IMP[:], ALU.mult, ALU.add)
                epilogue(0, g, True)
                c.tt(SCO[:], IMP[:], SMUL[:], ALU.mult)
                c.tt(SCO[:], SCO[:], SADD[:], ALU.add)
                c.op("dve", lambda: c.nc.vector.max(out=M8.ap[:, 0:8], in_=SCO.ap[:]), reads=[SCO], writes=[M8])
                c.op("dve", lambda: c.nc.vector.match_replace(out=SWK.ap[:], in_to_replace=M8.ap[:, 0:8],
                                                              in_values=SCO.ap[:], imm_value=-3.0e38),
                     reads=[SCO, M8], writes=[SWK])
                c.op("dve", lambda: c.nc.vector.max(out=M8.ap[:, 8:16], in_=SWK.ap[:]), reads=[SWK], writes=[M8])
                c.ts(SEL[:], SCO[:], M8[:, 15:16], None, ALU.is_ge)
                c.ts(SELV[:], SCO[:], -1.0e29, None, ALU.is_gt)
                c.tt(SEL[:], SEL[:], SELV[:], ALU.mult)
                def win_s(mw):
                    sp = PSS.next()
                    for hf in range(2):
                        cs = slice(hf * 512, (hf + 1) * 512)
                        edge = (mw == i) or (mw == i + 4)
                        c.mm(sp[:, cs], KW[g][0:72, mw * 128:(mw + 1) * 128], qa[0:72, cs], start=True, stop=(not edge))
                        if mw == i:
                            c.mm(sp[:, cs], IDB[:], TRI2[:, cs], start=False, stop=True)
                        if mw == i + 4:
                            c.mm(sp[:, cs], IDB[:], TRI[:, cs], start=False, stop=True)
                    return (sp,)

                def win_rest(mw, sp):
                    pt = PT.next()
                    c.act(pt[:], sp[:], AF.Exp, bias=WVAL[:, mw:mw + 1])
                    for hf in range(2):
                        cs = slice(hf * 512, (hf + 1) * 512)
                        c.mm(PSO[0:65, cs], VWA[:, mw, g, :], pt[:, cs], start=(mw == i), stop=(mw == i + 4))

                pend = []
                for mw in range(i, i + 5):
                    pend.append((mw,) + win_s(mw))
                    if len(pend) > 1:
                        win_rest(*pend.pop(0))
                while pend:
                    win_rest(*pend.pop(0))
                epilogue(2, g, False)
                tp = PSM.next()
                c.transpose(tp[:, 0:128], SEL[:], ident[:])
                c.copy(SLT[:], tp[:, 0:128], e="act")
                mlast = 56 + i
                mfirst = max(0, mlast - NSA_PRUNE) if g == 0 else 0
                def sel_s(m, hf):
                    mk = mk_of.get(m)
                    if mk is None:
                        mk = PSM.next()
                        c.mm(mk[:, 0:128], EE[:, m * 128:(m + 1) * 128], SLT[:])
                        mk_of.clear()
                        mk_of[m] = mk
                    sp = HROT.next()
                    cs = slice(hf * 512, (hf + 1) * 512)
                    c.mm(sp[:], KS[g][0:72, m * 128:(m + 1) * 128], qa[0:72, cs], start=True, stop=(m != mlast))
                    if m == mlast:
                        c.mm(sp[:], IDB[:], TRI[:, cs], start=False, stop=True)
                    return sp, mk

                def sel_rest(m, hf, sp, mk):
                    cs = slice(hf * 512, (hf + 1) * 512)
                    pt = PTH.next()
                    c.act(pt[:], sp[:], AF.Exp)
                    pm = PMH.next()
                    c.tt(V(pm, pm.ap.rearrange("p (h q) -> p h q", q=128)),
                         V(pt, pt.ap.rearrange("p (h q) -> p h q", q=128)),
                         V(mk, bcast_ap(mk.ap[:, 0:128], [[0, 4], [1, 128]])), ALU.mult)
                    c.mm(PSO[0:65, cs], VSA[:, m, g, :], pm[:], start=(m == mfirst), stop=(m == mlast))

                mk_of = {}
                pend = []
                for m in range(mfirst, mlast + 1):
                    for hf in range(2):
                        pend.append((m, hf) + sel_s(m, hf))
                        if len(pend) > 2:
                            sel_rest(*pend.pop(0))
                while pend:
                    sel_rest(*pend.pop(0))
                epilogue(1, g, False)
                for j in range(4):
                    tp = PSM.next()
                    c.transpose(tp[:, 0:128], YB[:, j * 128:(j + 1) * 128], ident[:])
                    yt = YBT.next()
                    c.copy(yt[:], tp[:, 0:128], e="act")
                    c.dma(S["YBT"][g * 4 + j, :, i * 128:(i + 1) * 128], yt[:])
        c.barrier()
    c.stack = top

def load_w_bf16(c, dst, src_t, src_ap, stg_rot, nk, ncols):
    v = src_ap.rearrange("(k p) n -> p k n", p=128)
    for k in range(nk):
        st = stg_rot.next()
        c.dma(st[:, 0:ncols], V(src_t, v[:, k, :]))
        c.copy(dst[:, k, 0:ncols], st[:, 0:ncols], e=("act", "dve", "pool")[k % 3])


def rms_rows(c, r_out, xin, sq_scratch):
    c.act(sq_scratch, xin, AF.Square, accum=r_out)
    c.ts(r_out, r_out, 1.0 / D, EPS, ALU.mult, ALU.add)
    c.act(r_out, r_out, AF.Sqrt)
    c.recip(r_out, r_out)


def phase4(c, io, S):
    top = c.stack
    with contextlib.ExitStack() as ph:
        c.stack = ph
        ident = c.sb([128, 128], F32, "p4_id")
        c.dma(ident[:], io["ident"][:])
        IDB = c.sb([128, 128], BF16, "p4_idb")
        c.copy(IDB[:], ident[:])
        ones = c.sb([128, 128], BF16, "p4_ones")
        c.memset(ones[:], 1.0)
        STGW = Rot([c.sb([128, 2048], F32, f"p4_stg{i}") for i in range(2)])
        PS = Rot([c.ps([128, 512], F32, f"p4_ps{i}") for i in range(6)])
        PSB = Rot([c.ps([128, 512], BF16, f"p4_psb{i}") for i in range(2)])
        pmix = contextlib.ExitStack()
        c.stack = pmix
        MIXT = c.sb([128, 16, NOWN], BF16, "p4_mixt")
        c.stack = ph
        with contextlib.ExitStack() as pa:
            c.stack = pa
            WUA = c.sb([128, 4, 2048], BF16, "p4_wua")
            WUB = c.sb([128, 8, 2048], BF16, "p4_wub")
            load_w_bf16(c, WUA, io["w_up_a"], io["w_up_a"].ap, STGW, 4, 2048)
            load_w_bf16(c, WUB, io["w_up_b"], io["w_up_b"].ap, STGW, 8, 2048)
            YA = c.sb([128, 4, NOWN], BF16, "p4_ya")
            YBt = c.sb([128, 8, NOWN], BF16, "p4_yb")
            c.dma(YA[:], V(S["YAT"], S["YAT"].ap.rearrange("k p t -> p k t")))
            c.dma(YBt[:], V(S["YBT"], S["YBT"].ap.rearrange("k p t -> p k t")))
            MGA = Rot([c.sb([128, NOWN], BF16, f"p4_mga{i}") for i in range(2)])
            MGB = Rot([c.sb([128, NOWN], BF16, f"p4_mgb{i}") for i in range(2)])
            T1 = Rot([c.sb([128, 512], F32, f"p4_t1{i}") for i in range(2)])
            T2 = Rot([c.sb([128, 512], F32, f"p4_t2{i}") for i in range(2)])
            for j in range(16):
                ga = MGA.next(); gb = MGB.next()
                c.dma(ga[:], S["MG"][j, :, :])
                c.dma(gb[:], S["MG"][16 + j, :, :])
                for hf in range(2):
                    cs = slice(hf * 512, (hf + 1) * 512)
                    pa_ = PS.next()
                    for k in range(4):
                        c.mm(pa_[:], WUA[:, k, j * 128:(j + 1) * 128], YA[:, k, cs], start=(k == 0), stop=(k == 3))
                    pb_ = PS.next()
                    for k in range(8):
                        c.mm(pb_[:], WUB[:, k, j * 128:(j + 1) * 128], YBt[:, k, cs], start=(k == 0), stop=(k == 7))
                    t1 = T1.next(); t2 = T2.next()
                    c.tt(t1[:], pa_[:], ga[:, cs], ALU.mult)
                    c.tt(t2[:], pb_[:], gb[:, cs], ALU.mult)
                    c.tt(MIXT[:, j, cs], t1[:], t2[:], ALU.add, e="pool")
        c.stack = ph
        c.barrier()
        with contextlib.ExitStack() as pb:
            c.stack = pb
            WO = c.sb([128, 16, 2048], BF16, "p4_wo")
            load_w_bf16(c, WO, io["w_out"], io["w_out"].ap, STGW, 16, 2048)
            XO = Rot([c.sb([128, 2048], F32, f"p4_xo{i}") for i in range(2)])
            for tt in range(8):
                xo = XO.next()
                c.dma(xo[:], io["x_own"][tt * 128:(tt + 1) * 128, :])
                for cn in range(4):
                    ps = PS.next()
                    for j in range(16):
                        c.mm(ps[:], MIXT[:, j, tt * 128:(tt + 1) * 128], WO[:, j, cn * 512:(cn + 1) * 512],
                             start=(j == 0), stop=(j == 15))
                    c.tt(xo[:, cn * 512:(cn + 1) * 512], xo[:, cn * 512:(cn + 1) * 512], ps[:], ALU.add)
                c.dma(S["H"][tt * 128:(tt + 1) * 128, :], xo[:], eng="pool")
        c.stack = ph
        c.barrier()
        pmix.close()
        with contextlib.ExitStack() as pc:
            c.stack = pc
            gx = c.sb([128, KC], F32, "p4_gx")
            gm = c.sb([128, KC], F32, "p4_gm")
            c.dma(gx[:], io["g_x"][:])
            c.dma(gm[:], io["g_mem"][:])
            WQ = c.sb([128, 16, 512], BF16, "p4_wq")
            WKV = c.sb([128, 16, 1024], BF16, "p4_wkv")
            WXO = c.sb([128, 4, 2048], BF16, "p4_wxo")
            load_w_bf16(c, WQ, io["w_xq"], io["w_xq"].ap, STGW, 16, 512)
            load_w_bf16(c, WKV, io["w_xkv"], io["w_xkv"].ap, STGW, 16, 1024)
            load_w_bf16(c, WXO, io["w_xo"], io["w_xo"].ap, STGW, 4, 2048)
            HT_ = Rot([c.sb([128, 2048], F32, f"p4_h{i}") for i in range(2)])
            SQ = c.sb([128, 2048], BF16, "p4_sq")
            HB = Rot([c.sb([128, 2048], BF16, f"p4_hb{i}") for i in range(2)])
            RR = Rot([c.sb([128, 1], F32, f"p4_rr{i}") for i in range(4)])
            MNT = c.sb([128, 16, 256], BF16, "p4_mnt")
            KT = c.sb([128, 4, 256], BF16, "p4_kt")
            VM = c.sb([128, 2, 512], BF16, "p4_vm")

            def norm_T(src_tile, dst, tok0, gvec):
                rr = RR.next()
                rms_rows(c, rr[:], src_tile[:], SQ[:])
                hb = HB.next()
                c.act(hb[:], src_tile[:], AF.Copy, scale=rr[:, 0:1])
                for k4 in range(4):
                    tp = PSB.next()
                    for kk in range(4):
                        k = k4 * 4 + kk
                        c.transpose(tp[:, kk * 128:(kk + 1) * 128], hb[:, k * 128:(k + 1) * 128], IDB[:])
                    for kk in range(4):
                        k = k4 * 4 + kk
                        c.ts(dst[:, k, tok0:tok0 + 128], tp[:, kk * 128:(kk + 1) * 128], gvec[:, k:k + 1], None, ALU.mult)

            for mt in range(2):
                m_ = HT_.next()
                c.dma(m_[:], io["mem"][mt * 128:(mt + 1) * 128, :])
                norm_T(m_, MNT, mt * 128, gm)
            for h in range(4):
                ps = PS.next()
                for k in range(16):
                    c.mm(ps[:, 0:256], WKV[:, k, h * 128:(h + 1) * 128], MNT[:, k, :], start=(k == 0), stop=(k == 15))
                c.copy(KT[:, h, :], ps[:, 0:256])
            for mt in range(2):
                ps = PS.next()
                for k in range(16):
                    c.mm(ps[:], MNT[:, k, mt * 128:(mt + 1) * 128], WKV[:, k, 512:1024], start=(k == 0), stop=(k == 15))
                c.copy(VM[:, mt, :], ps[:])
            HNT = Rot([c.sb([128, 16, 512], BF16, f"p4_hnt{i}") for i in range(1)])
            HQ = [c.sb([128, 2048], F32, f"p4_hq{i}") for i in range(4)]
            QT = Rot([c.sb([128, 512], BF16, f"p4_qt{i}") for i in range(2)])
            PTm = Rot([c.sb([128, 512], BF16, f"p4_pt{i}") for i in range(4)])
            OT = c.sb([128, 4, 512], BF16, "p4_ot")
            RD = Rot([c.sb([128, 512], F32, f"p4_rd{i}") for i in range(2)])
            xs = float(128 ** -0.5)
            for quad in range(2):
                hnt = HNT.next()
                for t4 in range(4):
                    tt = quad * 4 + t4
                    c.dma(HQ[t4][:], S["H"][tt * 128:(tt + 1) * 128, :])
                    norm_T(HQ[t4], hnt, t4 * 128, gx)
                for h in range(4):
                    ps = PS.next()
                    for k in range(16):
                        c.mm(ps[:], WQ[:, k, h * 128:(h + 1) * 128], hnt[:, k, :], start=(k == 0), stop=(k == 15))
                    qt = QT.next()
                    c.copy(qt[:], ps[:])
                    pts = []
                    for mt in range(2):
                        sp = PS.next()
                        c.mm(sp[:], KT[:, h, mt * 128:(mt + 1) * 128], qt[:])
                        pt = PTm.next()
                        c.act(pt[:], sp[:], AF.Exp, scale=xs)
                        pts.append(pt)
                    op_ = PS.next(); dp_ = PS.next()
                    for mt in range(2):
                        c.mm(op_[:], VM[:, mt, h * 128:(h + 1) * 128], pts[mt][:], start=(mt == 0), stop=(mt == 1))
                    for mt in range(2):
                        c.mm(dp_[:], ones[:], pts[mt][:], start=(mt == 0), stop=(mt == 1))
                    rd = RD.next()
                    c.recip(rd[:], dp_[:])
                    c.tt(OT[:, h, :], op_[:], rd[:], ALU.mult)
                for t4 in range(4):
                    tt = quad * 4 + t4
                    for cn in range(4):
                        ps = PS.next()
                        for h in range(4):
                            c.mm(ps[:], OT[:, h, t4 * 128:(t4 + 1) * 128], WXO[:, h, cn * 512:(cn + 1) * 512],
                                 start=(h == 0), stop=(h == 3))
                        c.tt(HQ[t4][:, cn * 512:(cn + 1) * 512], HQ[t4][:, cn * 512:(cn + 1) * 512], ps[:], ALU.add)
                    c.dma(S["H2"][tt * 128:(tt + 1) * 128, :], HQ[t4][:], eng="pool")
        c.barrier()
    c.stack = top

def bcast_ap(ap, dims):
    return bass.AP(tensor=ap.tensor, offset=ap.offset, ap=[list(ap.ap[0])] + [list(d) for d in dims])


class ExpertConv:
    def __init__(self, c, io, S):
        self.c, self.io, self.S = c, io, S

    def emit(self):
        c, io, S = self.c, self.io, self.S
        for r in range(16):
            for (src, dst) in (("expert_u", "UB"), ("expert_v", "VB")):
                c.dma(S[dst][r * 1024:(r + 1) * 1024, :], io[src][r * 1024:(r + 1) * 1024, :], eng="pool")

    def close(self):
        pass


def phase5(c, io, S, out_t):
    nc = c.nc
    top = c.stack
    with contextlib.ExitStack() as ph:
        c.stack = ph
        ident = c.sb([128, 128], F32, "p5_id")
        c.dma(ident[:], io["ident"][:])
        IDB = c.sb([128, 128], BF16, "p5_idb")
        c.copy(IDB[:], ident[:])
        WPQ = c.sb([128, 16, 2048], BF16, "p5_wpq")
        with contextlib.ExitStack() as pl:
            c.stack = pl
            STGW = Rot([c.sb([128, 2048], F32, f"p5_stg{i}") for i in range(2)])
            load_w_bf16(c, WPQ, io["w_pq"], io["w_pq"].ap, STGW, 16, 2048)
            c.barrier()
        c.stack = ph
        SKF = c.sb([128, 2, 128], F32, "p5_skf")
        SKT = c.sb([128, 2, 128], BF16, "p5_skt")
        c.dma(SKF[:, 0, :], io["sk1_T"][:, :])
        c.dma(SKF[:, 1, :], io["sk2_T"][:, :])
        c.copy(SKT[:], SKF[:])
        GBC = c.sb([128, D], F32, "p5_gbc")
        GFIN = c.sb([128, D], F32, "p5_gfin")
        c.dma(GBC[:], io["g_ffn_bc"][:, :])
        c.dma(GFIN[:], io["g_fin_bc"][:, :])
        HT2 = [c.sb([128, D], F32, f"p5_ht{i}") for i in range(2)]
        XN32 = [c.sb([128, D], F32, f"p5_xn3{i}") for i in range(2)]
        OUTB = c.sb([128, D], F32, "p5_outb")
        XB = c.sb([128, D], BF16, "p5_xb")
        XT3 = c.sb([128, 16, 128], BF16, "p5_xt3")
        QT = c.sb([128, 16, 128], BF16, "p5_qt")
        SC = c.sb([128, 16, 128], F32, "p5_sc")
        WK = c.sb([128, 256], F32, "p5_wk")
        V16 = c.sb([128, 16, 16], F32, "p5_v16")
        I16 = c.sb([128, 16, 16], U32, "p5_i16")
        IF = c.sb([128, 16, 16], F32, "p5_if")
        CA = c.sb([128, 256], F32, "p5_ca")
        EI = c.sb([128, 256], F32, "p5_ei")
        JK2 = c.sb([128, 256], F32, "p5_jk2")
        VS = c.sb([128, 16], F32, "p5_vs")
        NB = c.sb([128, 1], F32, "p5_nb")
        ZS = c.sb([128, 1], F32, "p5_zs")
        EIDS = c.sb([128, 128], F32, "p5_eids")
        EI322 = [c.sb([128, 128], I32, f"p5_ei32{i}") for i in range(2)]
        GW2 = [c.sb([128, 128], F32, f"p5_gw{i}") for i in range(2)]
        AA2 = [c.sb([128, 128], F32, f"p5_aa{i}") for i in range(2)]
        G1 = c.sb([128, 128], F32, "p5_g1")
        G2 = c.sb([128, 128], F32, "p5_g2")
        WT2 = [c.sb([128, 128], F32, f"p5_wt{i}") for i in range(2)]
        RR = c.sb([128, 1], F32, "p5_rr")
        RR2 = c.sb([128, 1], F32, "p5_rr2")
        UG = Rot([c.sb([128, D], BF16, f"p5_ug{i}") for i in range(4)])
        VG = Rot([c.sb([128, D], BF16, f"p5_vg{i}") for i in range(4)])
        TB = Rot([c.sb([128, D], BF16, f"p5_tb{i}") for i in range(2)])
        JK = c.sb([128, D], BF16, "p5_jk")
        JKA = c.sb([128, D], BF16, "p5_jka")
        YP = c.ps([128, D], F32, "p5_yp")
        PS = Rot([c.ps([128, 512], F32, f"p5_ps{i}") for i in range(2)])
        PSB = c.ps([128, 1024], BF16, "p5_psb")

        def top16(src, ncol, mv, iu):
            c.op("dve", lambda: nc.vector.max(out=mv.ap[:, 0:8], in_=src.ap), reads=[src], writes=[mv])
            if iu is not None:
                c.op("dve", lambda: nc.vector.max_index(out=iu.ap[:, 0:8], in_max=mv.ap[:, 0:8], in_values=src.ap),
                     reads=[src, mv], writes=[iu])
            c.op("dve", lambda: nc.vector.match_replace(out=WK.ap[:, 0:ncol], in_to_replace=mv.ap[:, 0:8],
                                                        in_values=src.ap, imm_value=-3.0e38),
                 reads=[src, mv], writes=[WK])
            c.op("dve", lambda: nc.vector.max(out=mv.ap[:, 8:16], in_=WK.ap[:, 0:ncol]), reads=[WK], writes=[mv])
            if iu is not None:
                c.op("dve", lambda: nc.vector.max_index(out=iu.ap[:, 8:16], in_max=mv.ap[:, 8:16],
                                                        in_values=WK.ap[:, 0:ncol]),
                     reads=[WK, mv], writes=[iu])

        def stage_a(tt):
            HT, XN3, EI32, GW = HT2[tt % 2], XN32[tt % 2], EI322[tt % 2], GW2[tt % 2]
            c.dma(HT[:], S["H2"][tt * 128:(tt + 1) * 128, :])
            rms_rows(c, RR[:], HT[:], JKA[:])
            c.stt(XN3[:], HT[:], RR[:, 0:1], GBC[:], ALU.mult, ALU.mult)
            c.copy(XB[:], XN3[:], e="act")
            for k4 in range(4):
                for kk in range(4):
                    k = k4 * 4 + kk
                    c.transpose(PSB[:, kk * 128:(kk + 1) * 128], XB[:, k * 128:(k + 1) * 128], IDB[:])
                c.copy(V(XT3, XT3.ap[:, k4 * 4:(k4 + 1) * 4, :].rearrange("p a b -> p (a b)")), PSB[:, 0:512])
            for c4 in range(4):
                ps = PS.next()
                for cc_ in range(4):
                    cq = c4 * 4 + cc_
                    for k in range(16):
                        c.mm(ps[:, cc_ * 128:(cc_ + 1) * 128], WPQ[:, k, cq * 128:(cq + 1) * 128], XT3[:, k, :],
                             start=(k == 0), stop=(k == 15))
                c.copy(V(QT, QT.ap[:, c4 * 4:(c4 + 1) * 4, :].rearrange("p a b -> p (a b)")), ps[:], e="act")
            for c4 in range(4):
                ps = PS.next()
                for cc_ in range(4):
                    cq = c4 * 4 + cc_
                    c.mm(ps[:, cc_ * 128:(cc_ + 1) * 128], QT[:, cq, :], SKT[:, cq % 2, :])
                c.copy(V(SC, SC.ap[:, c4 * 4:(c4 + 1) * 4, :].rearrange("p a b -> p (a b)")), ps[:], e="act")
            for cq in range(16):
                top16(SC[:, cq, :], 128, V(V16, V16.ap[:, cq, :]), V(I16, I16.ap[:, cq, :]))
            c.copy(IF[:], I16[:])
            c.memset(EIDS[:], 0.0)
            for h in range(8):
                a0 = V(V16, bcast_ap(V16.ap[:, 2 * h, :], [[1, 16], [0, 16]]))
                a1 = V(V16, bcast_ap(V16.ap[:, 2 * h + 1, :], [[0, 16], [1, 16]]))
                c.tt(V(CA, CA.ap.rearrange("p (a b) -> p a b", b=16)), a0, a1, ALU.add)
                e0 = V(IF, bcast_ap(IF.ap[:, 2 * h, :], [[1, 16], [0, 16]]))
                e1 = V(IF, bcast_ap(IF.ap[:, 2 * h + 1, :], [[0, 16], [1, 16]]))
                c.stt(V(EI, EI.ap.rearrange("p (a b) -> p a b", b=16)), e0, 128.0, e1, ALU.mult, ALU.add)
                top16(CA[:], 256, VS[:], None)
                for k in range(16):
                    c.stt(JK2[:], CA[:], VS[:, k:k + 1], EI[:], ALU.is_equal, ALU.mult,
                          accum=EIDS[:, h * 16 + k:h * 16 + k + 1])
                c.ts(NB[:], VS[:, 0:1], -1.0, None, ALU.mult)
                c.memset(ZS[:], 0.0)
                c.act(GW[:, h * 16:(h + 1) * 16], VS[:], AF.Exp, bias=NB[:, 0:1], accum=ZS[:])
                c.recip(ZS[:], ZS[:])
                c.ts(GW[:, h * 16:(h + 1) * 16], GW[:, h * 16:(h + 1) * 16], ZS[:, 0:1], None, ALU.mult)
            c.ts(EIDS[:], EIDS[:], 0.0, float(128 * 128 - 1), ALU.max, ALU.min)
            c.copy(EI32[:], EIDS[:])
            c.memset(AA2[tt % 2][:], 0.0)

        def step_u(tt, j):
            EI32, XN3, AA = EI322[tt % 2], XN32[tt % 2], AA2[tt % 2]
            ug = UG.next()
            c.op("pool", lambda ug=ug, j=j: nc.gpsimd.indirect_dma_start(
                out=ug.ap[:, :], out_offset=None, in_=S["UB"].ap[:, :],
                in_offset=bass.IndirectOffsetOnAxis(ap=EI32.ap[:, j:j + 1], axis=0)),
                reads=[EI32, S["UB"]], writes=[ug], dma=True)
            c.stt(JK[:], ug[:], 1.0, XN3[:], ALU.mult, ALU.mult, accum=AA[:, j:j + 1])

        def stage_g(tt):
            WT = WT2[tt % 2]
            gelu_tanh(c, WT[:], AA2[tt % 2][:], G1[:], G2[:])
            c.tt(WT[:], WT[:], GW2[tt % 2][:], ALU.mult)

        def step_v(tt, j):
            EI32, WT = EI322[tt % 2], WT2[tt % 2]
            vg = VG.next()
            c.op("pool", lambda vg=vg, j=j: nc.gpsimd.indirect_dma_start(
                out=vg.ap[:, :], out_offset=None, in_=S["VB"].ap[:, :],
                in_offset=bass.IndirectOffsetOnAxis(ap=EI32.ap[:, j:j + 1], axis=0)),
                reads=[EI32, S["VB"]], writes=[vg], dma=True)
            tb = TB.next()
            c.act(tb[:], vg[:], AF.Copy, scale=WT[:, j:j + 1])
            for cn in range(4):
                c.mm(YP[:, cn * 512:(cn + 1) * 512], IDB[:], tb[:, cn * 512:(cn + 1) * 512],
                     start=(j == 0), stop=(j == 127))

        def stage_f(tt):
            HT = HT2[tt % 2]
            for cn in range(4):
                c.tt(HT[:, cn * 512:(cn + 1) * 512], HT[:, cn * 512:(cn + 1) * 512], YP[:, cn * 512:(cn + 1) * 512], ALU.add)
            rms_rows(c, RR2[:], HT[:], JKA[:])
            c.stt(OUTB[:], HT[:], RR2[:, 0:1], GFIN[:], ALU.mult, ALU.mult)
            c.dma(out_t[tt * 128:(tt + 1) * 128, :], OUTB[:])

        stage_a(0)
        for j in range(128):
            step_u(0, j)
        stage_g(0)
        for tt in range(8):
            if tt < 7:
                stage_a(tt + 1)
            for j in range(128):
                step_v(tt, j)
                if tt < 7:
                    step_u(tt + 1, j)
            stage_f(tt)
            if tt < 7:
                stage_g(tt + 1)
        c.barrier()
    c.stack = top

def scratch_spec():
    return {
        "XN": ([16, 128, KC, 512], BF16),
        "AQT": ([3, 4, 128, NOWN], BF16),
        "AKT": ([3, 4, 128, 3072], BF16),
        "AV": ([3, 3072, 512], BF16),
        "BQT": ([2, 64, 8, 8, 128], BF16),
        "KCT": ([128, NW], BF16),
        "VCT": ([128, NW], BF16),
        "KST": ([2, 64, NW], BF16),
        "VS": ([NW, 128], BF16),
        "KWT": ([2, 64, 1536], BF16),
        "VW": ([1536, 128], BF16),
        "BG": ([NOWN, 48], F32),
        "MG": ([32, 128, NOWN], BF16),
        "YAT": ([4, 128, NOWN], BF16),
        "YBT": ([8, 128, NOWN], BF16),
        "H": ([NOWN, D], F32),
        "H2": ([NOWN, D], F32),
        "UB": ([16384, D], BF16),
        "VB": ([16384, D], BF16),
    }


def input_spec():
    return {
        "xT": ([16, 128, KC, 512], F32),
        "w_in": ([D, IN_COLS], F32),
        "g_mix": ([128, KC], F32),
        "a_bias": ([128, AB_COLS], F32),
        "a_kvalid": ([128, 24], F32),
        "kaug_sel": ([8, NW], BF16), "kaug_cmp": ([8, 512], BF16), "qaug": ([2, 8, 8, 1024], BF16),
        "EE": ([128, NW], BF16), "TRI": ([128, 1024], BF16), "TRI2": ([128, 1024], BF16),
        "CM": ([8, 128, 1024], BF16), "OV": ([4, 128, 129], BF16), "ident": ([128, 128], F32),
        "cval": ([128, 4], F32), "wval": ([128, 12], F32),
        "smul": ([8, 128, 128], F32), "sadd": ([8, 128, 128], F32),
        "x_own": ([NOWN, D], F32), "mem": ([256, D], F32),
        "w_up_a": ([512, D], F32), "w_up_b": ([1024, D], F32), "w_out": ([D, D], F32),
        "w_xq": ([D, 512], F32), "w_xkv": ([D, 1024], F32), "w_xo": ([512, D], F32),
        "g_x": ([128, KC], F32), "g_mem": ([128, KC], F32),
        "w_pq": ([D, D], F32), "sk1_T": ([128, 128], F32), "sk2_T": ([128, 128], F32),
        "g_ffn_bc": ([128, D], F32), "g_fin_bc": ([128, D], F32),
        "expert_u": ([16384, D], F32), "expert_v": ([16384, D], F32),
        "w_cmp_k1": ([2048, 128], F32), "w_cmp_k2": ([128, 64], F32), "pe_k_T": ([64, 32], F32),
        "w_cmp_v1": ([2048, 128], F32), "w_cmp_v2": ([128, 64], F32), "pe_v_T": ([64, 32], F32),
    }


def build(debug_out=(), upto=99, skip=()):
    nc = bass.Bass("TRN2", target_bir_lowering=False)
    with contextlib.ExitStack() as st:
        c = Ctx(nc, st)
        io = {k: c.dram(k, shp, dt, kind="ExternalInput") for k, (shp, dt) in input_spec().items()}
        S = {}
        for k, (shp, dt) in scratch_spec().items():
            S[k] = c.dram("s_" + k, shp, dt, kind=("ExternalOutput" if k in debug_out else "Internal"))
        conv = ExpertConv(c, io, S)
        phase1(c, io, S)
        if upto >= 2 and 2 not in skip:
            phase2(c, io, S)
        if upto >= 3 and 3 not in skip:
            phase3(c, io, S, after_loads=conv.emit)
        else:
            conv.emit()
        c.barrier()
        conv.close()
        if upto >= 4 and 4 not in skip:
            phase4(c, io, S)
        out_t = c.dram("out", [NOWN, D], F32, kind="ExternalOutput")
        if upto >= 5:
            phase5(c, io, S, out_t)
        c.finish([])
    return nc, c


_CACHE = {}


def _gl(v):
    return np.ascontiguousarray(np.asarray(v, np.float32).reshape(16, 128).T)


def make_inputs(inp, cores=range(8)):
    x = np.asarray(inp["x"], np.float32)[0]
    xT = np.ascontiguousarray(x.T)
    st = host_b_static()
    shared = {
        "w_in": np.asarray(inp["w_in"], np.float32)[0],
        "g_mix": _gl(inp["norm_mix"][0]),
        "mem": np.asarray(inp["mem"], np.float32)[0],
        "g_x": _gl(inp["norm_x"][0]), "g_mem": _gl(inp["norm_mem"][0]),
        "pe_k_T": np.ascontiguousarray(np.asarray(inp["pe_cmp_k"], np.float32)[0].T),
        "pe_v_T": np.ascontiguousarray(np.asarray(inp["pe_cmp_v"], np.float32)[0].T),
        "sk1_T": np.ascontiguousarray(np.asarray(inp["sub_keys1"], np.float32)[0].T),
        "sk2_T": np.ascontiguousarray(np.asarray(inp["sub_keys2"], np.float32)[0].T),
        "g_ffn_bc": np.ascontiguousarray(np.broadcast_to(np.asarray(inp["norm_ffn"], np.float32)[0][None, :], (128, D))),
        "g_fin_bc": np.ascontiguousarray(np.broadcast_to(np.asarray(inp["norm_final"], np.float32)[None, :], (128, D))),
        "expert_u": np.asarray(inp["expert_u"], np.float32)[0],
        "expert_v": np.asarray(inp["expert_v"], np.float32)[0],
    }
    for k in ("w_cmp_k1", "w_cmp_k2", "w_cmp_v1", "w_cmp_v2", "w_up_a", "w_up_b", "w_out", "w_xq", "w_xkv", "w_xo", "w_pq"):
        shared[k] = np.asarray(inp[k], np.float32)[0]
    shared.update(st)
    maps = []
    for cc in cores:
        lo = 1024 * cc - 7168
        w = np.zeros((D, NW), np.float32)
        if lo >= 0:
            w[:] = xT[:, lo:lo + NW]
        else:
            w[:, -lo:] = xT[:, :NW + lo]
        tab, kval = host_a_tables(cc)
        d = dict(shared)
        w = np.ascontiguousarray(w.reshape(KC, 128, 16, 512).transpose(2, 1, 0, 3))
        d.update({"xT": w, "x_own": np.ascontiguousarray(x[1024 * cc:1024 * cc + 1024]), "a_bias": tab, "a_kvalid": kval})
        d.update(host_b_core(cc))
        maps.append(d)
    return maps


def kernel(**inputs):
    if "nc" not in _CACHE:
        _CACHE["nc"] = build()[0]
    nc = _CACHE["nc"]
    maps = make_inputs(inputs)
    res = run_bass_kernel_spmd(nc, maps, core_ids=list(range(8)))
    out = np.concatenate([np.asarray(r["out"], np.float32) for r in res.results], axis=0)
    return out.reshape(1, 8192, D)
```
